# Optimizing a Trainium2 kernel written in Bass

```python
import math
import jax, jax.numpy as jnp
from jax import lax
import numpy as np

D_MODEL = 1024
BATCH = 16
SEQ = 2048
DEPTH = 4

HEAD_DIM = 64
SWA_Q_HEADS = 8
SWA_KV_HEADS = 2
SWA_GROUP = SWA_Q_HEADS // SWA_KV_HEADS
SWA_WINDOW = 128
SB_HEADS = 8
BLOCK = 128
N_EXPERTS = 32
N_GROUPS = 8
EXPERTS_PER_GROUP = N_EXPERTS // N_GROUPS
TOP_K = 2
D_EXPERT = 512
MOE_BLOCK = 128
LN_EPS = 1e-5
DEEPNORM_ALPHA = (2 * DEPTH) ** 0.25
DEEPNORM_BETA = (8 * DEPTH) ** -0.25
PROJ_SIZES = (SWA_Q_HEADS * HEAD_DIM, SWA_KV_HEADS * HEAD_DIM, SWA_KV_HEADS * HEAD_DIM,
              SB_HEADS * HEAD_DIM, SB_HEADS * HEAD_DIM, SB_HEADS * HEAD_DIM,
              D_MODEL, D_MODEL)
VALUE_SLOTS = (2, 5)

kernel_name = "hybrid_swa_stickbreak_groupmoe_deepnorm"


def layer_norm(x, g, b):
    xf = x.astype(jnp.float32)
    mu = xf.mean(-1, keepdims=True)
    var = jnp.square(xf - mu).mean(-1, keepdims=True)
    y = (xf - mu) * lax.rsqrt(var + LN_EPS) * g.astype(jnp.float32) + b.astype(jnp.float32)
    return y.astype(x.dtype)


def alibi_slopes(n_heads):
    return jnp.exp2(-8.0 * (jnp.arange(n_heads, dtype=jnp.float32) + 1.0) / n_heads)


def sliding_window_attention(q, k, v, sinks):
    B, S = q.shape[0], q.shape[1]
    nb = S // BLOCK
    qb = q.reshape(B, nb, BLOCK, SWA_KV_HEADS, SWA_GROUP, HEAD_DIM).astype(jnp.float32)

    def with_prev(t):
        tb = t.reshape(B, nb, BLOCK, SWA_KV_HEADS, HEAD_DIM)
        prev = jnp.pad(tb[:, :-1], ((0, 0), (1, 0), (0, 0), (0, 0), (0, 0)))
        return jnp.concatenate([prev, tb], axis=2).astype(jnp.float32)

    kb, vb = with_prev(k), with_prev(v)
    scores = jnp.einsum('bnqhgd,bnkhd->bnhgqk', qb, kb) * (HEAD_DIM ** -0.5)
    i = jnp.arange(BLOCK)[:, None]
    j = jnp.arange(2 * BLOCK)[None, :]
    dist = i + BLOCK - j
    blk = jnp.arange(nb)[:, None, None]
    valid = (dist >= 0) & (dist < SWA_WINDOW) & ((blk - 1) * BLOCK + j[None] >= 0)
    slopes = alibi_slopes(SWA_Q_HEADS).reshape(SWA_KV_HEADS, SWA_GROUP)
    scores = scores - slopes[:, :, None, None] * dist.astype(jnp.float32)
    scores = jnp.where(valid[None, :, None, None], scores, -jnp.inf)
    sink = sinks.astype(jnp.float32).reshape(SWA_KV_HEADS, SWA_GROUP)[:, :, None, None]
    m = jnp.maximum(scores.max(-1, keepdims=True), sink)
    p = jnp.exp(scores - m)
    probs = p / (p.sum(-1, keepdims=True) + jnp.exp(sink - m))
    out = jnp.einsum('bnhgqk,bnkhd->bnqhgd', probs, vb)
    return out.reshape(B, S, SWA_Q_HEADS * HEAD_DIM)


def stick_breaking_attention(q, k, v):
    B, S = q.shape[0], q.shape[1]
    qf, kf, vf = q.astype(jnp.float32), k.astype(jnp.float32), v.astype(jnp.float32)
    outs = []
    for t0 in range(0, S, BLOCK):
        n_keys = t0 + BLOCK
        z = jnp.einsum('bqhd,bkhd->bhqk', qf[:, t0:t0 + BLOCK], kf[:, :n_keys]) * (HEAD_DIM ** -0.5)
        strict = jnp.arange(n_keys)[None, :] < (t0 + jnp.arange(BLOCK))[:, None]
        log_keep = jnp.where(strict, jax.nn.log_sigmoid(-z), 0.0)
        between = lax.cumsum(log_keep, axis=3, reverse=True) - log_keep
        a = jnp.where(strict, jnp.exp(jax.nn.log_sigmoid(z) + between), 0.0)
        outs.append(jnp.einsum('bhqk,bkhd->bqhd', a, vf[:, :n_keys]))
    return jnp.concatenate(outs, axis=1).reshape(B, S, SB_HEADS * HEAD_DIM)


def token_mixer(x, w_in, b_in, sinks, w_branch_a, w_branch_b, w_out):
    B, S, _ = x.shape
    proj = jnp.einsum('bsd,dn->bsn', x, w_in) + b_in
    cuts, acc = [], 0
    for n in PROJ_SIZES[:-1]:
        acc += n
        cuts.append(acc)
    a_q, a_k, a_v, s_q, s_k, s_v, g_a, g_b = jnp.split(proj, cuts, axis=-1)
    y_a = sliding_window_attention(a_q.reshape(B, S, SWA_Q_HEADS, HEAD_DIM),
                                   a_k.reshape(B, S, SWA_KV_HEADS, HEAD_DIM),
                                   a_v.reshape(B, S, SWA_KV_HEADS, HEAD_DIM), sinks).astype(x.dtype)
    y_b = stick_breaking_attention(s_q.reshape(B, S, SB_HEADS, HEAD_DIM),
                                   s_k.reshape(B, S, SB_HEADS, HEAD_DIM),
                                   s_v.reshape(B, S, SB_HEADS, HEAD_DIM)).astype(x.dtype)
    merged = (jax.nn.sigmoid(g_a) * jnp.einsum('bsc,cd->bsd', y_a, w_branch_a)
              + jax.nn.sigmoid(g_b) * jnp.einsum('bsc,cd->bsd', y_b, w_branch_b))
    return jnp.einsum('bsd,de->bse', merged, w_out)


def group_route(x2d, w_router, router_bias):
    T = x2d.shape[0]
    aff = jax.nn.sigmoid(x2d.astype(jnp.float32) @ w_router.astype(jnp.float32))
    biased = (aff + router_bias.astype(jnp.float32)).reshape(T, N_GROUPS, EXPERTS_PER_GROUP)
    group_score = lax.top_k(biased, TOP_K)[0].sum(-1)
    g_sel = jnp.argmax(group_score, axis=-1)
    in_group = jnp.take_along_axis(biased, g_sel[:, None, None], axis=1)[:, 0]
    _, local = lax.top_k(in_group, TOP_K)
    idx = g_sel[:, None] * EXPERTS_PER_GROUP + local
    w = jnp.take_along_axis(aff, idx, axis=1)
    return idx, w / w.sum(-1, keepdims=True)


def moe(x, w_router, router_bias, w_gate, w_up, w_down):
    B, S, D = x.shape
    T = B * S
    x2d = x.reshape(T, D)
    idx, gates = group_route(x2d, w_router, router_bias)
    flat_e = idx.reshape(-1)
    flat_tok = jnp.repeat(jnp.arange(T, dtype=jnp.int32), TOP_K)
    flat_gate = gates.reshape(-1)
    order = jnp.argsort(flat_e)
    sorted_e = flat_e[order]
    counts = jnp.zeros((N_EXPERTS,), jnp.int32).at[flat_e].add(1)
    padded = ((counts + MOE_BLOCK - 1) // MOE_BLOCK) * MOE_BLOCK
    pad_end = jnp.cumsum(padded)
    pad_start = pad_end - padded
    start = jnp.cumsum(counts) - counts
    rank = jnp.arange(T * TOP_K) - start[sorted_e]
    dest = pad_start[sorted_e] + rank
    n_blocks = -(-(T * TOP_K) // MOE_BLOCK) + N_EXPERTS
    P = n_blocks * MOE_BLOCK
    buf_tok = jnp.full((P,), T, jnp.int32).at[dest].set(flat_tok[order])
    buf_gate = jnp.zeros((P,), jnp.float32).at[dest].set(flat_gate[order])
    block_expert = jnp.minimum(
        jnp.searchsorted(pad_end, jnp.arange(n_blocks) * MOE_BLOCK, side='right'), N_EXPERTS - 1)
    xpad = jnp.concatenate([x2d, jnp.zeros((1, D), x2d.dtype)], axis=0)
    xblocks = xpad[buf_tok].reshape(n_blocks, MOE_BLOCK, D)

    def expert_block(args):
        xb, e = args
        h = jax.nn.silu(xb @ w_gate[e]) * (xb @ w_up[e])
        return h @ w_down[e]

    yb = lax.map(expert_block, (xblocks, block_expert)).reshape(P, D)
    y = jax.ops.segment_sum(yb.astype(jnp.float32) * buf_gate[:, None], buf_tok, num_segments=T + 1)[:T]
    return y.astype(x.dtype).reshape(B, S, D)


def setup_inputs(seed: int = 0) -> dict:
    key = jax.random.key(seed)
    ks = jax.random.split(key, 16)
    n_in = sum(PROJ_SIZES)
    col_scale = jnp.concatenate([
        jnp.full((n,), DEEPNORM_BETA if i in VALUE_SLOTS else 1.0, jnp.float32)
        for i, n in enumerate(PROJ_SIZES)])
    d_a = SWA_Q_HEADS * HEAD_DIM
    d_b = SB_HEADS * HEAD_DIM
    nrm = lambda k, shape: jax.random.normal(k, shape, jnp.float32)
    return {
        "x": nrm(ks[0], (BATCH, SEQ, D_MODEL)),
        "w_in": nrm(ks[1], (DEPTH, D_MODEL, n_in)) * (D_MODEL ** -0.5) * col_scale,
        "b_in": 0.02 * nrm(ks[2], (DEPTH, n_in)),
        "attn_sinks": nrm(ks[3], (DEPTH, SWA_Q_HEADS)),
        "w_branch_a": nrm(ks[4], (DEPTH, d_a, D_MODEL)) * (d_a ** -0.5) * DEEPNORM_BETA,
        "w_branch_b": nrm(ks[5], (DEPTH, d_b, D_MODEL)) * (d_b ** -0.5) * DEEPNORM_BETA,
        "w_out": nrm(ks[6], (DEPTH, D_MODEL, D_MODEL)) * (D_MODEL ** -0.5) * DEEPNORM_BETA,
        "ln1_g": 1.0 + 0.02 * nrm(ks[7], (DEPTH, D_MODEL)),
        "ln1_b": 0.02 * nrm(ks[8], (DEPTH, D_MODEL)),
        "w_router": nrm(ks[9], (D_MODEL, N_EXPERTS)) * (D_MODEL ** -0.5),
        "router_bias": 0.01 * nrm(ks[10], (N_EXPERTS,)),
        "w_gate": nrm(ks[11], (DEPTH, N_EXPERTS, D_MODEL, D_EXPERT)) * (D_MODEL ** -0.5),
        "w_up": nrm(ks[12], (DEPTH, N_EXPERTS, D_MODEL, D_EXPERT)) * (D_MODEL ** -0.5) * DEEPNORM_BETA,
        "w_down": nrm(ks[13], (DEPTH, N_EXPERTS, D_EXPERT, D_MODEL)) * (D_EXPERT ** -0.5) * DEEPNORM_BETA,
        "ln2_g": 1.0 + 0.02 * nrm(ks[14], (DEPTH, D_MODEL)),
        "ln2_b": 0.02 * nrm(ks[15], (DEPTH, D_MODEL)),
    }


def reference(x, w_in, b_in, attn_sinks, w_branch_a, w_branch_b, w_out, ln1_g, ln1_b,
              w_router, router_bias, w_gate, w_up, w_down, ln2_g, ln2_b):
    for l in range(DEPTH):
        mix = token_mixer(x, w_in[l], b_in[l], attn_sinks[l], w_branch_a[l], w_branch_b[l], w_out[l])
        x = layer_norm(DEEPNORM_ALPHA * x + mix, ln1_g[l], ln1_b[l])
        ffn = moe(x, w_router, router_bias, w_gate[l], w_up[l], w_down[l])
        x = layer_norm(DEEPNORM_ALPHA * x + ffn, ln2_g[l], ln2_b[l])
    return x
```

```python
from contextlib import ExitStack

import ml_dtypes
import numpy as np

import concourse.bass as bass
import concourse.mybir as mybir
from concourse.bass_utils import run_bass_kernel_spmd

F32 = mybir.dt.float32
BF16 = mybir.dt.bfloat16
I32 = mybir.dt.int32
U8 = mybir.dt.uint8
ALU = mybir.AluOpType
AF = mybir.ActivationFunctionType
AX = mybir.AxisListType
NPBF = ml_dtypes.bfloat16

D = 1024
NE = 32
DE = 512
NPROJ = 4352
ALPHA = float((2 * 4) ** 0.25)
EPS = 1e-5
NEG = -30000.0
QPERM = [0, 4, 1, 5, 2, 6, 3, 7]
SBUF_BYTES = 200 * 1024


class Buf:
    __slots__ = ("w", "r")

    def __init__(self):
        self.w = {}
        self.r = {}


class TT:
    __slots__ = ("ap", "buf")

    def __init__(self, ap, buf=None):
        self.ap = ap
        self.buf = buf if buf is not None else Buf()


class Rot:
    def __init__(self, items):
        self.items = items
        self.i = 0

    def next(self):
        it = self.items[self.i % len(self.items)]
        self.i += 1
        return it


class Tracker:
    COMPUTE = ("pe", "act", "dve", "pool")
    ALL = ("pe", "act", "dve", "pool", "sp")

    def __init__(self, nc, stack, n_dma_sems=8):
        self.nc = nc
        self.stack = stack
        self.ops = {e: [] for e in self.ALL}
        self.sem = {}
        self.cnt = {}
        self.nsem = 0
        self.seen = {e: {} for e in self.ALL}
        for e in self.COMPUTE:
            self._new_sem(e)
        self.dpool = {}
        self.dnext = {}
        for q in ("sp", "act", "pool"):
            self.dpool[q] = [[self._alloc(f"d_{q}_{i}"), 0] for i in range(n_dma_sems)]
            self.dnext[q] = 0
        self.bsem = self._alloc("barrier")
        self.bcount = 0
        self.ninstr = {e: 0 for e in self.ALL}

    def _alloc(self, name):
        self.nsem += 1
        return self.stack.enter_context(self.nc.semaphore(name))

    def _new_sem(self, e):
        self.sem[e] = self._alloc(f"c_{e}_{self.nsem}")
        self.cnt[e] = 0

    def _wait(self, eng, s, v):
        if self.seen[eng].get(s, 0) >= v:
            return
        self.seen[eng][s] = v
        self.ops[eng].append(lambda E, s=s, v=v: E.wait_ge(s, v))
        self.ninstr[eng] += 1

    @staticmethod
    def _deps(reads, writes):
        deps = {}
        for b in reads:
            for s, v in b.w.items():
                if deps.get(s, 0) < v:
                    deps[s] = v
        for b in writes:
            for d in (b.w, b.r):
                for s, v in d.items():
                    if deps.get(s, 0) < v:
                        deps[s] = v
        return deps

    def op(self, eng, fn, reads=(), writes=()):
        deps = self._deps(reads, writes)
        own = self.sem[eng]
        for s, v in deps.items():
            if eng == "pe" and s is own:
                continue
            self._wait(eng, s, v)
        self.cnt[eng] += 1
        v = self.cnt[eng]
        self.ops[eng].append(lambda E, fn=fn, own=own: fn(E).then_inc(own, 1))
        self.ninstr[eng] += 1
        for b in reads:
            if b.r.get(own, 0) < v:
                b.r[own] = v
        for b in writes:
            b.w = {own: v}
            b.r = {}
        if v >= 60000:
            self._new_sem(eng)

    def dma(self, q, fn, reads=(), writes=(), swrites=()):
        deps = self._deps(reads, writes)
        for b in swrites:
            for s, v in b.r.items():
                if deps.get(s, 0) < v:
                    deps[s] = v
        for s, v in deps.items():
            self._wait(q, s, v)
        slot = self.dpool[q][self.dnext[q]]
        self.dnext[q] = (self.dnext[q] + 1) % len(self.dpool[q])
        s = slot[0]
        if slot[1] > 0:
            self._wait(q, s, slot[1])
        slot[1] += 16
        v = slot[1]
        assert v < 65000
        self.ops[q].append(lambda E, fn=fn, s=s: fn(E).then_inc(s, 16))
        self.ninstr[q] += 1
        for b in reads:
            if b.r.get(s, 0) < v:
                b.r[s] = v
        for b in writes:
            b.w = {s: v}
            b.r = {}
        for b in swrites:
            b.w[s] = v

    def barrier(self):
        self.bcount += len(self.ALL)
        bs, bc = self.bsem, self.bcount
        assert bc < 65000
        for e in self.ALL:
            if e in self.COMPUTE and self.cnt[e] > 0:
                self._wait(e, self.sem[e], self.cnt[e])
            if e in self.dpool:
                for s, v in self.dpool[e]:
                    if v > 0:
                        self._wait(e, s, v)
            self.ops[e].append(lambda E: E.sem_inc(bs, 1))
            self.ops[e].append(lambda E: E.wait_ge(bs, bc))
        floor = {}
        for e in self.COMPUTE:
            floor[self.sem[e]] = self.cnt[e]
        for q in self.dpool:
            for s, v in self.dpool[q]:
                floor[s] = v
        for e in self.ALL:
            for s, v in floor.items():
                if self.seen[e].get(s, 0) < v:
                    self.seen[e][s] = v

    def finish(self, block):
        self.barrier()
        ops = self.ops

        @block.tensor
        def _(E):
            for f in ops["pe"]:
                f(E)

        @block.scalar
        def _(E):
            for f in ops["act"]:
                f(E)

        @block.vector
        def _(E):
            for f in ops["dve"]:
                f(E)

        @block.gpsimd
        def _(E):
            for f in ops["pool"]:
                f(E)

        @block.sync
        def _(E):
            for f in ops["sp"]:
                f(E)


class Carver:
    def __init__(self, big, nbytes):
        self.big = big
        self.nbytes = nbytes
        self.off = 0
        self.marks = []
        self.peak = 0

    def mark(self):
        self.marks.append(self.off)

    def release(self):
        self.off = self.marks.pop()

    def alloc(self, shape, dtype, buf=None):
        esz = {F32: 4, BF16: 2, I32: 4}[dtype]
        n = int(np.prod(shape))
        nb = n * esz
        self.off = (self.off + 63) // 64 * 64
        assert self.off + nb <= self.nbytes, f"SBUF carve overflow {self.off}+{nb}>{self.nbytes}"
        ap = self.big[:, self.off:self.off + nb].bitcast(dtype)
        self.off += nb
        self.peak = max(self.peak, self.off)
        if len(shape) == 2:
            ap = ap.rearrange("p (a b) -> p a b", b=shape[1])
        elif len(shape) == 3:
            ap = ap.rearrange("p (a b c) -> p a b c", b=shape[1], c=shape[2])
        return TT(ap, buf)


class Prog:
    def __init__(self, S, NB, L, debug=False):
        self.S, self.NB, self.L, self.debug = S, NB, L, debug
        self.T = S * NB
        self.NT = self.T // 128
        self.NQB = S // 128
        self.NG = S // 512
        self.NBLK = 2 * self.NT + NE
        self.NCF = 128 + 8 * 256 + 1 + self.NBLK + 2

    def mm(self, out, lhsT, rhs, start, stop, reads, writes):
        self.tr.op("pe", lambda E: E.matmul(out, lhsT=lhsT, rhs=rhs, start=start, stop=stop), reads, writes)

    def tp(self, out, in_, ident, reads, writes):
        self.tr.op("pe", lambda E: E.transpose(out=out, in_=in_, identity=ident), reads, writes)

    def act(self, out, in_, func, reads, writes, bias=0.0, scale=1.0, accum=None):
        if accum is None:
            self.tr.op("act", lambda E: E.activation(out=out, in_=in_, func=func, bias=bias, scale=scale), reads, writes)
        else:
            self.tr.op("act", lambda E: E.activation(out=out, in_=in_, func=func, bias=bias, scale=scale,
                                                     accum_out=accum), reads, writes)

    def tt(self, eng, out, in0, in1, op, reads, writes):
        self.tr.op(eng, lambda E: E.tensor_tensor(out=out, in0=in0, in1=in1, op=op), reads, writes)

    def ts(self, eng, out, in0, s1, s2, op0, op1, reads, writes):
        if s2 is None:
            self.tr.op(eng, lambda E: E.tensor_scalar(out=out, in0=in0, scalar1=s1, scalar2=None, op0=op0), reads, writes)
        else:
            self.tr.op(eng, lambda E: E.tensor_scalar(out=out, in0=in0, scalar1=s1, scalar2=s2, op0=op0, op1=op1),
                       reads, writes)

    def stt(self, eng, out, in0, scalar, in1, op0, op1, reads, writes):
        self.tr.op(eng, lambda E: E.scalar_tensor_tensor(out=out, in0=in0, scalar=scalar, in1=in1, op0=op0, op1=op1),
                   reads, writes)

    def cp(self, eng, out, in_, reads, writes):
        if eng == "act":
            self.tr.op("act", lambda E: E.activation(out=out, in_=in_, func=AF.Copy), reads, writes)
        else:
            self.tr.op(eng, lambda E: E.tensor_copy(out=out, in_=in_), reads, writes)

    def red(self, eng, out, in_, op, reads, writes):
        self.tr.op(eng, lambda E: E.tensor_reduce(out=out, in_=in_, axis=AX.X, op=op), reads, writes)

    def memset(self, eng, ap, val, writes):
        self.tr.op(eng, lambda E: E.memset(ap, val), (), writes)

    def dma(self, q, out, in_, reads, writes, swrites=()):
        self.tr.dma(q, lambda E: E.dma_start(out=out, in_=in_), reads, writes, swrites)

    def gather(self, out, src, idx, reads, writes):
        self.tr.dma("pool", lambda E: E.indirect_dma_start(
            out=out, out_offset=None, in_=src, in_offset=bass.IndirectOffsetOnAxis(ap=idx, axis=0)), reads, writes)

    def scatter(self, dst, idx, in_, reads, swrites):
        self.tr.dma("pool", lambda E: E.indirect_dma_start(
            out=dst, out_offset=bass.IndirectOffsetOnAxis(ap=idx, axis=0), in_=in_, in_offset=None), reads, (), swrites)

    def build(self):
        S, NB, L, T, NT, NBLK = self.S, self.NB, self.L, self.T, self.NT, self.NBLK
        nc = bass.Bass("TRN2", target_bir_lowering=False)
        self.nc = nc

        def din(name, shape, dt=F32):
            return nc.dram_tensor(name, list(shape), dt, kind="ExternalInput").ap()

        def dscr(name, shape, dt):
            return nc.dram_tensor(name, list(shape), dt, kind="Internal").ap()

        d = self.d = {}
        d["x"] = din("x", [T, D])
        d["w_in"] = din("w_in", [L, D, NPROJ])
        d["b_in"] = din("b_in", [L, NPROJ])
        d["b_in_fm"] = din("b_in_fm", [L, 128, 34])
        d["sinks"] = din("sinks", [L, 8])
        d["w_a"] = din("w_a", [L, 512, D])
        d["w_b"] = din("w_b", [L, 512, D])
        d["w_out"] = din("w_out", [L, D, D])
        for n in ("ln1_g", "ln1_b", "ln2_g", "ln2_b"):
            d[n] = din(n, [L, D])
        d["w_router"] = din("w_router", [D, NE])
        d["router_bias"] = din("router_bias", [1, NE])
        d["w_gate"] = din("w_gate", [L, NE, D, DE])
        d["w_up"] = din("w_up", [L, NE, D, DE])
        d["w_down"] = din("w_down", [L, NE, DE, D])
        d["cb"] = din("cb", [128, 6 * 128], BF16)
        d["cf"] = din("cf", [128, self.NCF])
        d["y"] = nc.dram_tensor("y", [T, D], F32, kind="ExternalOutput").ap()
        if self.debug:
            d["dbg_x1"] = nc.dram_tensor("dbg_x1", [T, D], F32, kind="ExternalOutput").ap()
            d["dbg_y"] = nc.dram_tensor("dbg_y", [T, D], BF16, kind="ExternalOutput").ap()
        d["xres"] = dscr("xres", [T, D], F32)
        d["x1"] = d["dbg_x1"] if self.debug else dscr("x1", [T, D], F32)
        d["x1b"] = dscr("x1b", [T, D], BF16)
        d["xT"] = dscr("xT", [NT * 128, D], BF16)
        d["xs"] = dscr("xs", [NBLK * 128, D], BF16)
        d["yb"] = dscr("yb", [NBLK * 128, D], F32)
        d["wg_d"] = dscr("wg_d", [NE * 128, 8 * DE], BF16)
        d["wu_d"] = dscr("wu_d", [NE * 128, 8 * DE], BF16)
        d["wd_d"] = dscr("wd_d", [NE * 128, 4 * D], BF16)
        self.db = {k: Buf() for k in d}

        with ExitStack() as st:
            big = st.enter_context(nc.sbuf_tensor("big", [128, SBUF_BYTES], U8))
            self.banks = []
            for i in range(8):
                t = st.enter_context(nc.psum_tensor(f"bank{i}", [128, 512], F32))
                self.banks.append(TT(t[:, :]))
            self.tr = Tracker(nc, st)
            self.cv = Carver(big, SBUF_BYTES)
            blk = st.enter_context(nc.Block())
            self.emit()
            self.tr.finish(blk)
            self.stats = dict(instr=dict(self.tr.ninstr), sems=self.tr.nsem, sbuf_peak=self.cv.peak)
        return nc

    def bank_bf(self, b):
        return self.banks[b].ap.bitcast(BF16).rearrange("p (a b) -> p a b", b=128)

    def emit(self):
        cv, d, db = self.cv, self.d, self.db
        NT, NBLK, L = self.NT, self.NBLK, self.L
        self.cb = cv.alloc([6, 128], BF16)
        self.cf = cv.alloc([self.NCF], F32)
        self.dma("sp", self.cb.ap, d["cb"].rearrange("p (a b) -> p a b", b=128), [], [self.cb.buf])
        self.dma("sp", self.cf.ap, d["cf"], [], [self.cf.buf])
        cbb = self.cb.buf
        self.ident = self.cb.ap[:, 0, :]
        self.wsuf = self.cb.ap[:, 1, :]
        self.negones = self.cb.ap[:, 2, :]
        self.negmask = self.cb.ap[:, 3, :]
        self.ustrict = self.cb.ap[:, 4, :]
        self.ones = self.cb.ap[:, 5, :]
        self.identf = self.cf.ap[:, 0:128]
        self.swab = self.cf.ap[:, 128:128 + 2048].rearrange("p (h k) -> p h k", k=256)
        o = 128 + 2048
        self.pidx = self.cf.ap[:, o:o + 1]
        self.bstart = self.cf.ap[:, o + 1:o + 1 + NBLK]
        self.aff = cv.alloc([NT, NE], F32)
        self.wr = cv.alloc([8, NE], F32)
        self.rb = cv.alloc([NE], F32)
        self.dma("sp", self.wr.ap, d["w_router"].rearrange("(k p) e -> p k e", p=128), [], [self.wr.buf])
        self.dma("sp", self.rb.ap, d["router_bias"][0, :].partition_broadcast(128), [], [self.rb.buf])

        cv.mark()
        self.phase_xT0()
        self.tr.barrier()
        cv.release()
        for l in range(L):
            self.precast_experts(l)
            for b in range(self.NB):
                cv.mark()
                self.alloc_seq()
                cv.mark()
                self.phase_inproj(l, b)
                self.tr.barrier()
                cv.release()
                cv.mark()
                self.phase_swa(l, b)
                self.tr.barrier()
                cv.release()
                cv.mark()
                self.phase_sb(l, b)
                self.tr.barrier()
                cv.release()
                cv.release()
                cv.mark()
                self.phase_tok(l, b)
                self.tr.barrier()
                cv.release()
                cv.release()
            cv.mark()
            self.phase_route(l)
            self.tr.barrier()
            cv.release()
            cv.mark()
            self.phase_moe_blocks(l)
            self.tr.barrier()
            cv.release()
            cv.mark()
            self.phase_combine(l)
            self.tr.barrier()
            cv.release()
            cv.release()

    def emit_xT(self, src, i, xb, xTt, bank):
        d, db = self.d, self.db
        self.cp("pool", xb.ap, src.ap, [src.buf], [xb.buf])
        pt = self.bank_bf(bank)
        for k in range(8):
            self.tp(pt[:, k, :], xb.ap[:, k * 128:(k + 1) * 128], self.ident, [xb.buf, self.cb.buf], [self.banks[bank].buf])
        self.cp("act", xTt.ap, pt, [self.banks[bank].buf], [xTt.buf])
        self.dma("sp", d["xT"][i * 128:(i + 1) * 128, :].rearrange("p (k t) -> p k t", t=128), xTt.ap, [xTt.buf], [], [db["xT"]])

    def layernorm(self, u, g_bc, b_bc, out, st6, mv, rstd):
        for c in range(2):
            self.tr.op("dve", lambda E, c=c: E.bn_stats(out=st6.ap[:, c, :], in_=u.ap[:, c * 512:(c + 1) * 512]),
                       [u.buf], [st6.buf])
        self.tr.op("dve", lambda E: E.bn_aggr(out=mv.ap, in_=st6.ap), [st6.buf], [mv.buf])
        self.ts("dve", rstd.ap, mv.ap[:, 1:2], EPS, None, ALU.add, None, [mv.buf], [rstd.buf])
        self.act(rstd.ap, rstd.ap, AF.Sqrt, [rstd.buf], [rstd.buf])
        self.tr.op("dve", lambda E: E.reciprocal(out=rstd.ap, in_=rstd.ap), [rstd.buf], [rstd.buf])
        self.ts("dve", out.ap, u.ap, mv.ap[:, 0:1], rstd.ap[:, 0:1], ALU.subtract, ALU.mult,
                [u.buf, mv.buf, rstd.buf], [out.buf])
        self.tt("pool", out.ap, out.ap, g_bc.ap, ALU.mult, [out.buf, g_bc.buf], [out.buf])
        self.tt("dve", out.ap, out.ap, b_bc.ap, ALU.add, [out.buf, b_bc.buf], [out.buf])

    def phase_xT0(self):
        cv, d, db = self.cv, self.d, self.db
        xin = Rot([cv.alloc([D], F32) for _ in range(2)])
        xb = Rot([cv.alloc([D], BF16) for _ in range(2)])
        xTt = Rot([cv.alloc([8, 128], BF16) for _ in range(2)])
        bk = Rot([6, 7])
        for i in range(self.NT):
            xi = xin.next()
            self.dma("sp", xi.ap, d["x"][i * 128:(i + 1) * 128, :], [db["x"]], [xi.buf])
            self.emit_xT(xi, i, xb.next(), xTt.next(), bk.next())

    def precast_experts(self, l):
        d, db = self.d, self.db
        for e in range(NE):
            self.dma("pool", d["wg_d"][e * 128:(e + 1) * 128, :].rearrange("p (k n) -> p k n", n=DE),
                     d["w_gate"][l, e].rearrange("(k p) n -> p k n", p=128), [db["w_gate"]], [], [db["wg_d"]])
            self.dma("pool", d["wu_d"][e * 128:(e + 1) * 128, :].rearrange("p (k n) -> p k n", n=DE),
                     d["w_up"][l, e].rearrange("(k p) n -> p k n", p=128), [db["w_up"]], [], [db["wu_d"]])
            self.dma("pool", d["wd_d"][e * 128:(e + 1) * 128, :].rearrange("p (k n) -> p k n", n=D),
                     d["w_down"][l, e].rearrange("(k p) n -> p k n", p=128), [db["w_down"]], [], [db["wd_d"]])

    def alloc_seq(self):
        cv, S, NQB = self.cv, self.S, self.NQB
        self.ya = cv.alloc([NQB, 512], BF16)
        self.ybs = cv.alloc([NQB, 512], BF16)
        cv.mark()
        self.qTa = cv.alloc([4, S], BF16)
        self.kTa = cv.alloc([S], BF16)
        self.va = cv.alloc([NQB, 128], BF16)
        self.qTs = cv.alloc([4, S], BF16)
        self.kTs = cv.alloc([4, S], BF16)
        self.vs = cv.alloc([NQB, 512], BF16)

    def phase_inproj(self, l, b):
        cv, d, db, S = self.cv, self.d, self.db, self.S
        wq = cv.alloc([8, 2304], BF16)
        for k in range(8):
            self.dma("pool", wq.ap[:, k, :], d["w_in"][l, k * 128:(k + 1) * 128, 0:2304], [db["w_in"]], [wq.buf])
        bfm = cv.alloc([34], F32)
        self.dma("sp", bfm.ap, d["b_in_fm"][l], [db["b_in_fm"]], [bfm.buf])
        bfs = cv.alloc([34], F32)
        self.ts("dve", bfs.ap, bfm.ap, 0.125, None, ALU.mult, None, [bfm.buf], [bfs.buf])
        bva = cv.alloc([128], F32)
        bvs = cv.alloc([512], F32)
        self.dma("sp", bva.ap, d["b_in"][l, 640:768].partition_broadcast(128), [db["b_in"]], [bva.buf])
        self.dma("sp", bvs.ap, d["b_in"][l, 1792:2304].partition_broadcast(128), [db["b_in"]], [bvs.buf])
        xc = Rot([cv.alloc([4, 8, 128], BF16) for _ in range(2)])
        bk = Rot([0, 1, 2, 3, 4, 5])
        fm = []
        for c in range(4):
            fm.append((c * 128, self.qTa, c, 0.125))
        fm.append((512, self.kTa, None, 1.0))
        for c in range(4):
            fm.append((768 + c * 128, self.qTs, c, 0.125))
        for c in range(4):
            fm.append((1280 + c * 128, self.kTs, c, 1.0))
        t0 = b * S // 128
        ev = 0
        for tg in range(S // 512):
            x4 = xc.next()
            r0 = (t0 + tg * 4) * 128
            self.dma("sp", x4.ap, d["xT"][r0:r0 + 512, :].rearrange("(j p) (k t) -> p j k t", p=128, t=128),
                     [db["xT"]], [x4.buf])
            for (col, dst, chunk, scale) in fm:
                bi = bk.next()
                bank = self.banks[bi]
                for k in range(8):
                    self.mm(bank.ap.rearrange("p (j t) -> p j t", t=128), wq.ap[:, k, col:col + 128], x4.ap[:, :, k, :],
                            k == 0, k == 7, [wq.buf, x4.buf], [bank.buf])
                if chunk is None:
                    dap = dst.ap[:, tg * 512:(tg + 1) * 512]
                else:
                    dap = dst.ap[:, chunk, tg * 512:(tg + 1) * 512]
                bias = bfm.ap[:, col // 128:col // 128 + 1]
                if scale != 1.0:
                    self.act(dap, bank.ap, AF.Identity, [bank.buf, bfs.buf], [dst.buf],
                             bias=bfs.ap[:, col // 128:col // 128 + 1], scale=scale)
                else:
                    self.ts("dve", dap, bank.ap, bias, None, ALU.add, None, [bank.buf, bfm.buf], [dst.buf])
            for j in range(4):
                kb = tg * 4 + j
                bi = bk.next()
                bank = self.banks[bi]
                for k in range(8):
                    self.mm(bank.ap[:, 0:128], x4.ap[:, j, k, :], wq.ap[:, k, 640:768], k == 0, k == 7, [wq.buf, x4.buf], [bank.buf])
                self.tt("dve", self.va.ap[:, kb, :], bank.ap[:, 0:128], bva.ap, ALU.add, [bank.buf, bva.buf], [self.va.buf])
                bi = bk.next()
                bank = self.banks[bi]
                for k in range(8):
                    self.mm(bank.ap, x4.ap[:, j, k, :], wq.ap[:, k, 1792:2304], k == 0, k == 7, [wq.buf, x4.buf], [bank.buf])
                self.tt("dve", self.vs.ap[:, kb, :], bank.ap, bvs.ap, ALU.add, [bank.buf, bvs.buf], [self.vs.buf])

    def phase_swa(self, l, b):
        cv, d, db, S, NQB = self.cv, self.d, self.db, self.S, self.NQB
        sink = cv.alloc([8], F32)
        self.dma("sp", sink.ap, d["sinks"][l, :].partition_broadcast(128), [db["sinks"]], [sink.buf])
        sets = []
        for base in (0, 4):
            sets.append(dict(
                ps=[self.banks[base], self.banks[base + 1]], pt=base + 2, po=self.banks[base + 3],
                s=cv.alloc([4, 256], F32), p=cv.alloc([4, 256], BF16), pT=cv.alloc([4, 2, 128], BF16),
                m=cv.alloc([4], F32), negm=cv.alloc([4], F32), rs=cv.alloc([4], F32), es=cv.alloc([4], F32),
                rden=cv.alloc([4], F32)))
        it = 0
        for qb in range(NQB):
            nkb = 1 if qb == 0 else 2
            nk = nkb * 128
            k0 = qb * 128 if qb == 0 else (qb - 1) * 128
            koff = 128 if qb == 0 else 0
            for g in range(2):
                W = sets[it % 2]
                it += 1
                for hh in range(4):
                    bank = W["ps"][hh // 2]
                    self.mm(bank.ap[:, (hh % 2) * 256:(hh % 2) * 256 + nk],
                            self.qTa.ap[g * 64:(g + 1) * 64, hh, qb * 128:(qb + 1) * 128],
                            self.kTa.ap[g * 64:(g + 1) * 64, k0:k0 + nk], True, True,
                            [self.qTa.buf, self.kTa.buf], [bank.buf])
                s, p, pT = W["s"], W["p"], W["pT"]
                for half in range(2):
                    bank = W["ps"][half]
                    self.tt("dve", s.ap[:, half * 2:half * 2 + 2, 0:nk],
                            bank.ap.rearrange("p (h k) -> p h k", k=256)[:, :, 0:nk],
                            self.swab[:, g * 4 + half * 2:g * 4 + half * 2 + 2, koff:koff + nk], ALU.add,
                            [bank.buf, self.cf.buf], [s.buf])
                m, negm, rs, es, rden = W["m"], W["negm"], W["rs"], W["es"], W["rden"]
                self.red("dve", m.ap, s.ap[:, :, 0:nk], ALU.max, [s.buf], [m.buf])
                self.tt("dve", m.ap, m.ap, sink.ap[:, g * 4:(g + 1) * 4], ALU.max, [m.buf, sink.buf], [m.buf])
                self.ts("dve", negm.ap, m.ap, -1.0, None, ALU.mult, None, [m.buf], [negm.buf])
                self.memset("pool", rs.ap, 0.0, [rs.buf])
                for hh in range(4):
                    self.act(p.ap[:, hh, 0:nk], s.ap[:, hh, 0:nk], AF.Exp, [s.buf, negm.buf, rs.buf], [p.buf, rs.buf],
                             bias=negm.ap[:, hh:hh + 1], accum=rs.ap[:, hh:hh + 1])
                self.tt("dve", es.ap, sink.ap[:, g * 4:(g + 1) * 4], negm.ap, ALU.add, [sink.buf, negm.buf], [es.buf])
                self.act(es.ap, es.ap, AF.Exp, [es.buf], [es.buf])
                self.tt("dve", rden.ap, rs.ap, es.ap, ALU.add, [rs.buf, es.buf], [rden.buf])
                self.tr.op("dve", lambda E, rden=rden: E.reciprocal(out=rden.ap, in_=rden.ap), [rden.buf], [rden.buf])
                ptv = self.bank_bf(W["pt"]).rearrange("p (h k) t -> p h k t", k=2)
                ptb = self.banks[W["pt"]].buf
                for hh in range(4):
                    for kk in range(nkb):
                        self.tp(ptv[:, hh, kk, :], p.ap[:, hh, kk * 128:(kk + 1) * 128], self.ident, [p.buf, self.cb.buf], [ptb])
                self.cp("act", pT.ap[:, :, 0:nkb, :], ptv[:, :, 0:nkb, :], [ptb], [pT.buf])
                po = W["po"]
                for hh in range(4):
                    for kk in range(nkb):
                        kb = qb if qb == 0 else qb - 1 + kk
                        self.mm(po.ap[:, hh * 64:(hh + 1) * 64], pT.ap[:, hh, kk, :], self.va.ap[:, kb, g * 64:(g + 1) * 64],
                                kk == 0, kk == nkb - 1, [pT.buf, self.va.buf], [po.buf])
                for hh in range(4):
                    h = g * 4 + hh
                    self.ts("dve", self.ya.ap[:, qb, h * 64:(h + 1) * 64], po.ap[:, hh * 64:(hh + 1) * 64],
                            rden.ap[:, hh:hh + 1], None, ALU.mult, None, [po.buf, rden.buf], [self.ya.buf])

    def phase_sb(self, l, b):
        cv, S, NG = self.cv, self.S, self.NG
        zb = Rot([0, 1, 2, 3])
        ob = Rot([4, 5])
        e_r = Rot([cv.alloc([512], F32) for _ in range(3)])
        sp_r = Rot([cv.alloc([512], BF16) for _ in range(3)])
        a_r = Rot([cv.alloc([512], BF16) for _ in range(3)])
        Sbufs = [cv.alloc([512], BF16) for _ in range(2)]
        cbb = self.cb.buf
        for h in range(8):
            c, pb = h // 2, (h % 2) * 64
            for G in range(NG):
                q0 = G * 512
                self.memset("pool", Sbufs[0].ap, 0.0, [Sbufs[0].buf])
                self.memset("pool", Sbufs[1].ap, 0.0, [Sbufs[1].buf])
                pos = [self.banks[4 + j] for j in range(4)]
                step = 0
                for kb in range(4 * G + 3, -1, -1):
                    qlo = max(kb * 128, q0)
                    off = qlo - q0
                    wdt = 512 - off
                    zbank = self.banks[zb.next()]
                    z = zbank.ap[:, off:512]
                    diag = kb >= 4 * G
                    self.mm(z, self.kTs.ap[pb:pb + 64, c, kb * 128:(kb + 1) * 128], self.qTs.ap[pb:pb + 64, c, qlo:q0 + 512],
                            True, False, [self.kTs.buf, self.qTs.buf], [zbank.buf])
                    if diag:
                        self.mm(zbank.ap[:, off:off + 128], self.ident, self.negmask, False, False, [cbb], [zbank.buf])
                    e, sp, a = e_r.next(), sp_r.next(), a_r.next()
                    self.act(e.ap[:, off:512], z, AF.Exp, [zbank.buf], [e.buf])
                    self.act(sp.ap[:, off:512], e.ap[:, off:512], AF.Ln, [e.buf], [sp.buf], bias=1.0)
                    Scur, Snxt = Sbufs[step % 2], Sbufs[(step + 1) % 2]
                    self.mm(z, self.wsuf, sp.ap[:, off:512], False, False, [cbb, sp.buf], [zbank.buf])
                    self.mm(z, self.negones, Scur.ap[:, off:512], False, True, [cbb, Scur.buf], [zbank.buf])
                    if kb > 0:
                        self.tt("pool", Snxt.ap[:, off:512], Scur.ap[:, off:512], sp.ap[:, off:512], ALU.add,
                                [Scur.buf, sp.buf], [Snxt.buf])
                    self.act(a.ap[:, off:512], z, AF.Exp, [zbank.buf], [a.buf])
                    for j in range(off // 128, 4):
                        self.mm(pos[j].ap[:, 0:64], a.ap[:, j * 128:(j + 1) * 128], self.vs.ap[:, kb, h * 64:(h + 1) * 64],
                                kb == 4 * G + j, kb == 0, [a.buf, self.vs.buf], [pos[j].buf])
                    step += 1
                for j in range(4):
                    self.cp("dve", self.ybs.ap[:, 4 * G + j, h * 64:(h + 1) * 64], pos[j].ap[:, 0:64], [pos[j].buf], [self.ybs.buf])

    def phase_tok(self, l, b):
        cv, d, db, S, NQB = self.cv, self.d, self.db, self.S, self.NQB
        wg = cv.alloc([8, 2048], BF16)
        for k in range(8):
            self.dma("pool", wg.ap[:, k, :], d["w_in"][l, k * 128:(k + 1) * 128, 2304:4352], [db["w_in"]], [wg.buf])
        wab = cv.alloc([8, D], BF16)
        self.dma("pool", wab.ap[:, 0:4, :], d["w_a"][l].rearrange("(k p) n -> p k n", p=128), [db["w_a"]], [wab.buf])
        self.dma("pool", wab.ap[:, 4:8, :], d["w_b"][l].rearrange("(k p) n -> p k n", p=128), [db["w_b"]], [wab.buf])
        wo = cv.alloc([8, D], BF16)
        self.dma("pool", wo.ap, d["w_out"][l].rearrange("(k p) n -> p k n", p=128), [db["w_out"]], [wo.buf])
        bg = cv.alloc([2048], F32)
        self.dma("sp", bg.ap, d["b_in"][l, 2304:4352].partition_broadcast(128), [db["b_in"]], [bg.buf])
        lg = cv.alloc([D], F32)
        lb = cv.alloc([D], F32)
        self.dma("sp", lg.ap, d["ln1_g"][l, :].partition_broadcast(128), [db["ln1_g"]], [lg.buf])
        self.dma("sp", lb.ap, d["ln1_b"][l, :].partition_broadcast(128), [db["ln1_b"]], [lb.buf])
        xTt = Rot([cv.alloc([8, 128], BF16) for _ in range(2)])
        xr = Rot([cv.alloc([D], F32) for _ in range(2)])
        gt = Rot([cv.alloc([2048], F32) for _ in range(1)])
        yT = Rot([cv.alloc([8, 128], BF16) for _ in range(2)])
        t1 = Rot([cv.alloc([512], F32) for _ in range(2)])
        t2 = Rot([cv.alloc([512], F32) for _ in range(2)])
        mg = Rot([cv.alloc([D], BF16) for _ in range(1)])
        mT = Rot([cv.alloc([8, 128], BF16) for _ in range(2)])
        u = Rot([cv.alloc([D], F32) for _ in range(1)])
        x1 = Rot([cv.alloc([D], F32) for _ in range(1)])
        x1b = Rot([cv.alloc([D], BF16) for _ in range(2)])
        x1T = Rot([cv.alloc([8, 128], F32) for _ in range(1)])
        st6 = Rot([cv.alloc([2, 6], F32) for _ in range(2)])
        mv = Rot([cv.alloc([2], F32) for _ in range(2)])
        rstd = Rot([cv.alloc([1], F32) for _ in range(2)])
        bk = Rot([0, 1, 2, 3])
        tb = Rot([4, 5])
        t0 = b * NQB
        src = d["x"] if l == 0 else d["xres"]
        sb_ = db["x"] if l == 0 else db["xres"]
        loaded = {}

        def load(t):
            i = t0 + t
            xt, xrt = xTt.next(), xr.next()
            self.dma("sp", xt.ap, d["xT"][i * 128:(i + 1) * 128, :].rearrange("p (k t) -> p k t", t=128), [db["xT"]], [xt.buf])
            self.dma("sp", xrt.ap, src[i * 128:(i + 1) * 128, :], [sb_], [xrt.buf])
            loaded[t] = (xt, xrt)

        load(0)
        for t in range(NQB):
            i = t0 + t
            if t + 1 < NQB:
                load(t + 1)
            xt, xrt = loaded.pop(t)
            g = gt.next()
            for nh in range(4):
                bank = self.banks[bk.next()]
                for k in range(8):
                    self.mm(bank.ap, xt.ap[:, k, :], wg.ap[:, k, nh * 512:(nh + 1) * 512], k == 0, k == 7, [xt.buf, wg.buf], [bank.buf])
                self.tt("dve", g.ap[:, nh * 512:(nh + 1) * 512], bank.ap, bg.ap[:, nh * 512:(nh + 1) * 512], ALU.add,
                        [bank.buf, bg.buf], [g.buf])
            self.act(g.ap, g.ap, AF.Sigmoid, [g.buf], [g.buf])
            tbi = tb.next()
            pt = self.bank_bf(tbi)
            ptb = self.banks[tbi].buf
            for k in range(4):
                self.tp(pt[:, k, :], self.ya.ap[:, t, k * 128:(k + 1) * 128], self.ident, [self.ya.buf, self.cb.buf], [ptb])
            for k in range(4):
                self.tp(pt[:, 4 + k, :], self.ybs.ap[:, t, k * 128:(k + 1) * 128], self.ident, [self.ybs.buf, self.cb.buf], [ptb])
            yTt = yT.next()
            self.cp("act", yTt.ap, pt, [ptb], [yTt.buf])
            mgt = mg.next()
            for nh in range(2):
                ba = self.banks[bk.next()]
                for k in range(4):
                    self.mm(ba.ap, yTt.ap[:, k, :], wab.ap[:, k, nh * 512:(nh + 1) * 512], k == 0, k == 3, [yTt.buf, wab.buf], [ba.buf])
                bb = self.banks[bk.next()]
                for k in range(4):
                    self.mm(bb.ap, yTt.ap[:, 4 + k, :], wab.ap[:, 4 + k, nh * 512:(nh + 1) * 512], k == 0, k == 3,
                            [yTt.buf, wab.buf], [bb.buf])
                a1, a2 = t1.next(), t2.next()
                self.tt("dve", a1.ap, ba.ap, g.ap[:, nh * 512:(nh + 1) * 512], ALU.mult, [ba.buf, g.buf], [a1.buf])
                self.tt("dve", a2.ap, bb.ap, g.ap[:, 1024 + nh * 512:1024 + (nh + 1) * 512], ALU.mult, [bb.buf, g.buf], [a2.buf])
                self.tt("pool", mgt.ap[:, nh * 512:(nh + 1) * 512], a1.ap, a2.ap, ALU.add, [a1.buf, a2.buf], [mgt.buf])
            tbi = tb.next()
            pt = self.bank_bf(tbi)
            ptb = self.banks[tbi].buf
            for k in range(8):
                self.tp(pt[:, k, :], mgt.ap[:, k * 128:(k + 1) * 128], self.ident, [mgt.buf, self.cb.buf], [ptb])
            mTt = mT.next()
            self.cp("act", mTt.ap, pt, [ptb], [mTt.buf])
            ut = u.next()
            for nh in range(2):
                bo = self.banks[bk.next()]
                for k in range(8):
                    self.mm(bo.ap, mTt.ap[:, k, :], wo.ap[:, k, nh * 512:(nh + 1) * 512], k == 0, k == 7, [mTt.buf, wo.buf], [bo.buf])
                self.stt("dve", ut.ap[:, nh * 512:(nh + 1) * 512], xrt.ap[:, nh * 512:(nh + 1) * 512], ALPHA, bo.ap,
                         ALU.mult, ALU.add, [xrt.buf, bo.buf], [ut.buf])
            x1t = x1.next()
            self.layernorm(ut, lg, lb, x1t, st6.next(), mv.next(), rstd.next())
            self.dma("sp", d["x1"][i * 128:(i + 1) * 128, :], x1t.ap, [x1t.buf], [], [db["x1"]])
            x1bt = x1b.next()
            self.cp("pool", x1bt.ap, x1t.ap, [x1t.buf], [x1bt.buf])
            self.dma("sp", d["x1b"][i * 128:(i + 1) * 128, :], x1bt.ap, [x1bt.buf], [], [db["x1b"]])
            if self.debug:
                self.dma("sp", d["dbg_y"][i * 128:(i + 1) * 128, 0:512], self.ya.ap[:, t, :], [self.ya.buf], [], [db["dbg_y"]])
                self.dma("sp", d["dbg_y"][i * 128:(i + 1) * 128, 512:1024], self.ybs.ap[:, t, :], [self.ybs.buf], [], [db["dbg_y"]])
            x1Tt = x1T.next()
            for hf in range(2):
                fb = self.banks[6 + hf]
                for k in range(4):
                    kk = hf * 4 + k
                    self.tp(fb.ap[:, k * 128:(k + 1) * 128], x1t.ap[:, kk * 128:(kk + 1) * 128], self.identf,
                            [x1t.buf, self.cf.buf], [fb.buf])
                self.cp("act", x1Tt.ap[:, hf * 4:hf * 4 + 4, :], fb.ap.rearrange("p (k t) -> p k t", t=128), [fb.buf], [x1Tt.buf])
            rbk = self.banks[bk.next()]
            for k in range(8):
                self.mm(rbk.ap[:, 0:NE], x1Tt.ap[:, k, :], self.wr.ap[:, k, :], k == 0, k == 7, [x1Tt.buf, self.wr.buf], [rbk.buf])
            self.act(self.aff.ap[:, i, :], rbk.ap[:, 0:NE], AF.Sigmoid, [rbk.buf], [self.aff.buf])

    def phase_route(self, l):
        cv, d, db, NT, NBLK = self.cv, self.d, self.db, self.NT, self.NBLK
        N = NT * NE
        A = lambda shape, dt=F32: cv.alloc(shape, dt)
        self.dlo_i = A([NT], I32)
        self.dhi_i = A([NT], I32)
        self.glo = A([NT])
        self.ghi = A([NT])
        self.widx = A([NBLK], I32)
        cv.mark()
        bsd = A([NT, NE])
        self.tt("dve", bsd.ap, self.aff.ap, self.rb.ap.unsqueeze(1).to_broadcast([128, NT, NE]), ALU.add,
                [self.aff.buf, self.rb.buf], [bsd.buf])
        b4 = bsd.ap.rearrange("p t (g f) -> p (t g) f", f=4)
        NGp = NT * 8
        top2 = A([NGp])
        thr = A([NGp])
        tmp = A([NGp])
        first = True
        for i in range(4):
            for j in range(i + 1, 4):
                if first:
                    self.tt("dve", top2.ap, b4[:, :, i], b4[:, :, j], ALU.add, [bsd.buf], [top2.buf])
                    self.tt("dve", thr.ap, b4[:, :, i], b4[:, :, j], ALU.min, [bsd.buf], [thr.buf])
                    first = False
                else:
                    self.tt("dve", tmp.ap, b4[:, :, i], b4[:, :, j], ALU.add, [bsd.buf], [tmp.buf])
                    self.tt("dve", top2.ap, top2.ap, tmp.ap, ALU.max, [top2.buf, tmp.buf], [top2.buf])
                    self.tt("dve", tmp.ap, b4[:, :, i], b4[:, :, j], ALU.min, [bsd.buf], [tmp.buf])
                    self.tt("dve", thr.ap, thr.ap, tmp.ap, ALU.max, [thr.buf, tmp.buf], [thr.buf])
        gmax = A([NT])
        t2v = top2.ap.rearrange("p (t g) -> p t g", g=8)
        self.red("dve", gmax.ap, t2v, ALU.max, [top2.buf], [gmax.buf])
        gsel = A([NT, 8])
        self.tt("dve", gsel.ap, t2v, gmax.ap.unsqueeze(2).to_broadcast([128, NT, 8]), ALU.is_ge, [top2.buf, gmax.buf], [gsel.buf])
        sel = A([NT, NE])
        s4 = sel.ap.rearrange("p t (g f) -> p (t g) f", f=4)
        self.tt("dve", s4, b4, thr.ap.unsqueeze(2).to_broadcast([128, NGp, 4]), ALU.is_ge, [bsd.buf, thr.buf], [sel.buf])
        self.tt("dve", s4, s4, gsel.ap.rearrange("p t g -> p (t g)").unsqueeze(2).to_broadcast([128, NGp, 4]), ALU.mult,
                [sel.buf, gsel.buf], [sel.buf])
        gd = A([NT, NE])
        self.tt("dve", gd.ap, sel.ap, self.aff.ap, ALU.mult, [sel.buf, self.aff.buf], [gd.buf])
        wsum = A([NT])
        self.red("dve", wsum.ap, gd.ap, ALU.add, [gd.buf], [wsum.buf])
        self.tr.op("dve", lambda E: E.reciprocal(out=wsum.ap, in_=wsum.ap), [wsum.buf], [wsum.buf])
        self.tt("dve", gd.ap, gd.ap, wsum.ap.unsqueeze(2).to_broadcast([128, NT, NE]), ALU.mult, [gd.buf, wsum.buf], [gd.buf])
        selb = A([N], BF16)
        self.cp("dve", selb.ap, sel.ap.rearrange("p t e -> p (t e)"), [sel.buf], [selb.buf])
        cnt = A([NT, NE])
        rank = A([NT, NE])
        cntf = cnt.ap.rearrange("p t e -> p (t e)")
        rankf = rank.ap.rearrange("p t e -> p (t e)")
        for c0 in range(0, N, 512):
            w = min(512, N - c0)
            b0, b1 = self.banks[0], self.banks[1]
            self.mm(b0.ap[:, 0:w], self.ones, selb.ap[:, c0:c0 + w], True, True, [self.cb.buf, selb.buf], [b0.buf])
            self.mm(b1.ap[:, 0:w], self.ustrict, selb.ap[:, c0:c0 + w], True, True, [self.cb.buf, selb.buf], [b1.buf])
            self.cp("dve", cntf[:, c0:c0 + w], b0.ap[:, 0:w], [b0.buf], [cnt.buf])
            self.cp("dve", rankf[:, c0:c0 + w], b1.ap[:, 0:w], [b1.buf], [rank.buf])
        cum = A([NT + 1, NE])
        self.memset("dve", cum.ap[:, 0, :], 0.0, [cum.buf])
        for i in range(NT):
            self.tt("dve", cum.ap[:, i + 1, :], cum.ap[:, i, :], cnt.ap[:, i, :], ALU.add, [cum.buf, cnt.buf], [cum.buf])
        pad = A([NE])
        cmp_ = A([NE, NT])
        self.tt("dve", cmp_.ap, cum.ap[:, NT, :].unsqueeze(2).to_broadcast([128, NE, NT]),
                self.bstart[:, 0:NT].unsqueeze(1).to_broadcast([128, NE, NT]), ALU.is_gt, [cum.buf, self.cf.buf], [cmp_.buf])
        self.red("dve", pad.ap, cmp_.ap, ALU.add, [cmp_.buf], [pad.buf])
        self.ts("dve", pad.ap, pad.ap, 128.0, None, ALU.mult, None, [pad.buf], [pad.buf])
        pend = A([NE + 1])
        self.memset("dve", pend.ap[:, 0:1], 0.0, [pend.buf])
        for e in range(NE):
            self.tt("dve", pend.ap[:, e + 1:e + 2], pend.ap[:, e:e + 1], pad.ap[:, e:e + 1], ALU.add, [pend.buf, pad.buf], [pend.buf])
        dest = A([NT, NE])
        self.tt("dve", dest.ap, cum.ap[:, 0:NT, :], rank.ap, ALU.add, [cum.buf, rank.buf], [dest.buf])
        self.tt("dve", dest.ap, dest.ap, pend.ap[:, 0:NE].unsqueeze(1).to_broadcast([128, NT, NE]), ALU.add,
                [dest.buf, pend.buf], [dest.buf])
        BIG = 1.0e6
        dm = A([NT, NE])
        msk = A([NT, NE])
        self.ts("dve", msk.ap, sel.ap, -1.0, None, ALU.add, None, [sel.buf], [msk.buf])
        self.ts("dve", msk.ap, msk.ap, -BIG, None, ALU.mult, None, [msk.buf], [msk.buf])
        self.tt("dve", dm.ap, dest.ap, msk.ap, ALU.add, [dest.buf, msk.buf], [dm.buf])
        dlo = A([NT])
        dhi = A([NT])
        self.red("dve", dlo.ap, dm.ap, ALU.min, [dm.buf], [dlo.buf])
        self.tt("dve", dm.ap, dest.ap, msk.ap, ALU.subtract, [dest.buf, msk.buf], [dm.buf])
        self.red("dve", dhi.ap, dm.ap, ALU.max, [dm.buf], [dhi.buf])
        glo, ghi = self.glo, self.ghi
        eq = A([NT, NE])
        self.tt("dve", eq.ap, dest.ap, dlo.ap.unsqueeze(2).to_broadcast([128, NT, NE]), ALU.is_equal, [dest.buf, dlo.buf], [eq.buf])
        self.tt("dve", eq.ap, eq.ap, gd.ap, ALU.mult, [eq.buf, gd.buf], [eq.buf])
        self.red("dve", glo.ap, eq.ap, ALU.add, [eq.buf], [glo.buf])
        self.tt("dve", eq.ap, dest.ap, dhi.ap.unsqueeze(2).to_broadcast([128, NT, NE]), ALU.is_equal, [dest.buf, dhi.buf], [eq.buf])
        self.tt("dve", eq.ap, eq.ap, gd.ap, ALU.mult, [eq.buf, gd.buf], [eq.buf])
        self.red("dve", ghi.ap, eq.ap, ALU.add, [eq.buf], [ghi.buf])
        self.cp("dve", self.dlo_i.ap, dlo.ap, [dlo.buf], [self.dlo_i.buf])
        self.cp("dve", self.dhi_i.ap, dhi.ap, [dhi.buf], [self.dhi_i.buf])
        be = A([NBLK])
        self.memset("dve", be.ap, 0.0, [be.buf])
        for e in range(NE):
            self.stt("dve", be.ap, self.bstart, pend.ap[:, e + 1:e + 2], be.ap, ALU.is_ge, ALU.add,
                     [self.cf.buf, pend.buf, be.buf], [be.buf])
        self.ts("dve", be.ap, be.ap, float(NE - 1), None, ALU.min, None, [be.buf], [be.buf])
        self.ts("dve", be.ap, be.ap, 128.0, None, ALU.mult, None, [be.buf], [be.buf])
        self.ts("dve", be.ap, be.ap, self.pidx, None, ALU.add, None, [be.buf, self.cf.buf], [be.buf])
        self.cp("dve", self.widx.ap, be.ap, [be.buf], [self.widx.buf])
        xl = Rot([cv.alloc([D], BF16) for _ in range(3)])
        for i in range(NT):
            xt = xl.next()
            self.dma("sp", xt.ap, d["x1b"][i * 128:(i + 1) * 128, :], [db["x1b"]], [xt.buf])
            self.scatter(d["xs"], self.dlo_i.ap[:, i:i + 1], xt.ap, [xt.buf, self.dlo_i.buf], [db["xs"]])
            self.scatter(d["xs"], self.dhi_i.ap[:, i:i + 1], xt.ap, [xt.buf, self.dhi_i.buf], [db["xs"]])

    def phase_moe_blocks(self, l):
        cv, d, db, NBLK = self.cv, self.d, self.db, self.NBLK
        wgs = Rot([cv.alloc([8, DE], BF16) for _ in range(2)])
        wus = Rot([cv.alloc([8, DE], BF16) for _ in range(2)])
        wds = Rot([cv.alloc([4, D], BF16) for _ in range(2)])
        xsb = Rot([cv.alloc([D], BF16) for _ in range(2)])
        xsT = Rot([cv.alloc([8, 128], BF16) for _ in range(2)])
        sg = Rot([cv.alloc([DE], F32) for _ in range(2)])
        hb = Rot([cv.alloc([DE], BF16) for _ in range(2)])
        hT = Rot([cv.alloc([4, 128], BF16) for _ in range(2)])
        yo = Rot([cv.alloc([D], F32) for _ in range(2)])
        bk = Rot([0, 1, 2, 3, 4, 5])
        tb = Rot([6, 7])
        for blk in range(NBLK):
            wgt, wut, wdt = wgs.next(), wus.next(), wds.next()
            ix = self.widx.ap[:, blk:blk + 1]
            self.gather(wgt.ap.rearrange("p k n -> p (k n)"), d["wg_d"], ix, [db["wg_d"], self.widx.buf], [wgt.buf])
            self.gather(wut.ap.rearrange("p k n -> p (k n)"), d["wu_d"], ix, [db["wu_d"], self.widx.buf], [wut.buf])
            self.gather(wdt.ap.rearrange("p k n -> p (k n)"), d["wd_d"], ix, [db["wd_d"], self.widx.buf], [wdt.buf])
            xt = xsb.next()
            self.dma("sp", xt.ap, d["xs"][blk * 128:(blk + 1) * 128, :], [db["xs"]], [xt.buf])
            tbi = tb.next()
            pt, ptb = self.bank_bf(tbi), self.banks[tbi].buf
            for k in range(8):
                self.tp(pt[:, k, :], xt.ap[:, k * 128:(k + 1) * 128], self.ident, [xt.buf, self.cb.buf], [ptb])
            xT = xsT.next()
            self.cp("act", xT.ap, pt, [ptb], [xT.buf])
            bg_, bu_ = self.banks[bk.next()], self.banks[bk.next()]
            for k in range(8):
                self.mm(bg_.ap, xT.ap[:, k, :], wgt.ap[:, k, :], k == 0, k == 7, [xT.buf, wgt.buf], [bg_.buf])
            for k in range(8):
                self.mm(bu_.ap, xT.ap[:, k, :], wut.ap[:, k, :], k == 0, k == 7, [xT.buf, wut.buf], [bu_.buf])
            sgt, ht = sg.next(), hb.next()
            self.act(sgt.ap, bg_.ap, AF.Silu, [bg_.buf], [sgt.buf])
            self.tt("dve", ht.ap, sgt.ap, bu_.ap, ALU.mult, [sgt.buf, bu_.buf], [ht.buf])
            tbi = tb.next()
            pt, ptb = self.bank_bf(tbi), self.banks[tbi].buf
            for k in range(4):
                self.tp(pt[:, k, :], ht.ap[:, k * 128:(k + 1) * 128], self.ident, [ht.buf, self.cb.buf], [ptb])
            hTt = hT.next()
            self.cp("act", hTt.ap, pt[:, 0:4, :], [ptb], [hTt.buf])
            yt = yo.next()
            for nh in range(2):
                by = self.banks[bk.next()]
                for k in range(4):
                    self.mm(by.ap, hTt.ap[:, k, :], wdt.ap[:, k, nh * 512:(nh + 1) * 512], k == 0, k == 3, [hTt.buf, wdt.buf], [by.buf])
                self.cp("dve", yt.ap[:, nh * 512:(nh + 1) * 512], by.ap, [by.buf], [yt.buf])
            self.dma("pool", d["yb"][blk * 128:(blk + 1) * 128, :], yt.ap, [yt.buf], [], [db["yb"]])

    def phase_combine(self, l):
        cv, d, db, NT = self.cv, self.d, self.db, self.NT
        last = l == self.L - 1
        lg = cv.alloc([D], F32)
        lb = cv.alloc([D], F32)
        self.dma("sp", lg.ap, d["ln2_g"][l, :].partition_broadcast(128), [db["ln2_g"]], [lg.buf])
        self.dma("sp", lb.ap, d["ln2_b"][l, :].partition_broadcast(128), [db["ln2_b"]], [lb.buf])
        y0 = Rot([cv.alloc([D], F32) for _ in range(2)])
        y1 = Rot([cv.alloc([D], F32) for _ in range(2)])
        x1 = Rot([cv.alloc([D], F32) for _ in range(2)])
        u = Rot([cv.alloc([D], F32) for _ in range(2)])
        xo = Rot([cv.alloc([D], F32) for _ in range(2)])
        xb = Rot([cv.alloc([D], BF16) for _ in range(2)])
        xTt = Rot([cv.alloc([8, 128], BF16) for _ in range(2)])
        st6 = Rot([cv.alloc([2, 6], F32) for _ in range(2)])
        mv = Rot([cv.alloc([2], F32) for _ in range(2)])
        rstd = Rot([cv.alloc([1], F32) for _ in range(2)])
        bk = Rot([6, 7])
        loaded = {}

        def load(i):
            a0, a1, xt = y0.next(), y1.next(), x1.next()
            self.gather(a0.ap, d["yb"], self.dlo_i.ap[:, i:i + 1], [db["yb"], self.dlo_i.buf], [a0.buf])
            self.gather(a1.ap, d["yb"], self.dhi_i.ap[:, i:i + 1], [db["yb"], self.dhi_i.buf], [a1.buf])
            self.dma("sp", xt.ap, d["x1"][i * 128:(i + 1) * 128, :], [db["x1"]], [xt.buf])
            loaded[i] = (a0, a1, xt)

        load(0)
        for i in range(NT):
            if i + 1 < NT:
                load(i + 1)
            a0, a1, xt = loaded.pop(i)
            ut = u.next()
            self.ts("dve", ut.ap, a0.ap, self.glo.ap[:, i:i + 1], None, ALU.mult, None, [a0.buf, self.glo.buf], [ut.buf])
            self.stt("dve", ut.ap, a1.ap, self.ghi.ap[:, i:i + 1], ut.ap, ALU.mult, ALU.add, [a1.buf, self.ghi.buf, ut.buf], [ut.buf])
            self.stt("dve", ut.ap, xt.ap, ALPHA, ut.ap, ALU.mult, ALU.add, [xt.buf, ut.buf], [ut.buf])
            xot = xo.next()
            self.layernorm(ut, lg, lb, xot, st6.next(), mv.next(), rstd.next())
            if last:
                self.dma("sp", d["y"][i * 128:(i + 1) * 128, :], xot.ap, [xot.buf], [], [db["y"]])
            else:
                self.dma("sp", d["xres"][i * 128:(i + 1) * 128, :], xot.ap, [xot.buf], [], [db["xres"]])
                self.emit_xT(xot, i, xb.next(), xTt.next(), bk.next())


def make_consts(NBLK):
    cb = np.zeros((128, 6, 128), np.float32)
    i = np.arange(128)
    cb[:, 0] = np.eye(128)
    cb[:, 1] = -1.0 * (i[:, None] >= i[None, :])
    cb[:, 2] = -1.0
    cb[:, 3] = np.where(i[:, None] >= i[None, :], NEG, 0.0)
    cb[:, 4] = (i[:, None] < i[None, :])
    cb[:, 5] = 1.0
    ncf = 128 + 8 * 256 + 1 + NBLK + 2
    cf = np.zeros((128, ncf), np.float32)
    cf[:, 0:128] = np.eye(128)
    slopes = np.exp2(-8.0 * (np.arange(8, dtype=np.float32) + 1.0) / 8).astype(np.float32)
    j = np.arange(256)
    dist = i[:, None] + 128 - j[None, :]
    valid = (dist >= 0) & (dist < 128)
    sb = np.where(valid[None], -slopes[:, None, None] * dist[None].astype(np.float32), NEG).astype(np.float32)
    cf[:, 128:128 + 2048] = sb.transpose(1, 0, 2).reshape(128, 2048)
    o = 128 + 2048
    cf[:, o] = i
    cf[:, o + 1:o + 1 + NBLK] = 128.0 * np.arange(NBLK)[None, :]
    return cb.reshape(128, 768).astype(NPBF), cf


_CACHE = {}


def get_prog(S, NB, L, debug=False):
    key = (S, NB, L, debug)
    if key not in _CACHE:
        p = Prog(S, NB, L, debug)
        nc = p.build()
        _CACHE[key] = (p, nc)
    return _CACHE[key]


def prep_shared(inp, L):
    f = lambda a: np.ascontiguousarray(np.asarray(a, dtype=np.float32))
    w_in = f(inp["w_in"])[:L]
    b_in = f(inp["b_in"])[:L]
    perm = np.arange(NPROJ)
    perm[:512] = np.concatenate([np.arange(h * 64, (h + 1) * 64) for h in QPERM])
    w_in = np.ascontiguousarray(w_in[:, :, perm])
    b_in = np.ascontiguousarray(b_in[:, perm])
    b_in_fm = np.ascontiguousarray(b_in.reshape(L, 34, 128).transpose(0, 2, 1))
    sh = dict(
        w_in=w_in, b_in=b_in, b_in_fm=b_in_fm, sinks=f(inp["attn_sinks"])[:L],
        w_a=f(inp["w_branch_a"])[:L], w_b=f(inp["w_branch_b"])[:L], w_out=f(inp["w_out"])[:L],
        ln1_g=f(inp["ln1_g"])[:L], ln1_b=f(inp["ln1_b"])[:L], ln2_g=f(inp["ln2_g"])[:L], ln2_b=f(inp["ln2_b"])[:L],
        w_router=f(inp["w_router"]), router_bias=f(inp["router_bias"]).reshape(1, NE),
        w_gate=f(inp["w_gate"])[:L], w_up=f(inp["w_up"])[:L], w_down=f(inp["w_down"])[:L],
    )
    return sh


def run(inp, S, NB, L, n_cores, debug=False):
    p, nc = get_prog(S, NB, L, debug)
    sh = prep_shared(inp, L)
    cb, cf = make_consts(p.NBLK)
    sh["cb"] = cb
    sh["cf"] = cf
    x = np.ascontiguousarray(np.asarray(inp["x"], dtype=np.float32)).reshape(n_cores, NB * S, D)
    in_maps = []
    for c in range(n_cores):
        m = dict(sh)
        m["x"] = x[c]
        in_maps.append(m)
    res = run_bass_kernel_spmd(nc, in_maps, core_ids=list(range(n_cores)))
    return res.results


def kernel(x, w_in, b_in, attn_sinks, w_branch_a, w_branch_b, w_out, ln1_g, ln1_b,
           w_router, router_bias, w_gate, w_up, w_down, ln2_g, ln2_b):
    inp = dict(x=x, w_in=w_in, b_in=b_in, attn_sinks=attn_sinks, w_branch_a=w_branch_a, w_branch_b=w_branch_b,
               w_out=w_out, ln1_g=ln1_g, ln1_b=ln1_b, w_router=w_router, router_bias=router_bias,
               w_gate=w_gate, w_up=w_up, w_down=w_down, ln2_g=ln2_g, ln2_b=ln2_b)
    B, S, _ = np.asarray(x).shape
    n_cores = 8
    NB = B // n_cores
    res = run(inp, S, NB, 4, n_cores)
    out = np.stack([r["y"] for r in res], 0).reshape(B, S, D)
    return out.astype(np.float32)
```

```python
from contextlib import ExitStack

import ml_dtypes
import numpy as np

import concourse.bass as bass
import concourse.mybir as mybir
from concourse.bass_utils import run_bass_kernel_spmd

F32 = mybir.dt.float32
BF16 = mybir.dt.bfloat16
I32 = mybir.dt.int32
U8 = mybir.dt.uint8
ALU = mybir.AluOpType
AF = mybir.ActivationFunctionType
AX = mybir.AxisListType
NPBF = ml_dtypes.bfloat16

D = 1024
NE = 32
DE = 512
NPROJ = 4352
ALPHA = float((2 * 4) ** 0.25)
EPS = 1e-5
NEG = -30000.0
QPERM = [0, 4, 1, 5, 2, 6, 3, 7]
SBUF_BYTES = 200 * 1024
BG_IN_SB = False


class Buf:
    __slots__ = ("w", "r")

    def __init__(self):
        self.w = {}
        self.r = {}


class TT:
    __slots__ = ("ap", "buf")

    def __init__(self, ap, buf=None):
        self.ap = ap
        self.buf = buf if buf is not None else Buf()


class Rot:
    def __init__(self, items):
        self.items = items
        self.i = 0

    def next(self):
        it = self.items[self.i % len(self.items)]
        self.i += 1
        return it


class Tracker:
    COMPUTE = ("pe", "act", "dve", "pool")
    ALL = ("pe", "act", "dve", "pool", "sp")

    def __init__(self, nc, stack, n_dma_sems=8):
        self.nc = nc
        self.stack = stack
        self.ops = {e: [] for e in self.ALL}
        self.sem = {}
        self.cnt = {}
        self.nsem = 0
        self.seen = {e: {} for e in self.ALL}
        for e in self.COMPUTE:
            self._new_sem(e)
        self.dpool = {}
        self.dnext = {}
        for q in ("sp", "act", "pool"):
            self.dpool[q] = [[self._alloc(f"d_{q}_{i}"), 0] for i in range(n_dma_sems)]
            self.dnext[q] = 0
        self.bsem = self._alloc("barrier")
        self.bcount = 0
        self.ninstr = {e: 0 for e in self.ALL}
        self.abs = {e: [] for e in self.ALL}

    def _alloc(self, name):
        self.nsem += 1
        return self.stack.enter_context(self.nc.semaphore(name))

    def _new_sem(self, e):
        self.sem[e] = self._alloc(f"c_{e}_{self.nsem}")
        self.cnt[e] = 0

    def _wait(self, eng, s, v):
        if self.seen[eng].get(s, 0) >= v:
            return
        self.seen[eng][s] = v
        self.ops[eng].append(lambda E, s=s, v=v: E.wait_ge(s, v))
        self.abs[eng].append(("w", id(s), v))
        self.ninstr[eng] += 1

    @staticmethod
    def _deps(reads, writes):
        deps = {}
        for b in reads:
            for s, v in b.w.items():
                if deps.get(s, 0) < v:
                    deps[s] = v
        for b in writes:
            for d in (b.w, b.r):
                for s, v in d.items():
                    if deps.get(s, 0) < v:
                        deps[s] = v
        return deps

    def op(self, eng, fn, reads=(), writes=()):
        deps = self._deps(reads, writes)
        own = self.sem[eng]
        for s, v in deps.items():
            if eng == "pe" and s is own:
                continue
            self._wait(eng, s, v)
        self.cnt[eng] += 1
        v = self.cnt[eng]
        self.ops[eng].append(lambda E, fn=fn, own=own: fn(E).then_inc(own, 1))
        self.abs[eng].append(("i", id(own), 1))
        self.ninstr[eng] += 1
        for b in reads:
            if b.r.get(own, 0) < v:
                b.r[own] = v
        for b in writes:
            b.w = {own: v}
            b.r = {}
        if v >= 60000:
            self._new_sem(eng)

    def dma(self, q, fn, reads=(), writes=(), swrites=()):
        deps = self._deps(reads, writes)
        for b in swrites:
            for s, v in b.r.items():
                if deps.get(s, 0) < v:
                    deps[s] = v
        for s, v in deps.items():
            self._wait(q, s, v)
        slot = self.dpool[q][self.dnext[q]]
        self.dnext[q] = (self.dnext[q] + 1) % len(self.dpool[q])
        s = slot[0]
        if slot[1] > 0:
            self._wait(q, s, slot[1])
        slot[1] += 16
        v = slot[1]
        assert v < 65000
        self.ops[q].append(lambda E, fn=fn, s=s: fn(E).then_inc(s, 16))
        self.abs[q].append(("i", id(s), 16))
        self.ninstr[q] += 1
        for b in reads:
            if b.r.get(s, 0) < v:
                b.r[s] = v
        for b in writes:
            b.w = {s: v}
            b.r = {}
        for b in swrites:
            b.w[s] = v

    def barrier(self):
        self.bcount += len(self.ALL)
        bs, bc = self.bsem, self.bcount
        assert bc < 65000
        for e in self.ALL:
            if e in self.COMPUTE and self.cnt[e] > 0:
                self._wait(e, self.sem[e], self.cnt[e])
            if e in self.dpool:
                for s, v in self.dpool[e]:
                    if v > 0:
                        self._wait(e, s, v)
            self.ops[e].append(lambda E: E.sem_inc(bs, 1))
            self.ops[e].append(lambda E: E.wait_ge(bs, bc))
            self.abs[e].append(("i", id(bs), 1))
            self.abs[e].append(("w", id(bs), bc))
        floor = {}
        for e in self.COMPUTE:
            floor[self.sem[e]] = self.cnt[e]
        for q in self.dpool:
            for s, v in self.dpool[q]:
                floor[s] = v
        for e in self.ALL:
            for s, v in floor.items():
                if self.seen[e].get(s, 0) < v:
                    self.seen[e][s] = v

    def finish(self, block):
        self.barrier()
        ops = self.ops

        @block.tensor
        def _(E):
            for f in ops["pe"]:
                f(E)

        @block.scalar
        def _(E):
            for f in ops["act"]:
                f(E)

        @block.vector
        def _(E):
            for f in ops["dve"]:
                f(E)

        @block.gpsimd
        def _(E):
            for f in ops["pool"]:
                f(E)

        @block.sync
        def _(E):
            for f in ops["sp"]:
                f(E)


class Carver:
    def __init__(self, big, nbytes):
        self.big = big
        self.nbytes = nbytes
        self.off = 0
        self.marks = []
        self.peak = 0

    def mark(self):
        self.marks.append(self.off)

    def release(self):
        self.off = self.marks.pop()

    def alloc(self, shape, dtype, buf=None):
        esz = {F32: 4, BF16: 2, I32: 4}[dtype]
        n = int(np.prod(shape))
        nb = n * esz
        self.off = (self.off + 63) // 64 * 64
        assert self.off + nb <= self.nbytes, f"SBUF carve overflow {self.off}+{nb}>{self.nbytes}"
        ap = self.big[:, self.off:self.off + nb].bitcast(dtype)
        self.off += nb
        self.peak = max(self.peak, self.off)
        if len(shape) == 2:
            ap = ap.rearrange("p (a b) -> p a b", b=shape[1])
        elif len(shape) == 3:
            ap = ap.rearrange("p (a b c) -> p a b c", b=shape[1], c=shape[2])
        return TT(ap, buf)


class Prog:
    def __init__(self, S, NB, L, debug=False):
        self.S, self.NB, self.L, self.debug = S, NB, L, debug
        self.T = S * NB
        self.NT = self.T // 128
        self.NQB = S // 128
        self.NG = S // 512
        self.NBLK = 2 * self.NT + NE
        self.NCF = 128 + 8 * 256 + 1 + self.NBLK + 2

    def mm(self, out, lhsT, rhs, start, stop, reads, writes):
        self.tr.op("pe", lambda E: E.matmul(out, lhsT=lhsT, rhs=rhs, start=start, stop=stop), reads, writes)

    def tp(self, out, in_, ident, reads, writes):
        self.tr.op("pe", lambda E: E.transpose(out=out, in_=in_, identity=ident), reads, writes)

    def act(self, out, in_, func, reads, writes, bias=0.0, scale=1.0, accum=None):
        if accum is None:
            self.tr.op("act", lambda E: E.activation(out=out, in_=in_, func=func, bias=bias, scale=scale), reads, writes)
        else:
            self.tr.op("act", lambda E: E.activation(out=out, in_=in_, func=func, bias=bias, scale=scale,
                                                     accum_out=accum), reads, writes)

    def tt(self, eng, out, in0, in1, op, reads, writes):
        self.tr.op(eng, lambda E: E.tensor_tensor(out=out, in0=in0, in1=in1, op=op), reads, writes)

    def ts(self, eng, out, in0, s1, s2, op0, op1, reads, writes):
        if s2 is None:
            self.tr.op(eng, lambda E: E.tensor_scalar(out=out, in0=in0, scalar1=s1, scalar2=None, op0=op0), reads, writes)
        else:
            self.tr.op(eng, lambda E: E.tensor_scalar(out=out, in0=in0, scalar1=s1, scalar2=s2, op0=op0, op1=op1),
                       reads, writes)

    def stt(self, eng, out, in0, scalar, in1, op0, op1, reads, writes):
        self.tr.op(eng, lambda E: E.scalar_tensor_tensor(out=out, in0=in0, scalar=scalar, in1=in1, op0=op0, op1=op1),
                   reads, writes)

    def cp(self, eng, out, in_, reads, writes):
        if eng == "act":
            self.tr.op("act", lambda E: E.activation(out=out, in_=in_, func=AF.Copy), reads, writes)
        else:
            self.tr.op(eng, lambda E: E.tensor_copy(out=out, in_=in_), reads, writes)

    def red(self, eng, out, in_, op, reads, writes):
        self.tr.op(eng, lambda E: E.tensor_reduce(out=out, in_=in_, axis=AX.X, op=op), reads, writes)

    def memset(self, eng, ap, val, writes):
        self.tr.op(eng, lambda E: E.memset(ap, val), (), writes)

    def dma(self, q, out, in_, reads, writes, swrites=()):
        self.tr.dma(q, lambda E: E.dma_start(out=out, in_=in_), reads, writes, swrites)

    def gather(self, out, src, idx, reads, writes):
        self.tr.dma("pool", lambda E: E.indirect_dma_start(
            out=out, out_offset=None, in_=src, in_offset=bass.IndirectOffsetOnAxis(ap=idx, axis=0)), reads, writes)

    def scatter(self, dst, idx, in_, reads, swrites):
        self.tr.dma("pool", lambda E: E.indirect_dma_start(
            out=dst, out_offset=bass.IndirectOffsetOnAxis(ap=idx, axis=0), in_=in_, in_offset=None), reads, (), swrites)

    def build(self):
        S, NB, L, T, NT, NBLK = self.S, self.NB, self.L, self.T, self.NT, self.NBLK
        nc = bass.Bass("TRN2", target_bir_lowering=False)
        self.nc = nc

        def din(name, shape, dt=F32):
            return nc.dram_tensor(name, list(shape), dt, kind="ExternalInput").ap()

        def dscr(name, shape, dt):
            return nc.dram_tensor(name, list(shape), dt, kind="Internal").ap()

        d = self.d = {}
        d["x"] = din("x", [T, D])
        d["w_in"] = din("w_in", [L, D, NPROJ])
        d["b_in"] = din("b_in", [L, NPROJ])
        d["b_in_fm"] = din("b_in_fm", [L, 128, 34])
        d["sinks"] = din("sinks", [L, 8])
        d["w_a"] = din("w_a", [L, 512, D])
        d["w_b"] = din("w_b", [L, 512, D])
        d["w_out"] = din("w_out", [L, D, D])
        for n in ("ln1_g", "ln1_b", "ln2_g", "ln2_b"):
            d[n] = din(n, [L, D])
        d["w_router"] = din("w_router", [D, NE])
        d["router_bias"] = din("router_bias", [1, NE])
        d["w_gate"] = din("w_gate", [L, NE, D, DE])
        d["w_up"] = din("w_up", [L, NE, D, DE])
        d["w_down"] = din("w_down", [L, NE, DE, D])
        d["cb"] = din("cb", [128, 6 * 128], BF16)
        d["cf"] = din("cf", [128, self.NCF])
        d["y"] = nc.dram_tensor("y", [T, D], F32, kind="ExternalOutput").ap()
        if self.debug:
            d["dbg_x1"] = nc.dram_tensor("dbg_x1", [T, D], F32, kind="ExternalOutput").ap()
            d["dbg_y"] = nc.dram_tensor("dbg_y", [T, D], BF16, kind="ExternalOutput").ap()
        d["xres"] = dscr("xres", [T, D], F32)
        d["x1"] = d["dbg_x1"] if self.debug else dscr("x1", [T, D], F32)
        d["x1b"] = dscr("x1b", [T, D], BF16)
        d["xT"] = dscr("xT", [NT * 128, D], BF16)
        d["xs"] = dscr("xs", [NBLK * 128, D], BF16)
        d["yb"] = dscr("yb", [NBLK * 128, D], F32)
        for sfx in ("0", "1"):
            d["wg_d" + sfx] = dscr("wg_d" + sfx, [NE * 128, 8 * DE], BF16)
            d["wu_d" + sfx] = dscr("wu_d" + sfx, [NE * 128, 8 * DE], BF16)
            d["wd_d" + sfx] = dscr("wd_d" + sfx, [NE * 128, 4 * D], BF16)
        self.db = {k: Buf() for k in d}

        with ExitStack() as st:
            big = st.enter_context(nc.sbuf_tensor("big", [128, SBUF_BYTES], U8))
            self.banks = []
            for i in range(8):
                t = st.enter_context(nc.psum_tensor(f"bank{i}", [128, 512], F32))
                self.banks.append(TT(t[:, :]))
            self.tr = Tracker(nc, st)
            self.cv = Carver(big, SBUF_BYTES)
            blk = st.enter_context(nc.Block())
            self.emit()
            self.tr.finish(blk)
            self.stats = dict(instr=dict(self.tr.ninstr), sems=self.tr.nsem, sbuf_peak=self.cv.peak)
        return nc

    def bank_bf(self, b):
        return self.banks[b].ap.bitcast(BF16).rearrange("p (a b) -> p a b", b=128)

    def emit(self):
        cv, d, db = self.cv, self.d, self.db
        NT, NBLK, L = self.NT, self.NBLK, self.L
        self.cb = cv.alloc([6, 128], BF16)
        self.cf = cv.alloc([self.NCF], F32)
        self.dma("sp", self.cb.ap, d["cb"].rearrange("p (a b) -> p a b", b=128), [], [self.cb.buf])
        self.dma("sp", self.cf.ap, d["cf"], [], [self.cf.buf])
        cbb = self.cb.buf
        self.ident = self.cb.ap[:, 0, :]
        self.wsuf = self.cb.ap[:, 1, :]
        self.negones = self.cb.ap[:, 2, :]
        self.negmask = self.cb.ap[:, 3, :]
        self.ustrict = self.cb.ap[:, 4, :]
        self.ones = self.cb.ap[:, 5, :]
        self.identf = self.cf.ap[:, 0:128]
        self.swab = self.cf.ap[:, 128:128 + 2048].rearrange("p (h k) -> p h k", k=256)
        o = 128 + 2048
        self.pidx = self.cf.ap[:, o:o + 1]
        self.bstart = self.cf.ap[:, o + 1:o + 1 + NBLK]
        self.aff = cv.alloc([NT, NE], F32)
        self.wr = cv.alloc([8, NE], F32)
        self.rb = cv.alloc([NE], F32)
        self.dma("sp", self.wr.ap, d["w_router"].rearrange("(k p) e -> p k e", p=128), [], [self.wr.buf])
        self.dma("sp", self.rb.ap, d["router_bias"][0, :].partition_broadcast(128), [], [self.rb.buf])

        cv.mark()
        self.phase_xT0()
        self.tr.barrier()
        cv.release()
        self.bg = []
        self.precast_experts(0)
        for l in range(L):
            if l + 1 < L:
                self.precast_experts(l + 1)
            self.bg_slots_left = self.NB * 8 * self.NG
            if not BG_IN_SB:
                self.bg_flush()
            for b in range(self.NB):
                cv.mark()
                self.alloc_seq()
                cv.mark()
                self.phase_inproj(l, b)
                self.tr.barrier()
                cv.release()
                cv.mark()
                self.phase_swa(l, b)
                self.tr.barrier()
                cv.release()
                cv.mark()
                self.phase_sb(l, b)
                self.tr.barrier()
                cv.release()
                cv.release()
                cv.mark()
                self.phase_tok(l, b)
                self.tr.barrier()
                cv.release()
                cv.release()
            self.bg_flush()
            cv.mark()
            self.phase_route(l)
            self.tr.barrier()
            cv.release()
            cv.mark()
            self.phase_moe_blocks(l)
            self.tr.barrier()
            cv.release()
            cv.mark()
            self.phase_combine(l)
            self.tr.barrier()
            cv.release()
            cv.release()

    def emit_xT(self, src, i, xb, xTt, bank):
        d, db = self.d, self.db
        self.cp("pool", xb.ap, src.ap, [src.buf], [xb.buf])
        pt = self.bank_bf(bank)
        for k in range(8):
            self.tp(pt[:, k, :], xb.ap[:, k * 128:(k + 1) * 128], self.ident, [xb.buf, self.cb.buf], [self.banks[bank].buf])
        self.cp("act", xTt.ap, pt, [self.banks[bank].buf], [xTt.buf])
        self.dma("sp", d["xT"][i * 128:(i + 1) * 128, :].rearrange("p (k t) -> p k t", t=128), xTt.ap, [xTt.buf], [], [db["xT"]])

    def layernorm(self, u, g_bc, b_bc, out, st6, mv, rstd):
        for c in range(2):
            self.tr.op("dve", lambda E, c=c: E.bn_stats(out=st6.ap[:, c, :], in_=u.ap[:, c * 512:(c + 1) * 512]),
                       [u.buf], [st6.buf])
        self.tr.op("dve", lambda E: E.bn_aggr(out=mv.ap, in_=st6.ap), [st6.buf], [mv.buf])
        self.ts("dve", rstd.ap, mv.ap[:, 1:2], EPS, None, ALU.add, None, [mv.buf], [rstd.buf])
        self.act(rstd.ap, rstd.ap, AF.Sqrt, [rstd.buf], [rstd.buf])
        self.tr.op("dve", lambda E: E.reciprocal(out=rstd.ap, in_=rstd.ap), [rstd.buf], [rstd.buf])
        self.ts("dve", out.ap, u.ap, mv.ap[:, 0:1], rstd.ap[:, 0:1], ALU.subtract, ALU.mult,
                [u.buf, mv.buf, rstd.buf], [out.buf])
        self.tt("pool", out.ap, out.ap, g_bc.ap, ALU.mult, [out.buf, g_bc.buf], [out.buf])
        self.tt("dve", out.ap, out.ap, b_bc.ap, ALU.add, [out.buf, b_bc.buf], [out.buf])

    def phase_xT0(self):
        cv, d, db = self.cv, self.d, self.db
        xin = Rot([cv.alloc([D], F32) for _ in range(2)])
        xb = Rot([cv.alloc([D], BF16) for _ in range(2)])
        xTt = Rot([cv.alloc([8, 128], BF16) for _ in range(2)])
        bk = Rot([6, 7])
        for i in range(self.NT):
            xi = xin.next()
            self.dma("sp", xi.ap, d["x"][i * 128:(i + 1) * 128, :], [db["x"]], [xi.buf])
            self.emit_xT(xi, i, xb.next(), xTt.next(), bk.next())

    def precast_experts(self, l):
        d, db = self.d, self.db
        sfx = str(l % 2)
        for e in range(NE):
            for (dst, src, n) in (("wg_d", "w_gate", DE), ("wu_d", "w_up", DE), ("wd_d", "w_down", D)):
                self.bg.append(lambda dst=dst, src=src, n=n, e=e: self.dma(
                    "pool", d[dst + sfx][e * 128:(e + 1) * 128, :].rearrange("p (k n) -> p k n", n=n),
                    d[src][l, e].rearrange("(k p) n -> p k n", p=128), [db[src]], [], [db[dst + sfx]]))

    def alloc_seq(self):
        cv, S, NQB = self.cv, self.S, self.NQB
        self.ya = cv.alloc([NQB, 512], BF16)
        self.ybs = cv.alloc([NQB, 512], BF16)
        cv.mark()
        self.qTa = cv.alloc([4, S], BF16)
        self.kTa = cv.alloc([S], BF16)
        self.va = cv.alloc([NQB, 128], BF16)
        self.qTs = cv.alloc([4, S], BF16)
        self.kTs = cv.alloc([4, S], BF16)
        self.vs = cv.alloc([NQB, 512], BF16)

    def phase_inproj(self, l, b):
        cv, d, db, S = self.cv, self.d, self.db, self.S
        wq = cv.alloc([8, 2304], BF16)
        for k in range(8):
            self.dma("pool", wq.ap[:, k, :], d["w_in"][l, k * 128:(k + 1) * 128, 0:2304], [db["w_in"]], [wq.buf])
        bfm = cv.alloc([34], F32)
        self.dma("sp", bfm.ap, d["b_in_fm"][l], [db["b_in_fm"]], [bfm.buf])
        bfs = cv.alloc([34], F32)
        self.ts("dve", bfs.ap, bfm.ap, 0.125, None, ALU.mult, None, [bfm.buf], [bfs.buf])
        bva = cv.alloc([128], F32)
        bvs = cv.alloc([512], F32)
        self.dma("sp", bva.ap, d["b_in"][l, 640:768].partition_broadcast(128), [db["b_in"]], [bva.buf])
        self.dma("sp", bvs.ap, d["b_in"][l, 1792:2304].partition_broadcast(128), [db["b_in"]], [bvs.buf])
        xc = Rot([cv.alloc([4, 8, 128], BF16) for _ in range(2)])
        bk = Rot([0, 1, 2, 3, 4, 5])
        fm = []
        for c in range(4):
            fm.append((c * 128, self.qTa, c, 0.125))
        fm.append((512, self.kTa, None, 1.0))
        for c in range(4):
            fm.append((768 + c * 128, self.qTs, c, 0.125))
        for c in range(4):
            fm.append((1280 + c * 128, self.kTs, c, 1.0))
        t0 = b * S // 128
        ev = 0
        for tg in range(S // 512):
            x4 = xc.next()
            r0 = (t0 + tg * 4) * 128
            self.dma("sp", x4.ap, d["xT"][r0:r0 + 512, :].rearrange("(j p) (k t) -> p j k t", p=128, t=128),
                     [db["xT"]], [x4.buf])
            for (col, dst, chunk, scale) in fm:
                bi = bk.next()
                bank = self.banks[bi]
                for k in range(8):
                    self.mm(bank.ap.rearrange("p (j t) -> p j t", t=128), wq.ap[:, k, col:col + 128], x4.ap[:, :, k, :],
                            k == 0, k == 7, [wq.buf, x4.buf], [bank.buf])
                if chunk is None:
                    dap = dst.ap[:, tg * 512:(tg + 1) * 512]
                else:
                    dap = dst.ap[:, chunk, tg * 512:(tg + 1) * 512]
                bias = bfm.ap[:, col // 128:col // 128 + 1]
                if scale != 1.0:
                    self.act(dap, bank.ap, AF.Identity, [bank.buf, bfs.buf], [dst.buf],
                             bias=bfs.ap[:, col // 128:col // 128 + 1], scale=scale)
                else:
                    self.ts("dve", dap, bank.ap, bias, None, ALU.add, None, [bank.buf, bfm.buf], [dst.buf])
            for j in range(4):
                kb = tg * 4 + j
                bi = bk.next()
                bank = self.banks[bi]
                for k in range(8):
                    self.mm(bank.ap[:, 0:128], x4.ap[:, j, k, :], wq.ap[:, k, 640:768], k == 0, k == 7, [wq.buf, x4.buf], [bank.buf])
                self.tt("dve", self.va.ap[:, kb, :], bank.ap[:, 0:128], bva.ap, ALU.add, [bank.buf, bva.buf], [self.va.buf])
                bi = bk.next()
                bank = self.banks[bi]
                for k in range(8):
                    self.mm(bank.ap, x4.ap[:, j, k, :], wq.ap[:, k, 1792:2304], k == 0, k == 7, [wq.buf, x4.buf], [bank.buf])
                self.tt("dve", self.vs.ap[:, kb, :], bank.ap, bvs.ap, ALU.add, [bank.buf, bvs.buf], [self.vs.buf])

    def phase_swa(self, l, b):
        cv, d, db, S, NQB = self.cv, self.d, self.db, self.S, self.NQB
        sink = cv.alloc([8], F32)
        self.dma("sp", sink.ap, d["sinks"][l, :].partition_broadcast(128), [db["sinks"]], [sink.buf])
        sets = []
        for base in (0, 4):
            sets.append(dict(
                ps=[self.banks[base], self.banks[base + 1]], pt=base + 2, po=self.banks[base + 3],
                s=cv.alloc([4, 256], F32), p=cv.alloc([4, 256], BF16), pT=cv.alloc([4, 2, 128], BF16),
                m=cv.alloc([4], F32), negm=cv.alloc([4], F32), rs=cv.alloc([4], F32), es=cv.alloc([4], F32),
                rden=cv.alloc([4], F32)))
        it = 0
        for qb in range(NQB):
            nkb = 1 if qb == 0 else 2
            nk = nkb * 128
            k0 = qb * 128 if qb == 0 else (qb - 1) * 128
            koff = 128 if qb == 0 else 0
            for g in range(2):
                W = sets[it % 2]
                it += 1
                for hh in range(4):
                    bank = W["ps"][hh // 2]
                    self.mm(bank.ap[:, (hh % 2) * 256:(hh % 2) * 256 + nk],
                            self.qTa.ap[g * 64:(g + 1) * 64, hh, qb * 128:(qb + 1) * 128],
                            self.kTa.ap[g * 64:(g + 1) * 64, k0:k0 + nk], True, True,
                            [self.qTa.buf, self.kTa.buf], [bank.buf])
                s, p, pT = W["s"], W["p"], W["pT"]
                for half in range(2):
                    bank = W["ps"][half]
                    self.tt("dve", s.ap[:, half * 2:half * 2 + 2, 0:nk],
                            bank.ap.rearrange("p (h k) -> p h k", k=256)[:, :, 0:nk],
                            self.swab[:, g * 4 + half * 2:g * 4 + half * 2 + 2, koff:koff + nk], ALU.add,
                            [bank.buf, self.cf.buf], [s.buf])
                m, negm, rs, es, rden = W["m"], W["negm"], W["rs"], W["es"], W["rden"]
                self.red("dve", m.ap, s.ap[:, :, 0:nk], ALU.max, [s.buf], [m.buf])
                self.tt("dve", m.ap, m.ap, sink.ap[:, g * 4:(g + 1) * 4], ALU.max, [m.buf, sink.buf], [m.buf])
                self.ts("dve", negm.ap, m.ap, -1.0, None, ALU.mult, None, [m.buf], [negm.buf])
                self.memset("pool", rs.ap, 0.0, [rs.buf])
                for hh in range(4):
                    self.act(p.ap[:, hh, 0:nk], s.ap[:, hh, 0:nk], AF.Exp, [s.buf, negm.buf, rs.buf], [p.buf, rs.buf],
                             bias=negm.ap[:, hh:hh + 1], accum=rs.ap[:, hh:hh + 1])
                self.tt("dve", es.ap, sink.ap[:, g * 4:(g + 1) * 4], negm.ap, ALU.add, [sink.buf, negm.buf], [es.buf])
                self.act(es.ap, es.ap, AF.Exp, [es.buf], [es.buf])
                self.tt("dve", rden.ap, rs.ap, es.ap, ALU.add, [rs.buf, es.buf], [rden.buf])
                self.tr.op("dve", lambda E, rden=rden: E.reciprocal(out=rden.ap, in_=rden.ap), [rden.buf], [rden.buf])
                ptv = self.bank_bf(W["pt"]).rearrange("p (h k) t -> p h k t", k=2)
                ptb = self.banks[W["pt"]].buf
                for hh in range(4):
                    for kk in range(nkb):
                        self.tp(ptv[:, hh, kk, :], p.ap[:, hh, kk * 128:(kk + 1) * 128], self.ident, [p.buf, self.cb.buf], [ptb])
                self.cp("act", pT.ap[:, :, 0:nkb, :], ptv[:, :, 0:nkb, :], [ptb], [pT.buf])
                po = W["po"]
                for hh in range(4):
                    for kk in range(nkb):
                        kb = qb if qb == 0 else qb - 1 + kk
                        self.mm(po.ap[:, hh * 64:(hh + 1) * 64], pT.ap[:, hh, kk, :], self.va.ap[:, kb, g * 64:(g + 1) * 64],
                                kk == 0, kk == nkb - 1, [pT.buf, self.va.buf], [po.buf])
                for hh in range(4):
                    h = g * 4 + hh
                    self.ts("dve", self.ya.ap[:, qb, h * 64:(h + 1) * 64], po.ap[:, hh * 64:(hh + 1) * 64],
                            rden.ap[:, hh:hh + 1], None, ALU.mult, None, [po.buf, rden.buf], [self.ya.buf])

    def phase_sb(self, l, b):
        cv, S, NG = self.cv, self.S, self.NG
        zb = Rot([0, 1, 2, 3])
        e_r = Rot([cv.alloc([512], F32) for _ in range(3)])
        sp_r = Rot([cv.alloc([512], BF16) for _ in range(3)])
        a_r = Rot([cv.alloc([512], BF16) for _ in range(3)])
        Ssets = [[cv.alloc([512], BF16) for _ in range(2)] for _ in range(2)]
        cbb = self.cb.buf
        pos = [self.banks[4 + j] for j in range(4)]
        steps = []
        hg = 0
        for h in range(8):
            for G in range(NG):
                for kb in range(4 * G + 3, -1, -1):
                    steps.append(dict(h=h, G=G, kb=kb, hg=hg, first=(kb == 4 * G + 3), last=(kb == 0),
                                      step=4 * G + 3 - kb))
                hg += 1
        n_slots = 8 * NG

        def stage_a(st):
            h, G, kb = st["h"], st["G"], st["kb"]
            c, pb = h // 2, (h % 2) * 64
            q0 = G * 512
            qlo = max(kb * 128, q0)
            off = qlo - q0
            zbank = self.banks[zb.next()]
            z = zbank.ap[:, off:512]
            self.mm(z, self.kTs.ap[pb:pb + 64, c, kb * 128:(kb + 1) * 128], self.qTs.ap[pb:pb + 64, c, qlo:q0 + 512],
                    True, False, [self.kTs.buf, self.qTs.buf], [zbank.buf])
            if kb >= 4 * G:
                self.mm(zbank.ap[:, off:off + 128], self.ident, self.negmask, False, False, [cbb], [zbank.buf])
            e, sp = e_r.next(), sp_r.next()
            self.act(e.ap[:, off:512], z, AF.Exp, [zbank.buf], [e.buf])
            self.act(sp.ap[:, off:512], e.ap[:, off:512], AF.Ln, [e.buf], [sp.buf], bias=1.0)
            st.update(zbank=zbank, z=z, off=off, sp=sp)

        def stage_b(st):
            h, G, kb, off, zbank, z, sp = st["h"], st["G"], st["kb"], st["off"], st["zbank"], st["z"], st["sp"]
            Sb = Ssets[st["hg"] % 2]
            if st["first"]:
                self.memset("pool", Sb[0].ap, 0.0, [Sb[0].buf])
                self.memset("pool", Sb[1].ap, 0.0, [Sb[1].buf])
            Scur, Snxt = Sb[st["step"] % 2], Sb[(st["step"] + 1) % 2]
            self.mm(z, self.wsuf, sp.ap[:, off:512], False, False, [cbb, sp.buf], [zbank.buf])
            self.mm(z, self.negones, Scur.ap[:, off:512], False, True, [cbb, Scur.buf], [zbank.buf])
            if kb > 0:
                self.tt("pool", Snxt.ap[:, off:512], Scur.ap[:, off:512], sp.ap[:, off:512], ALU.add,
                        [Scur.buf, sp.buf], [Snxt.buf])
            a = a_r.next()
            self.act(a.ap[:, off:512], z, AF.Exp, [zbank.buf], [a.buf])
            for j in range(off // 128, 4):
                self.mm(pos[j].ap[:, 0:64], a.ap[:, j * 128:(j + 1) * 128], self.vs.ap[:, kb, h * 64:(h + 1) * 64],
                        kb == 4 * G + j, kb == 0, [a.buf, self.vs.buf], [pos[j].buf])
            if st["last"]:
                for j in range(4):
                    self.cp("dve", self.ybs.ap[:, 4 * G + j, h * 64:(h + 1) * 64], pos[j].ap[:, 0:64], [pos[j].buf], [self.ybs.buf])
                self.bg_pump_slot()

        stage_a(steps[0])
        for i, st in enumerate(steps):
            if i + 1 < len(steps):
                stage_a(steps[i + 1])
            stage_b(st)

    def bg_pump_slot(self):
        self.bg_slots_left = max(self.bg_slots_left - 1, 0)
        n = -(-len(self.bg) // (self.bg_slots_left + 1))
        for _ in range(min(n, len(self.bg))):
            self.bg.pop(0)()

    def bg_flush(self):
        while self.bg:
            self.bg.pop(0)()

    def phase_tok(self, l, b):
        cv, d, db, S, NQB = self.cv, self.d, self.db, self.S, self.NQB
        wg = cv.alloc([8, 2048], BF16)
        for k in range(8):
            self.dma("pool", wg.ap[:, k, :], d["w_in"][l, k * 128:(k + 1) * 128, 2304:4352], [db["w_in"]], [wg.buf])
        wab = cv.alloc([8, D], BF16)
        self.dma("pool", wab.ap[:, 0:4, :], d["w_a"][l].rearrange("(k p) n -> p k n", p=128), [db["w_a"]], [wab.buf])
        self.dma("pool", wab.ap[:, 4:8, :], d["w_b"][l].rearrange("(k p) n -> p k n", p=128), [db["w_b"]], [wab.buf])
        wo = cv.alloc([8, D], BF16)
        self.dma("pool", wo.ap, d["w_out"][l].rearrange("(k p) n -> p k n", p=128), [db["w_out"]], [wo.buf])
        bg = cv.alloc([2048], F32)
        self.dma("sp", bg.ap, d["b_in"][l, 2304:4352].partition_broadcast(128), [db["b_in"]], [bg.buf])
        lg = cv.alloc([D], F32)
        lb = cv.alloc([D], F32)
        self.dma("sp", lg.ap, d["ln1_g"][l, :].partition_broadcast(128), [db["ln1_g"]], [lg.buf])
        self.dma("sp", lb.ap, d["ln1_b"][l, :].partition_broadcast(128), [db["ln1_b"]], [lb.buf])
        xTt = Rot([cv.alloc([8, 128], BF16) for _ in range(2)])
        xr = Rot([cv.alloc([D], F32) for _ in range(2)])
        gt = Rot([cv.alloc([2048], F32) for _ in range(1)])
        yT = Rot([cv.alloc([8, 128], BF16) for _ in range(2)])
        t1 = Rot([cv.alloc([512], F32) for _ in range(2)])
        t2 = Rot([cv.alloc([512], F32) for _ in range(2)])
        mg = Rot([cv.alloc([D], BF16) for _ in range(1)])
        mT = Rot([cv.alloc([8, 128], BF16) for _ in range(2)])
        u = Rot([cv.alloc([D], F32) for _ in range(1)])
        x1 = Rot([cv.alloc([D], F32) for _ in range(1)])
        x1b = Rot([cv.alloc([D], BF16) for _ in range(2)])
        x1T = Rot([cv.alloc([8, 128], F32) for _ in range(1)])
        st6 = Rot([cv.alloc([2, 6], F32) for _ in range(2)])
        mv = Rot([cv.alloc([2], F32) for _ in range(2)])
        rstd = Rot([cv.alloc([1], F32) for _ in range(2)])
        bk = Rot([0, 1, 2, 3])
        tb = Rot([4, 5])
        t0 = b * NQB
        src = d["x"] if l == 0 else d["xres"]
        sb_ = db["x"] if l == 0 else db["xres"]
        loaded = {}

        def load(t):
            i = t0 + t
            xt, xrt = xTt.next(), xr.next()
            self.dma("sp", xt.ap, d["xT"][i * 128:(i + 1) * 128, :].rearrange("p (k t) -> p k t", t=128), [db["xT"]], [xt.buf])
            self.dma("sp", xrt.ap, src[i * 128:(i + 1) * 128, :], [sb_], [xrt.buf])
            loaded[t] = (xt, xrt)

        load(0)
        for t in range(NQB):
            i = t0 + t
            if t + 1 < NQB:
                load(t + 1)
            xt, xrt = loaded.pop(t)
            g = gt.next()
            for nh in range(4):
                bank = self.banks[bk.next()]
                for k in range(8):
                    self.mm(bank.ap, xt.ap[:, k, :], wg.ap[:, k, nh * 512:(nh + 1) * 512], k == 0, k == 7, [xt.buf, wg.buf], [bank.buf])
                self.tt("dve", g.ap[:, nh * 512:(nh + 1) * 512], bank.ap, bg.ap[:, nh * 512:(nh + 1) * 512], ALU.add,
                        [bank.buf, bg.buf], [g.buf])
            self.act(g.ap, g.ap, AF.Sigmoid, [g.buf], [g.buf])
            tbi = tb.next()
            pt = self.bank_bf(tbi)
            ptb = self.banks[tbi].buf
            for k in range(4):
                self.tp(pt[:, k, :], self.ya.ap[:, t, k * 128:(k + 1) * 128], self.ident, [self.ya.buf, self.cb.buf], [ptb])
            for k in range(4):
                self.tp(pt[:, 4 + k, :], self.ybs.ap[:, t, k * 128:(k + 1) * 128], self.ident, [self.ybs.buf, self.cb.buf], [ptb])
            yTt = yT.next()
            self.cp("act", yTt.ap, pt, [ptb], [yTt.buf])
            mgt = mg.next()
            for nh in range(2):
                ba = self.banks[bk.next()]
                for k in range(4):
                    self.mm(ba.ap, yTt.ap[:, k, :], wab.ap[:, k, nh * 512:(nh + 1) * 512], k == 0, k == 3, [yTt.buf, wab.buf], [ba.buf])
                bb = self.banks[bk.next()]
                for k in range(4):
                    self.mm(bb.ap, yTt.ap[:, 4 + k, :], wab.ap[:, 4 + k, nh * 512:(nh + 1) * 512], k == 0, k == 3,
                            [yTt.buf, wab.buf], [bb.buf])
                a1, a2 = t1.next(), t2.next()
                self.tt("dve", a1.ap, ba.ap, g.ap[:, nh * 512:(nh + 1) * 512], ALU.mult, [ba.buf, g.buf], [a1.buf])
                self.tt("dve", a2.ap, bb.ap, g.ap[:, 1024 + nh * 512:1024 + (nh + 1) * 512], ALU.mult, [bb.buf, g.buf], [a2.buf])
                self.tt("pool", mgt.ap[:, nh * 512:(nh + 1) * 512], a1.ap, a2.ap, ALU.add, [a1.buf, a2.buf], [mgt.buf])
            tbi = tb.next()
            pt = self.bank_bf(tbi)
            ptb = self.banks[tbi].buf
            for k in range(8):
                self.tp(pt[:, k, :], mgt.ap[:, k * 128:(k + 1) * 128], self.ident, [mgt.buf, self.cb.buf], [ptb])
            mTt = mT.next()
            self.cp("act", mTt.ap, pt, [ptb], [mTt.buf])
            ut = u.next()
            for nh in range(2):
                bo = self.banks[bk.next()]
                for k in range(8):
                    self.mm(bo.ap, mTt.ap[:, k, :], wo.ap[:, k, nh * 512:(nh + 1) * 512], k == 0, k == 7, [mTt.buf, wo.buf], [bo.buf])
                self.stt("dve", ut.ap[:, nh * 512:(nh + 1) * 512], xrt.ap[:, nh * 512:(nh + 1) * 512], ALPHA, bo.ap,
                         ALU.mult, ALU.add, [xrt.buf, bo.buf], [ut.buf])
            x1t = x1.next()
            self.layernorm(ut, lg, lb, x1t, st6.next(), mv.next(), rstd.next())
            self.dma("sp", d["x1"][i * 128:(i + 1) * 128, :], x1t.ap, [x1t.buf], [], [db["x1"]])
            x1bt = x1b.next()
            self.cp("pool", x1bt.ap, x1t.ap, [x1t.buf], [x1bt.buf])
            self.dma("sp", d["x1b"][i * 128:(i + 1) * 128, :], x1bt.ap, [x1bt.buf], [], [db["x1b"]])
            if self.debug:
                self.dma("sp", d["dbg_y"][i * 128:(i + 1) * 128, 0:512], self.ya.ap[:, t, :], [self.ya.buf], [], [db["dbg_y"]])
                self.dma("sp", d["dbg_y"][i * 128:(i + 1) * 128, 512:1024], self.ybs.ap[:, t, :], [self.ybs.buf], [], [db["dbg_y"]])
            x1Tt = x1T.next()
            for hf in range(2):
                fb = self.banks[6 + hf]
                for k in range(4):
                    kk = hf * 4 + k
                    self.tp(fb.ap[:, k * 128:(k + 1) * 128], x1t.ap[:, kk * 128:(kk + 1) * 128], self.identf,
                            [x1t.buf, self.cf.buf], [fb.buf])
                self.cp("act", x1Tt.ap[:, hf * 4:hf * 4 + 4, :], fb.ap.rearrange("p (k t) -> p k t", t=128), [fb.buf], [x1Tt.buf])
            rbk = self.banks[bk.next()]
            for k in range(8):
                self.mm(rbk.ap[:, 0:NE], x1Tt.ap[:, k, :], self.wr.ap[:, k, :], k == 0, k == 7, [x1Tt.buf, self.wr.buf], [rbk.buf])
            self.act(self.aff.ap[:, i, :], rbk.ap[:, 0:NE], AF.Sigmoid, [rbk.buf], [self.aff.buf])

    def phase_route(self, l):
        cv, d, db, NT, NBLK = self.cv, self.d, self.db, self.NT, self.NBLK
        N = NT * NE
        A = lambda shape, dt=F32: cv.alloc(shape, dt)
        self.dlo_i = A([NT], I32)
        self.dhi_i = A([NT], I32)
        self.glo = A([NT])
        self.ghi = A([NT])
        self.widx = A([NBLK], I32)
        cv.mark()
        bsd = A([NT, NE])
        self.tt("dve", bsd.ap, self.aff.ap, self.rb.ap.unsqueeze(1).to_broadcast([128, NT, NE]), ALU.add,
                [self.aff.buf, self.rb.buf], [bsd.buf])
        b4 = bsd.ap.rearrange("p t (g f) -> p (t g) f", f=4)
        NGp = NT * 8
        top2 = A([NGp])
        thr = A([NGp])
        tmp = A([NGp])
        first = True
        for i in range(4):
            for j in range(i + 1, 4):
                if first:
                    self.tt("dve", top2.ap, b4[:, :, i], b4[:, :, j], ALU.add, [bsd.buf], [top2.buf])
                    self.tt("dve", thr.ap, b4[:, :, i], b4[:, :, j], ALU.min, [bsd.buf], [thr.buf])
                    first = False
                else:
                    self.tt("dve", tmp.ap, b4[:, :, i], b4[:, :, j], ALU.add, [bsd.buf], [tmp.buf])
                    self.tt("dve", top2.ap, top2.ap, tmp.ap, ALU.max, [top2.buf, tmp.buf], [top2.buf])
                    self.tt("dve", tmp.ap, b4[:, :, i], b4[:, :, j], ALU.min, [bsd.buf], [tmp.buf])
                    self.tt("dve", thr.ap, thr.ap, tmp.ap, ALU.max, [thr.buf, tmp.buf], [thr.buf])
        gmax = A([NT])
        t2v = top2.ap.rearrange("p (t g) -> p t g", g=8)
        self.red("dve", gmax.ap, t2v, ALU.max, [top2.buf], [gmax.buf])
        gsel = A([NT, 8])
        self.tt("dve", gsel.ap, t2v, gmax.ap.unsqueeze(2).to_broadcast([128, NT, 8]), ALU.is_ge, [top2.buf, gmax.buf], [gsel.buf])
        sel = A([NT, NE])
        s4 = sel.ap.rearrange("p t (g f) -> p (t g) f", f=4)
        self.tt("dve", s4, b4, thr.ap.unsqueeze(2).to_broadcast([128, NGp, 4]), ALU.is_ge, [bsd.buf, thr.buf], [sel.buf])
        self.tt("dve", s4, s4, gsel.ap.rearrange("p t g -> p (t g)").unsqueeze(2).to_broadcast([128, NGp, 4]), ALU.mult,
                [sel.buf, gsel.buf], [sel.buf])
        gd = A([NT, NE])
        self.tt("dve", gd.ap, sel.ap, self.aff.ap, ALU.mult, [sel.buf, self.aff.buf], [gd.buf])
        wsum = A([NT])
        self.red("dve", wsum.ap, gd.ap, ALU.add, [gd.buf], [wsum.buf])
        self.tr.op("dve", lambda E: E.reciprocal(out=wsum.ap, in_=wsum.ap), [wsum.buf], [wsum.buf])
        self.tt("dve", gd.ap, gd.ap, wsum.ap.unsqueeze(2).to_broadcast([128, NT, NE]), ALU.mult, [gd.buf, wsum.buf], [gd.buf])
        selb = A([N], BF16)
        self.cp("dve", selb.ap, sel.ap.rearrange("p t e -> p (t e)"), [sel.buf], [selb.buf])
        cnt = A([NT, NE])
        rank = A([NT, NE])
        cntf = cnt.ap.rearrange("p t e -> p (t e)")
        rankf = rank.ap.rearrange("p t e -> p (t e)")
        for c0 in range(0, N, 512):
            w = min(512, N - c0)
            b0, b1 = self.banks[0], self.banks[1]
            self.mm(b0.ap[:, 0:w], self.ones, selb.ap[:, c0:c0 + w], True, True, [self.cb.buf, selb.buf], [b0.buf])
            self.mm(b1.ap[:, 0:w], self.ustrict, selb.ap[:, c0:c0 + w], True, True, [self.cb.buf, selb.buf], [b1.buf])
            self.cp("dve", cntf[:, c0:c0 + w], b0.ap[:, 0:w], [b0.buf], [cnt.buf])
            self.cp("dve", rankf[:, c0:c0 + w], b1.ap[:, 0:w], [b1.buf], [rank.buf])
        cum = A([NT + 1, NE])
        self.memset("dve", cum.ap[:, 0, :], 0.0, [cum.buf])
        for i in range(NT):
            self.tt("dve", cum.ap[:, i + 1, :], cum.ap[:, i, :], cnt.ap[:, i, :], ALU.add, [cum.buf, cnt.buf], [cum.buf])
        pad = A([NE])
        cmp_ = A([NE, NT])
        self.tt("dve", cmp_.ap, cum.ap[:, NT, :].unsqueeze(2).to_broadcast([128, NE, NT]),
                self.bstart[:, 0:NT].unsqueeze(1).to_broadcast([128, NE, NT]), ALU.is_gt, [cum.buf, self.cf.buf], [cmp_.buf])
        self.red("dve", pad.ap, cmp_.ap, ALU.add, [cmp_.buf], [pad.buf])
        self.ts("dve", pad.ap, pad.ap, 128.0, None, ALU.mult, None, [pad.buf], [pad.buf])
        pend = A([NE + 1])
        self.memset("dve", pend.ap[:, 0:1], 0.0, [pend.buf])
        for e in range(NE):
            self.tt("dve", pend.ap[:, e + 1:e + 2], pend.ap[:, e:e + 1], pad.ap[:, e:e + 1], ALU.add, [pend.buf, pad.buf], [pend.buf])
        dest = A([NT, NE])
        self.tt("dve", dest.ap, cum.ap[:, 0:NT, :], rank.ap, ALU.add, [cum.buf, rank.buf], [dest.buf])
        self.tt("dve", dest.ap, dest.ap, pend.ap[:, 0:NE].unsqueeze(1).to_broadcast([128, NT, NE]), ALU.add,
                [dest.buf, pend.buf], [dest.buf])
        BIG = 1.0e6
        dm = A([NT, NE])
        msk = A([NT, NE])
        self.ts("dve", msk.ap, sel.ap, -1.0, None, ALU.add, None, [sel.buf], [msk.buf])
        self.ts("dve", msk.ap, msk.ap, -BIG, None, ALU.mult, None, [msk.buf], [msk.buf])
        self.tt("dve", dm.ap, dest.ap, msk.ap, ALU.add, [dest.buf, msk.buf], [dm.buf])
        dlo = A([NT])
        dhi = A([NT])
        self.red("dve", dlo.ap, dm.ap, ALU.min, [dm.buf], [dlo.buf])
        self.tt("dve", dm.ap, dest.ap, msk.ap, ALU.subtract, [dest.buf, msk.buf], [dm.buf])
        self.red("dve", dhi.ap, dm.ap, ALU.max, [dm.buf], [dhi.buf])
        glo, ghi = self.glo, self.ghi
        eq = A([NT, NE])
        self.tt("dve", eq.ap, dest.ap, dlo.ap.unsqueeze(2).to_broadcast([128, NT, NE]), ALU.is_equal, [dest.buf, dlo.buf], [eq.buf])
        self.tt("dve", eq.ap, eq.ap, gd.ap, ALU.mult, [eq.buf, gd.buf], [eq.buf])
        self.red("dve", glo.ap, eq.ap, ALU.add, [eq.buf], [glo.buf])
        self.tt("dve", eq.ap, dest.ap, dhi.ap.unsqueeze(2).to_broadcast([128, NT, NE]), ALU.is_equal, [dest.buf, dhi.buf], [eq.buf])
        self.tt("dve", eq.ap, eq.ap, gd.ap, ALU.mult, [eq.buf, gd.buf], [eq.buf])
        self.red("dve", ghi.ap, eq.ap, ALU.add, [eq.buf], [ghi.buf])
        self.cp("dve", self.dlo_i.ap, dlo.ap, [dlo.buf], [self.dlo_i.buf])
        self.cp("dve", self.dhi_i.ap, dhi.ap, [dhi.buf], [self.dhi_i.buf])
        be = A([NBLK])
        self.memset("dve", be.ap, 0.0, [be.buf])
        for e in range(NE):
            self.stt("dve", be.ap, self.bstart, pend.ap[:, e + 1:e + 2], be.ap, ALU.is_ge, ALU.add,
                     [self.cf.buf, pend.buf, be.buf], [be.buf])
        self.ts("dve", be.ap, be.ap, float(NE - 1), None, ALU.min, None, [be.buf], [be.buf])
        self.ts("dve", be.ap, be.ap, 128.0, None, ALU.mult, None, [be.buf], [be.buf])
        self.ts("dve", be.ap, be.ap, self.pidx, None, ALU.add, None, [be.buf, self.cf.buf], [be.buf])
        self.cp("dve", self.widx.ap, be.ap, [be.buf], [self.widx.buf])
        xl = Rot([cv.alloc([D], BF16) for _ in range(3)])
        for i in range(NT):
            xt = xl.next()
            self.dma("sp", xt.ap, d["x1b"][i * 128:(i + 1) * 128, :], [db["x1b"]], [xt.buf])
            self.scatter(d["xs"], self.dlo_i.ap[:, i:i + 1], xt.ap, [xt.buf, self.dlo_i.buf], [db["xs"]])
            self.scatter(d["xs"], self.dhi_i.ap[:, i:i + 1], xt.ap, [xt.buf, self.dhi_i.buf], [db["xs"]])

    def phase_moe_blocks(self, l):
        cv, d, db, NBLK = self.cv, self.d, self.db, self.NBLK
        wgs = Rot([cv.alloc([8, DE], BF16) for _ in range(2)])
        wus = Rot([cv.alloc([8, DE], BF16) for _ in range(2)])
        wds = Rot([cv.alloc([4, D], BF16) for _ in range(2)])
        xsb = Rot([cv.alloc([D], BF16) for _ in range(2)])
        xsT = Rot([cv.alloc([8, 128], BF16) for _ in range(2)])
        sg = Rot([cv.alloc([DE], F32) for _ in range(2)])
        hb = Rot([cv.alloc([DE], BF16) for _ in range(2)])
        hT = Rot([cv.alloc([4, 128], BF16) for _ in range(2)])
        yo = Rot([cv.alloc([D], F32) for _ in range(2)])
        bk = Rot([0, 1, 2, 3, 4, 5])
        tb = Rot([6, 7])
        for blk in range(NBLK):
            wgt, wut, wdt = wgs.next(), wus.next(), wds.next()
            ix = self.widx.ap[:, blk:blk + 1]
            sfx = str(l % 2)
            self.gather(wgt.ap.rearrange("p k n -> p (k n)"), d["wg_d" + sfx], ix, [db["wg_d" + sfx], self.widx.buf], [wgt.buf])
            self.gather(wut.ap.rearrange("p k n -> p (k n)"), d["wu_d" + sfx], ix, [db["wu_d" + sfx], self.widx.buf], [wut.buf])
            self.gather(wdt.ap.rearrange("p k n -> p (k n)"), d["wd_d" + sfx], ix, [db["wd_d" + sfx], self.widx.buf], [wdt.buf])
            xt = xsb.next()
            self.dma("sp", xt.ap, d["xs"][blk * 128:(blk + 1) * 128, :], [db["xs"]], [xt.buf])
            tbi = tb.next()
            pt, ptb = self.bank_bf(tbi), self.banks[tbi].buf
            for k in range(8):
                self.tp(pt[:, k, :], xt.ap[:, k * 128:(k + 1) * 128], self.ident, [xt.buf, self.cb.buf], [ptb])
            xT = xsT.next()
            self.cp("act", xT.ap, pt, [ptb], [xT.buf])
            bg_, bu_ = self.banks[bk.next()], self.banks[bk.next()]
            for k in range(8):
                self.mm(bg_.ap, xT.ap[:, k, :], wgt.ap[:, k, :], k == 0, k == 7, [xT.buf, wgt.buf], [bg_.buf])
            for k in range(8):
                self.mm(bu_.ap, xT.ap[:, k, :], wut.ap[:, k, :], k == 0, k == 7, [xT.buf, wut.buf], [bu_.buf])
            sgt, ht = sg.next(), hb.next()
            self.act(sgt.ap, bg_.ap, AF.Silu, [bg_.buf], [sgt.buf])
            self.tt("dve", ht.ap, sgt.ap, bu_.ap, ALU.mult, [sgt.buf, bu_.buf], [ht.buf])
            tbi = tb.next()
            pt, ptb = self.bank_bf(tbi), self.banks[tbi].buf
            for k in range(4):
                self.tp(pt[:, k, :], ht.ap[:, k * 128:(k + 1) * 128], self.ident, [ht.buf, self.cb.buf], [ptb])
            hTt = hT.next()
            self.cp("act", hTt.ap, pt[:, 0:4, :], [ptb], [hTt.buf])
            yt = yo.next()
            for nh in range(2):
                by = self.banks[bk.next()]
                for k in range(4):
                    self.mm(by.ap, hTt.ap[:, k, :], wdt.ap[:, k, nh * 512:(nh + 1) * 512], k == 0, k == 3, [hTt.buf, wdt.buf], [by.buf])
                self.cp("dve", yt.ap[:, nh * 512:(nh + 1) * 512], by.ap, [by.buf], [yt.buf])
            self.dma("pool", d["yb"][blk * 128:(blk + 1) * 128, :], yt.ap, [yt.buf], [], [db["yb"]])

    def phase_combine(self, l):
        cv, d, db, NT = self.cv, self.d, self.db, self.NT
        last = l == self.L - 1
        lg = cv.alloc([D], F32)
        lb = cv.alloc([D], F32)
        self.dma("sp", lg.ap, d["ln2_g"][l, :].partition_broadcast(128), [db["ln2_g"]], [lg.buf])
        self.dma("sp", lb.ap, d["ln2_b"][l, :].partition_broadcast(128), [db["ln2_b"]], [lb.buf])
        y0 = Rot([cv.alloc([D], F32) for _ in range(2)])
        y1 = Rot([cv.alloc([D], F32) for _ in range(2)])
        x1 = Rot([cv.alloc([D], F32) for _ in range(2)])
        u = Rot([cv.alloc([D], F32) for _ in range(2)])
        xo = Rot([cv.alloc([D], F32) for _ in range(2)])
        xb = Rot([cv.alloc([D], BF16) for _ in range(2)])
        xTt = Rot([cv.alloc([8, 128], BF16) for _ in range(2)])
        st6 = Rot([cv.alloc([2, 6], F32) for _ in range(2)])
        mv = Rot([cv.alloc([2], F32) for _ in range(2)])
        rstd = Rot([cv.alloc([1], F32) for _ in range(2)])
        bk = Rot([6, 7])
        loaded = {}

        def load(i):
            a0, a1, xt = y0.next(), y1.next(), x1.next()
            self.gather(a0.ap, d["yb"], self.dlo_i.ap[:, i:i + 1], [db["yb"], self.dlo_i.buf], [a0.buf])
            self.gather(a1.ap, d["yb"], self.dhi_i.ap[:, i:i + 1], [db["yb"], self.dhi_i.buf], [a1.buf])
            self.dma("sp", xt.ap, d["x1"][i * 128:(i + 1) * 128, :], [db["x1"]], [xt.buf])
            loaded[i] = (a0, a1, xt)

        load(0)
        for i in range(NT):
            if i + 1 < NT:
                load(i + 1)
            a0, a1, xt = loaded.pop(i)
            ut = u.next()
            self.ts("dve", ut.ap, a0.ap, self.glo.ap[:, i:i + 1], None, ALU.mult, None, [a0.buf, self.glo.buf], [ut.buf])
            self.stt("dve", ut.ap, a1.ap, self.ghi.ap[:, i:i + 1], ut.ap, ALU.mult, ALU.add, [a1.buf, self.ghi.buf, ut.buf], [ut.buf])
            self.stt("dve", ut.ap, xt.ap, ALPHA, ut.ap, ALU.mult, ALU.add, [xt.buf, ut.buf], [ut.buf])
            xot = xo.next()
            self.layernorm(ut, lg, lb, xot, st6.next(), mv.next(), rstd.next())
            if last:
                self.dma("sp", d["y"][i * 128:(i + 1) * 128, :], xot.ap, [xot.buf], [], [db["y"]])
            else:
                self.dma("sp", d["xres"][i * 128:(i + 1) * 128, :], xot.ap, [xot.buf], [], [db["xres"]])
                self.emit_xT(xot, i, xb.next(), xTt.next(), bk.next())


def make_consts(NBLK):
    cb = np.zeros((128, 6, 128), np.float32)
    i = np.arange(128)
    cb[:, 0] = np.eye(128)
    cb[:, 1] = -1.0 * (i[:, None] >= i[None, :])
    cb[:, 2] = -1.0
    cb[:, 3] = np.where(i[:, None] >= i[None, :], NEG, 0.0)
    cb[:, 4] = (i[:, None] < i[None, :])
    cb[:, 5] = 1.0
    ncf = 128 + 8 * 256 + 1 + NBLK + 2
    cf = np.zeros((128, ncf), np.float32)
    cf[:, 0:128] = np.eye(128)
    slopes = np.exp2(-8.0 * (np.arange(8, dtype=np.float32) + 1.0) / 8).astype(np.float32)
    j = np.arange(256)
    dist = i[:, None] + 128 - j[None, :]
    valid = (dist >= 0) & (dist < 128)
    sb = np.where(valid[None], -slopes[:, None, None] * dist[None].astype(np.float32), NEG).astype(np.float32)
    cf[:, 128:128 + 2048] = sb.transpose(1, 0, 2).reshape(128, 2048)
    o = 128 + 2048
    cf[:, o] = i
    cf[:, o + 1:o + 1 + NBLK] = 128.0 * np.arange(NBLK)[None, :]
    return cb.reshape(128, 768).astype(NPBF), cf


_CACHE = {}


def get_prog(S, NB, L, debug=False):
    key = (S, NB, L, debug)
    if key not in _CACHE:
        p = Prog(S, NB, L, debug)
        nc = p.build()
        _CACHE[key] = (p, nc)
    return _CACHE[key]


def prep_shared(inp, L):
    f = lambda a: np.ascontiguousarray(np.asarray(a, dtype=np.float32))
    w_in = f(inp["w_in"])[:L]
    b_in = f(inp["b_in"])[:L]
    perm = np.arange(NPROJ)
    perm[:512] = np.concatenate([np.arange(h * 64, (h + 1) * 64) for h in QPERM])
    w_in = np.ascontiguousarray(w_in[:, :, perm])
    b_in = np.ascontiguousarray(b_in[:, perm])
    b_in_fm = np.ascontiguousarray(b_in.reshape(L, 34, 128).transpose(0, 2, 1))
    sh = dict(
        w_in=w_in, b_in=b_in, b_in_fm=b_in_fm, sinks=f(inp["attn_sinks"])[:L],
        w_a=f(inp["w_branch_a"])[:L], w_b=f(inp["w_branch_b"])[:L], w_out=f(inp["w_out"])[:L],
        ln1_g=f(inp["ln1_g"])[:L], ln1_b=f(inp["ln1_b"])[:L], ln2_g=f(inp["ln2_g"])[:L], ln2_b=f(inp["ln2_b"])[:L],
        w_router=f(inp["w_router"]), router_bias=f(inp["router_bias"]).reshape(1, NE),
        w_gate=f(inp["w_gate"])[:L], w_up=f(inp["w_up"])[:L], w_down=f(inp["w_down"])[:L],
    )
    return sh


def run(inp, S, NB, L, n_cores, debug=False):
    p, nc = get_prog(S, NB, L, debug)
    sh = prep_shared(inp, L)
    cb, cf = make_consts(p.NBLK)
    sh["cb"] = cb
    sh["cf"] = cf
    x = np.ascontiguousarray(np.asarray(inp["x"], dtype=np.float32)).reshape(n_cores, NB * S, D)
    in_maps = []
    for c in range(n_cores):
        m = dict(sh)
        m["x"] = x[c]
        in_maps.append(m)
    res = run_bass_kernel_spmd(nc, in_maps, core_ids=list(range(n_cores)))
    return res.results


def kernel(x, w_in, b_in, attn_sinks, w_branch_a, w_branch_b, w_out, ln1_g, ln1_b,
           w_router, router_bias, w_gate, w_up, w_down, ln2_g, ln2_b):
    inp = dict(x=x, w_in=w_in, b_in=b_in, attn_sinks=attn_sinks, w_branch_a=w_branch_a, w_branch_b=w_branch_b,
               w_out=w_out, ln1_g=ln1_g, ln1_b=ln1_b, w_router=w_router, router_bias=router_bias,
               w_gate=w_gate, w_up=w_up, w_down=w_down, ln2_g=ln2_g, ln2_b=ln2_b)
    B, S, _ = np.asarray(x).shape
    n_cores = 8
    NB = B // n_cores
    res = run(inp, S, NB, 4, n_cores)
    out = np.stack([r["y"] for r in res], 0).reshape(B, S, D)
    return out.astype(np.float32)
```

```python
from contextlib import ExitStack

import ml_dtypes
import numpy as np

import concourse.bass as bass
import concourse.mybir as mybir
from concourse.bass_utils import run_bass_kernel_spmd

F32 = mybir.dt.float32
BF16 = mybir.dt.bfloat16
I32 = mybir.dt.int32
U8 = mybir.dt.uint8
ALU = mybir.AluOpType
AF = mybir.ActivationFunctionType
AX = mybir.AxisListType
NPBF = ml_dtypes.bfloat16

D = 1024
NE = 32
DE = 512
NPROJ = 4352
ALPHA = float((2 * 4) ** 0.25)
EPS = 1e-5
NEG = -30000.0
QPERM = [0, 4, 1, 5, 2, 6, 3, 7]
SBUF_BYTES = 206 * 1024
BG_IN_SB = False


class Buf:
    __slots__ = ("w", "r")

    def __init__(self):
        self.w = {}
        self.r = {}


class TT:
    __slots__ = ("ap", "buf")

    def __init__(self, ap, buf=None):
        self.ap = ap
        self.buf = buf if buf is not None else Buf()


class Rot:
    def __init__(self, items):
        self.items = items
        self.i = 0

    def next(self):
        it = self.items[self.i % len(self.items)]
        self.i += 1
        return it


class Tracker:
    COMPUTE = ("pe", "act", "dve", "pool")
    ALL = ("pe", "act", "dve", "pool", "sp")

    def __init__(self, nc, stack, n_dma_sems=8):
        self.nc = nc
        self.stack = stack
        self.ops = {e: [] for e in self.ALL}
        self.sem = {}
        self.cnt = {}
        self.nsem = 0
        self.seen = {e: {} for e in self.ALL}
        for e in self.COMPUTE:
            self._new_sem(e)
        self.dpool = {}
        self.dnext = {}
        for q in ("sp", "act", "pool"):
            self.dpool[q] = [[self._alloc(f"d_{q}_{i}"), 0] for i in range(n_dma_sems)]
            self.dnext[q] = 0
        self.bsem = self._alloc("barrier")
        self.bcount = 0
        self.ninstr = {e: 0 for e in self.ALL}
        self.abs = {e: [] for e in self.ALL}

    def _alloc(self, name):
        self.nsem += 1
        return self.stack.enter_context(self.nc.semaphore(name))

    def _new_sem(self, e):
        self.sem[e] = self._alloc(f"c_{e}_{self.nsem}")
        self.cnt[e] = 0

    def _wait(self, eng, s, v):
        if self.seen[eng].get(s, 0) >= v:
            return
        self.seen[eng][s] = v
        self.ops[eng].append(lambda E, s=s, v=v: E.wait_ge(s, v))
        self.abs[eng].append(("w", id(s), v))
        self.ninstr[eng] += 1

    @staticmethod
    def _deps(reads, writes):
        deps = {}
        for b in reads:
            for s, v in b.w.items():
                if deps.get(s, 0) < v:
                    deps[s] = v
        for b in writes:
            for d in (b.w, b.r):
                for s, v in d.items():
                    if deps.get(s, 0) < v:
                        deps[s] = v
        return deps

    def op(self, eng, fn, reads=(), writes=()):
        deps = self._deps(reads, writes)
        own = self.sem[eng]
        for s, v in deps.items():
            if eng == "pe" and s is own:
                continue
            self._wait(eng, s, v)
        self.cnt[eng] += 1
        v = self.cnt[eng]
        self.ops[eng].append(lambda E, fn=fn, own=own: fn(E).then_inc(own, 1))
        self.abs[eng].append(("i", id(own), 1))
        self.ninstr[eng] += 1
        for b in reads:
            if b.r.get(own, 0) < v:
                b.r[own] = v
        for b in writes:
            b.w = {own: v}
            b.r = {}
        if v >= 60000:
            self._new_sem(eng)

    def dma(self, q, fn, reads=(), writes=(), swrites=()):
        deps = self._deps(reads, writes)
        for b in swrites:
            for s, v in b.r.items():
                if deps.get(s, 0) < v:
                    deps[s] = v
        for s, v in deps.items():
            self._wait(q, s, v)
        slot = self.dpool[q][self.dnext[q]]
        self.dnext[q] = (self.dnext[q] + 1) % len(self.dpool[q])
        s = slot[0]
        if slot[1] > 0:
            self._wait(q, s, slot[1])
        slot[1] += 16
        v = slot[1]
        assert v < 65000
        self.ops[q].append(lambda E, fn=fn, s=s: fn(E).then_inc(s, 16))
        self.abs[q].append(("i", id(s), 16))
        self.ninstr[q] += 1
        for b in reads:
            if b.r.get(s, 0) < v:
                b.r[s] = v
        for b in writes:
            b.w = {s: v}
            b.r = {}
        for b in swrites:
            b.w[s] = v

    def barrier(self):
        self.bcount += len(self.ALL)
        bs, bc = self.bsem, self.bcount
        assert bc < 65000
        for e in self.ALL:
            if e in self.COMPUTE and self.cnt[e] > 0:
                self._wait(e, self.sem[e], self.cnt[e])
            if e in self.dpool:
                for s, v in self.dpool[e]:
                    if v > 0:
                        self._wait(e, s, v)
            self.ops[e].append(lambda E: E.sem_inc(bs, 1))
            self.ops[e].append(lambda E: E.wait_ge(bs, bc))
            self.abs[e].append(("i", id(bs), 1))
            self.abs[e].append(("w", id(bs), bc))
        floor = {}
        for e in self.COMPUTE:
            floor[self.sem[e]] = self.cnt[e]
        for q in self.dpool:
            for s, v in self.dpool[q]:
                floor[s] = v
        for e in self.ALL:
            for s, v in floor.items():
                if self.seen[e].get(s, 0) < v:
                    self.seen[e][s] = v

    def finish(self, block):
        self.barrier()
        ops = self.ops

        @block.tensor
        def _(E):
            for f in ops["pe"]:
                f(E)

        @block.scalar
        def _(E):
            for f in ops["act"]:
                f(E)

        @block.vector
        def _(E):
            for f in ops["dve"]:
                f(E)

        @block.gpsimd
        def _(E):
            for f in ops["pool"]:
                f(E)

        @block.sync
        def _(E):
            for f in ops["sp"]:
                f(E)


class Carver:
    def __init__(self, big, nbytes):
        self.big = big
        self.nbytes = nbytes
        self.off = 0
        self.marks = []
        self.peak = 0

    def mark(self):
        self.marks.append(self.off)

    def release(self):
        self.off = self.marks.pop()

    def alloc(self, shape, dtype, buf=None):
        esz = {F32: 4, BF16: 2, I32: 4}[dtype]
        n = int(np.prod(shape))
        nb = n * esz
        self.off = (self.off + 63) // 64 * 64
        assert self.off + nb <= self.nbytes, f"SBUF carve overflow {self.off}+{nb}>{self.nbytes}"
        ap = self.big[:, self.off:self.off + nb].bitcast(dtype)
        self.off += nb
        self.peak = max(self.peak, self.off)
        if len(shape) == 2:
            ap = ap.rearrange("p (a b) -> p a b", b=shape[1])
        elif len(shape) == 3:
            ap = ap.rearrange("p (a b c) -> p a b c", b=shape[1], c=shape[2])
        return TT(ap, buf)


class Prog:
    def __init__(self, S, NB, L, debug=False):
        self.S, self.NB, self.L, self.debug = S, NB, L, debug
        self.T = S * NB
        self.NT = self.T // 128
        self.NQB = S // 128
        self.NG = S // 512
        self.NBLK = 2 * self.NT + NE
        self.NCF = 128 + 8 * 256 + 1 + self.NBLK + 2

    def mm(self, out, lhsT, rhs, start, stop, reads, writes):
        self.tr.op("pe", lambda E: E.matmul(out, lhsT=lhsT, rhs=rhs, start=start, stop=stop), reads, writes)

    def tp(self, out, in_, ident, reads, writes):
        self.tr.op("pe", lambda E: E.transpose(out=out, in_=in_, identity=ident), reads, writes)

    def act(self, out, in_, func, reads, writes, bias=0.0, scale=1.0, accum=None):
        if accum is None:
            self.tr.op("act", lambda E: E.activation(out=out, in_=in_, func=func, bias=bias, scale=scale), reads, writes)
        else:
            self.tr.op("act", lambda E: E.activation(out=out, in_=in_, func=func, bias=bias, scale=scale,
                                                     accum_out=accum), reads, writes)

    def tt(self, eng, out, in0, in1, op, reads, writes):
        self.tr.op(eng, lambda E: E.tensor_tensor(out=out, in0=in0, in1=in1, op=op), reads, writes)

    def ts(self, eng, out, in0, s1, s2, op0, op1, reads, writes):
        if s2 is None:
            self.tr.op(eng, lambda E: E.tensor_scalar(out=out, in0=in0, scalar1=s1, scalar2=None, op0=op0), reads, writes)
        else:
            self.tr.op(eng, lambda E: E.tensor_scalar(out=out, in0=in0, scalar1=s1, scalar2=s2, op0=op0, op1=op1),
                       reads, writes)

    def stt(self, eng, out, in0, scalar, in1, op0, op1, reads, writes):
        self.tr.op(eng, lambda E: E.scalar_tensor_tensor(out=out, in0=in0, scalar=scalar, in1=in1, op0=op0, op1=op1),
                   reads, writes)

    def cp(self, eng, out, in_, reads, writes):
        if eng == "act":
            self.tr.op("act", lambda E: E.activation(out=out, in_=in_, func=AF.Copy), reads, writes)
        else:
            self.tr.op(eng, lambda E: E.tensor_copy(out=out, in_=in_), reads, writes)

    def red(self, eng, out, in_, op, reads, writes):
        self.tr.op(eng, lambda E: E.tensor_reduce(out=out, in_=in_, axis=AX.X, op=op), reads, writes)

    def memset(self, eng, ap, val, writes):
        self.tr.op(eng, lambda E: E.memset(ap, val), (), writes)

    def dma(self, q, out, in_, reads, writes, swrites=()):
        self.tr.dma(q, lambda E: E.dma_start(out=out, in_=in_), reads, writes, swrites)

    def gather(self, out, src, idx, reads, writes, bound=None):
        if bound is None:
            self.tr.dma("pool", lambda E: E.indirect_dma_start(
                out=out, out_offset=None, in_=src, in_offset=bass.IndirectOffsetOnAxis(ap=idx, axis=0)), reads, writes)
        else:
            self.tr.dma("pool", lambda E: E.indirect_dma_start(
                out=out, out_offset=None, in_=src, in_offset=bass.IndirectOffsetOnAxis(ap=idx, axis=0),
                bounds_check=bound, oob_is_err=False), reads, writes)

    def scatter(self, dst, idx, in_, reads, swrites):
        self.tr.dma("pool", lambda E: E.indirect_dma_start(
            out=dst, out_offset=bass.IndirectOffsetOnAxis(ap=idx, axis=0), in_=in_, in_offset=None), reads, (), swrites)

    def build(self):
        S, NB, L, T, NT, NBLK = self.S, self.NB, self.L, self.T, self.NT, self.NBLK
        nc = bass.Bass("TRN2", target_bir_lowering=False)
        self.nc = nc

        def din(name, shape, dt=F32):
            return nc.dram_tensor(name, list(shape), dt, kind="ExternalInput").ap()

        def dscr(name, shape, dt):
            return nc.dram_tensor(name, list(shape), dt, kind="Internal").ap()

        d = self.d = {}
        d["x"] = din("x", [T, D])
        d["w_in"] = din("w_in", [L, D, NPROJ])
        d["b_in"] = din("b_in", [L, NPROJ])
        d["b_in_fm"] = din("b_in_fm", [L, 128, 34])
        d["sinks"] = din("sinks", [L, 8])
        d["w_a"] = din("w_a", [L, 512, D])
        d["w_b"] = din("w_b", [L, 512, D])
        d["w_out"] = din("w_out", [L, D, D])
        for n in ("ln1_g", "ln1_b", "ln2_g", "ln2_b"):
            d[n] = din(n, [L, D])
        d["w_router"] = din("w_router", [D, NE])
        d["router_bias"] = din("router_bias", [1, NE])
        d["w_gate"] = din("w_gate", [L, NE, D, DE])
        d["w_up"] = din("w_up", [L, NE, D, DE])
        d["w_down"] = din("w_down", [L, NE, DE, D])
        d["cb"] = din("cb", [128, 6 * 128], BF16)
        d["cf"] = din("cf", [128, self.NCF])
        d["y"] = nc.dram_tensor("y", [T, D], F32, kind="ExternalOutput").ap()
        if self.debug:
            d["dbg_x1"] = nc.dram_tensor("dbg_x1", [T, D], F32, kind="ExternalOutput").ap()
            d["dbg_y"] = nc.dram_tensor("dbg_y", [T, D], BF16, kind="ExternalOutput").ap()
        d["xres"] = dscr("xres", [T, D], F32)
        d["x1"] = d["dbg_x1"] if self.debug else dscr("x1", [T, D], F32)
        d["x1b"] = dscr("x1b", [T, D], BF16)
        d["xT"] = dscr("xT", [NT * 128, D], BF16)
        d["xs"] = dscr("xs", [NBLK * 128, D], BF16)
        d["yb"] = dscr("yb", [NBLK * 128, D], F32)
        for sfx in ("0", "1"):
            d["wg_d" + sfx] = dscr("wg_d" + sfx, [NE * 128, 8 * DE], BF16)
            d["wu_d" + sfx] = dscr("wu_d" + sfx, [NE * 128, 8 * DE], BF16)
            d["wd_d" + sfx] = dscr("wd_d" + sfx, [NE * 128, 4 * D], BF16)
        self.db = {k: Buf() for k in d}

        with ExitStack() as st:
            big = st.enter_context(nc.sbuf_tensor("big", [128, SBUF_BYTES], U8))
            self.banks = []
            for i in range(8):
                t = st.enter_context(nc.psum_tensor(f"bank{i}", [128, 512], F32))
                self.banks.append(TT(t[:, :]))
            self.tr = Tracker(nc, st)
            self.cv = Carver(big, SBUF_BYTES)
            blk = st.enter_context(nc.Block())
            self.emit()
            self.tr.finish(blk)
            self.stats = dict(instr=dict(self.tr.ninstr), sems=self.tr.nsem, sbuf_peak=self.cv.peak)
        return nc

    def bank_bf(self, b):
        return self.banks[b].ap.bitcast(BF16).rearrange("p (a b) -> p a b", b=128)

    def emit(self):
        cv, d, db = self.cv, self.d, self.db
        NT, NBLK, L = self.NT, self.NBLK, self.L
        self.cb = cv.alloc([6, 128], BF16)
        self.cf = cv.alloc([self.NCF], F32)
        self.dma("sp", self.cb.ap, d["cb"].rearrange("p (a b) -> p a b", b=128), [], [self.cb.buf])
        self.dma("sp", self.cf.ap, d["cf"], [], [self.cf.buf])
        cbb = self.cb.buf
        self.ident = self.cb.ap[:, 0, :]
        self.wsuf = self.cb.ap[:, 1, :]
        self.negones = self.cb.ap[:, 2, :]
        self.negmask = self.cb.ap[:, 3, :]
        self.ustrict = self.cb.ap[:, 4, :]
        self.ones = self.cb.ap[:, 5, :]
        self.identf = self.cf.ap[:, 0:128]
        self.swab = self.cf.ap[:, 128:128 + 2048].rearrange("p (h k) -> p h k", k=256)
        o = 128 + 2048
        self.pidx = self.cf.ap[:, o:o + 1]
        self.bstart = self.cf.ap[:, o + 1:o + 1 + NBLK]
        self.aff = cv.alloc([NT, NE], F32)
        self.wr = cv.alloc([8, NE], F32)
        self.rb = cv.alloc([NE], F32)
        self.dma("sp", self.wr.ap, d["w_router"].rearrange("(k p) e -> p k e", p=128), [], [self.wr.buf])
        self.dma("sp", self.rb.ap, d["router_bias"][0, :].partition_broadcast(128), [], [self.rb.buf])

        cv.mark()
        self.phase_xT0()
        self.tr.barrier()
        cv.release()
        self.bg = []
        self.precast_experts(0)
        for l in range(L):
            if l + 1 < L:
                self.precast_experts(l + 1)
            self.bg_slots_left = self.NB * 8 * self.NG
            if not BG_IN_SB:
                self.bg_flush()
            for b in range(self.NB):
                cv.mark()
                self.alloc_seq()
                cv.mark()
                self.phase_inproj(l, b)
                self.tr.barrier()
                cv.release()
                cv.mark()
                self.phase_swa(l, b)
                self.tr.barrier()
                cv.release()
                cv.mark()
                self.phase_sb(l, b)
                self.tr.barrier()
                cv.release()
                cv.release()
                cv.mark()
                self.phase_tok(l, b)
                self.tr.barrier()
                cv.release()
                cv.release()
            self.bg_flush()
            cv.mark()
            self.phase_route(l)
            self.tr.barrier()
            cv.release()
            cv.mark()
            self.phase_moe_blocks(l)
            self.tr.barrier()
            cv.release()
            cv.mark()
            self.phase_combine(l)
            self.tr.barrier()
            cv.release()
            cv.release()

    def emit_xT(self, src, i, xb, xTt, bank):
        d, db = self.d, self.db
        self.cp("pool", xb.ap, src.ap, [src.buf], [xb.buf])
        pt = self.bank_bf(bank)
        for k in range(8):
            self.tp(pt[:, k, :], xb.ap[:, k * 128:(k + 1) * 128], self.ident, [xb.buf, self.cb.buf], [self.banks[bank].buf])
        self.cp("act", xTt.ap, pt, [self.banks[bank].buf], [xTt.buf])
        self.dma("sp", d["xT"][i * 128:(i + 1) * 128, :].rearrange("p (k t) -> p k t", t=128), xTt.ap, [xTt.buf], [], [db["xT"]])

    def layernorm(self, u, g_bc, b_bc, out, st6, mv, rstd):
        for c in range(2):
            self.tr.op("dve", lambda E, c=c: E.bn_stats(out=st6.ap[:, c, :], in_=u.ap[:, c * 512:(c + 1) * 512]),
                       [u.buf], [st6.buf])
        self.tr.op("dve", lambda E: E.bn_aggr(out=mv.ap, in_=st6.ap), [st6.buf], [mv.buf])
        self.ts("dve", rstd.ap, mv.ap[:, 1:2], EPS, None, ALU.add, None, [mv.buf], [rstd.buf])
        self.act(rstd.ap, rstd.ap, AF.Sqrt, [rstd.buf], [rstd.buf])
        self.tr.op("dve", lambda E: E.reciprocal(out=rstd.ap, in_=rstd.ap), [rstd.buf], [rstd.buf])
        self.ts("dve", out.ap, u.ap, mv.ap[:, 0:1], rstd.ap[:, 0:1], ALU.subtract, ALU.mult,
                [u.buf, mv.buf, rstd.buf], [out.buf])
        self.tt("pool", out.ap, out.ap, g_bc.ap, ALU.mult, [out.buf, g_bc.buf], [out.buf])
        self.tt("dve", out.ap, out.ap, b_bc.ap, ALU.add, [out.buf, b_bc.buf], [out.buf])

    def phase_xT0(self):
        cv, d, db = self.cv, self.d, self.db
        xin = Rot([cv.alloc([D], F32) for _ in range(2)])
        xb = Rot([cv.alloc([D], BF16) for _ in range(2)])
        xTt = Rot([cv.alloc([8, 128], BF16) for _ in range(2)])
        bk = Rot([6, 7])
        for i in range(self.NT):
            xi = xin.next()
            self.dma("sp", xi.ap, d["x"][i * 128:(i + 1) * 128, :], [db["x"]], [xi.buf])
            self.emit_xT(xi, i, xb.next(), xTt.next(), bk.next())

    def precast_experts(self, l):
        d, db = self.d, self.db
        sfx = str(l % 2)
        for e in range(NE):
            for (dst, src, n) in (("wg_d", "w_gate", DE), ("wu_d", "w_up", DE), ("wd_d", "w_down", D)):
                self.bg.append(lambda dst=dst, src=src, n=n, e=e: self.dma(
                    "pool", d[dst + sfx][e * 128:(e + 1) * 128, :].rearrange("p (k n) -> p k n", n=n),
                    d[src][l, e].rearrange("(k p) n -> p k n", p=128), [db[src]], [], [db[dst + sfx]]))

    def alloc_seq(self):
        cv, S, NQB = self.cv, self.S, self.NQB
        self.ya = cv.alloc([NQB, 512], BF16)
        self.ybs = cv.alloc([NQB, 512], BF16)
        cv.mark()
        self.qTa = cv.alloc([4, S], BF16)
        self.kTa = cv.alloc([S], BF16)
        self.va = cv.alloc([NQB, 128], BF16)
        self.qTs = cv.alloc([4, S], BF16)
        self.kTs = cv.alloc([4, S], BF16)
        self.vs = cv.alloc([NQB, 512], BF16)

    def phase_inproj(self, l, b):
        cv, d, db, S = self.cv, self.d, self.db, self.S
        wq = cv.alloc([8, 2304], BF16)
        for k in range(8):
            self.dma("pool", wq.ap[:, k, :], d["w_in"][l, k * 128:(k + 1) * 128, 0:2304], [db["w_in"]], [wq.buf])
        bfm = cv.alloc([34], F32)
        self.dma("sp", bfm.ap, d["b_in_fm"][l], [db["b_in_fm"]], [bfm.buf])
        bfs = cv.alloc([34], F32)
        self.ts("dve", bfs.ap, bfm.ap, 0.125, None, ALU.mult, None, [bfm.buf], [bfs.buf])
        bva = cv.alloc([128], F32)
        bvs = cv.alloc([512], F32)
        self.dma("sp", bva.ap, d["b_in"][l, 640:768].partition_broadcast(128), [db["b_in"]], [bva.buf])
        self.dma("sp", bvs.ap, d["b_in"][l, 1792:2304].partition_broadcast(128), [db["b_in"]], [bvs.buf])
        xc = Rot([cv.alloc([4, 8, 128], BF16) for _ in range(2)])
        bk = Rot([0, 1, 2, 3, 4, 5])
        fm = []
        for c in range(4):
            fm.append((c * 128, self.qTa, c, 0.125))
        fm.append((512, self.kTa, None, 1.0))
        for c in range(4):
            fm.append((768 + c * 128, self.qTs, c, 0.125))
        for c in range(4):
            fm.append((1280 + c * 128, self.kTs, c, 1.0))
        t0 = b * S // 128
        ev = 0
        for tg in range(S // 512):
            x4 = xc.next()
            r0 = (t0 + tg * 4) * 128
            self.dma("sp", x4.ap, d["xT"][r0:r0 + 512, :].rearrange("(j p) (k t) -> p j k t", p=128, t=128),
                     [db["xT"]], [x4.buf])
            for (col, dst, chunk, scale) in fm:
                bi = bk.next()
                bank = self.banks[bi]
                for k in range(8):
                    self.mm(bank.ap.rearrange("p (j t) -> p j t", t=128), wq.ap[:, k, col:col + 128], x4.ap[:, :, k, :],
                            k == 0, k == 7, [wq.buf, x4.buf], [bank.buf])
                if chunk is None:
                    dap = dst.ap[:, tg * 512:(tg + 1) * 512]
                else:
                    dap = dst.ap[:, chunk, tg * 512:(tg + 1) * 512]
                bias = bfm.ap[:, col // 128:col // 128 + 1]
                if scale != 1.0:
                    self.act(dap, bank.ap, AF.Identity, [bank.buf, bfs.buf], [dst.buf],
                             bias=bfs.ap[:, col // 128:col // 128 + 1], scale=scale)
                else:
                    self.ts("dve", dap, bank.ap, bias, None, ALU.add, None, [bank.buf, bfm.buf], [dst.buf])
            for j in range(4):
                kb = tg * 4 + j
                bi = bk.next()
                bank = self.banks[bi]
                for k in range(8):
                    self.mm(bank.ap[:, 0:128], x4.ap[:, j, k, :], wq.ap[:, k, 640:768], k == 0, k == 7, [wq.buf, x4.buf], [bank.buf])
                self.tt("dve", self.va.ap[:, kb, :], bank.ap[:, 0:128], bva.ap, ALU.add, [bank.buf, bva.buf], [self.va.buf])
                bi = bk.next()
                bank = self.banks[bi]
                for k in range(8):
                    self.mm(bank.ap, x4.ap[:, j, k, :], wq.ap[:, k, 1792:2304], k == 0, k == 7, [wq.buf, x4.buf], [bank.buf])
                self.tt("dve", self.vs.ap[:, kb, :], bank.ap, bvs.ap, ALU.add, [bank.buf, bvs.buf], [self.vs.buf])

    def phase_swa(self, l, b):
        cv, d, db, S, NQB = self.cv, self.d, self.db, self.S, self.NQB
        sink = cv.alloc([8], F32)
        self.dma("sp", sink.ap, d["sinks"][l, :].partition_broadcast(128), [db["sinks"]], [sink.buf])
        sets = []
        for base in (0, 4):
            sets.append(dict(
                ps=[self.banks[base], self.banks[base + 1]], pt=base + 2, po=self.banks[base + 3],
                s=cv.alloc([4, 256], F32), p=cv.alloc([4, 256], BF16), pT=cv.alloc([4, 2, 128], BF16),
                m=cv.alloc([4], F32), negm=cv.alloc([4], F32), rs=cv.alloc([4], F32), es=cv.alloc([4], F32),
                rden=cv.alloc([4], F32)))
        iters = [(qb, g) for qb in range(NQB) for g in range(2)]

        def geom(qb):
            nkb = 1 if qb == 0 else 2
            k0 = qb * 128 if qb == 0 else (qb - 1) * 128
            koff = 128 if qb == 0 else 0
            return nkb, nkb * 128, k0, koff

        def stage_a(it):
            qb, g = iters[it]
            nkb, nk, k0, koff = geom(qb)
            W = sets[it % 2]
            for hh in range(4):
                bank = W["ps"][hh // 2]
                self.mm(bank.ap[:, (hh % 2) * 256:(hh % 2) * 256 + nk],
                        self.qTa.ap[g * 64:(g + 1) * 64, hh, qb * 128:(qb + 1) * 128],
                        self.kTa.ap[g * 64:(g + 1) * 64, k0:k0 + nk], True, True,
                        [self.qTa.buf, self.kTa.buf], [bank.buf])
            s, p = W["s"], W["p"]
            for half in range(2):
                bank = W["ps"][half]
                self.tt("dve", s.ap[:, half * 2:half * 2 + 2, 0:nk],
                        bank.ap.rearrange("p (h k) -> p h k", k=256)[:, :, 0:nk],
                        self.swab[:, g * 4 + half * 2:g * 4 + half * 2 + 2, koff:koff + nk], ALU.add,
                        [bank.buf, self.cf.buf], [s.buf])
            m, negm, rs, es, rden = W["m"], W["negm"], W["rs"], W["es"], W["rden"]
            self.red("dve", m.ap, s.ap[:, :, 0:nk], ALU.max, [s.buf], [m.buf])
            self.tt("dve", m.ap, m.ap, sink.ap[:, g * 4:(g + 1) * 4], ALU.max, [m.buf, sink.buf], [m.buf])
            self.ts("dve", negm.ap, m.ap, -1.0, None, ALU.mult, None, [m.buf], [negm.buf])
            self.memset("pool", rs.ap, 0.0, [rs.buf])
            for hh in range(4):
                self.act(p.ap[:, hh, 0:nk], s.ap[:, hh, 0:nk], AF.Exp, [s.buf, negm.buf, rs.buf], [p.buf, rs.buf],
                         bias=negm.ap[:, hh:hh + 1], accum=rs.ap[:, hh:hh + 1])
            self.tt("dve", es.ap, sink.ap[:, g * 4:(g + 1) * 4], negm.ap, ALU.add, [sink.buf, negm.buf], [es.buf])
            self.act(es.ap, es.ap, AF.Exp, [es.buf], [es.buf])
            self.tt("dve", rden.ap, rs.ap, es.ap, ALU.add, [rs.buf, es.buf], [rden.buf])
            self.tr.op("dve", lambda E, rden=rden: E.reciprocal(out=rden.ap, in_=rden.ap), [rden.buf], [rden.buf])

        def stage_b(it):
            qb, g = iters[it]
            nkb, nk, k0, koff = geom(qb)
            W = sets[it % 2]
            p, pT, rden = W["p"], W["pT"], W["rden"]
            ptv = self.bank_bf(W["pt"]).rearrange("p (h k) t -> p h k t", k=2)
            ptb = self.banks[W["pt"]].buf
            for hh in range(4):
                for kk in range(nkb):
                    self.tp(ptv[:, hh, kk, :], p.ap[:, hh, kk * 128:(kk + 1) * 128], self.ident, [p.buf, self.cb.buf], [ptb])
            self.cp("act", pT.ap[:, :, 0:nkb, :], ptv[:, :, 0:nkb, :], [ptb], [pT.buf])
            po = W["po"]
            for hh in range(4):
                for kk in range(nkb):
                    kb = qb if qb == 0 else qb - 1 + kk
                    self.mm(po.ap[:, hh * 64:(hh + 1) * 64], pT.ap[:, hh, kk, :], self.va.ap[:, kb, g * 64:(g + 1) * 64],
                            kk == 0, kk == nkb - 1, [pT.buf, self.va.buf], [po.buf])
            for hh in range(4):
                h = g * 4 + hh
                self.ts("dve", self.ya.ap[:, qb, h * 64:(h + 1) * 64], po.ap[:, hh * 64:(hh + 1) * 64],
                        rden.ap[:, hh:hh + 1], None, ALU.mult, None, [po.buf, rden.buf], [self.ya.buf])

        stage_a(0)
        for it in range(len(iters)):
            if it + 1 < len(iters):
                stage_a(it + 1)
            stage_b(it)

    def phase_sb(self, l, b):
        cv, S, NG = self.cv, self.S, self.NG
        zb = Rot([0, 1, 2, 3])
        e_r = Rot([cv.alloc([512], F32) for _ in range(3)])
        sp_r = Rot([cv.alloc([512], BF16) for _ in range(3)])
        a_r = Rot([cv.alloc([512], BF16) for _ in range(3)])
        Ssets = [[cv.alloc([512], BF16) for _ in range(2)] for _ in range(2)]
        cbb = self.cb.buf
        pos = [self.banks[4 + j] for j in range(4)]
        steps = []
        hg = 0
        for h in range(8):
            for G in range(NG):
                for kb in range(4 * G + 3, -1, -1):
                    steps.append(dict(h=h, G=G, kb=kb, hg=hg, first=(kb == 4 * G + 3), last=(kb == 0),
                                      step=4 * G + 3 - kb))
                hg += 1
        n_slots = 8 * NG

        def stage_a(st):
            h, G, kb = st["h"], st["G"], st["kb"]
            c, pb = h // 2, (h % 2) * 64
            q0 = G * 512
            qlo = max(kb * 128, q0)
            off = qlo - q0
            zbank = self.banks[zb.next()]
            z = zbank.ap[:, off:512]
            self.mm(z, self.kTs.ap[pb:pb + 64, c, kb * 128:(kb + 1) * 128], self.qTs.ap[pb:pb + 64, c, qlo:q0 + 512],
                    True, False, [self.kTs.buf, self.qTs.buf], [zbank.buf])
            if kb >= 4 * G:
                self.mm(zbank.ap[:, off:off + 128], self.ident, self.negmask, False, False, [cbb], [zbank.buf])
            e, sp = e_r.next(), sp_r.next()
            self.act(e.ap[:, off:512], z, AF.Exp, [zbank.buf], [e.buf])
            self.act(sp.ap[:, off:512], e.ap[:, off:512], AF.Ln, [e.buf], [sp.buf], bias=1.0)
            st.update(zbank=zbank, z=z, off=off, sp=sp)

        def stage_b(st):
            h, G, kb, off, zbank, z, sp = st["h"], st["G"], st["kb"], st["off"], st["zbank"], st["z"], st["sp"]
            Sb = Ssets[st["hg"] % 2]
            if st["first"]:
                self.memset("pool", Sb[0].ap, 0.0, [Sb[0].buf])
                self.memset("pool", Sb[1].ap, 0.0, [Sb[1].buf])
            Scur, Snxt = Sb[st["step"] % 2], Sb[(st["step"] + 1) % 2]
            self.mm(z, self.wsuf, sp.ap[:, off:512], False, False, [cbb, sp.buf], [zbank.buf])
            self.mm(z, self.negones, Scur.ap[:, off:512], False, True, [cbb, Scur.buf], [zbank.buf])
            if kb > 0:
                self.tt("pool", Snxt.ap[:, off:512], Scur.ap[:, off:512], sp.ap[:, off:512], ALU.add,
                        [Scur.buf, sp.buf], [Snxt.buf])
            a = a_r.next()
            self.act(a.ap[:, off:512], z, AF.Exp, [zbank.buf], [a.buf])
            for j in range(off // 128, 4):
                self.mm(pos[j].ap[:, 0:64], a.ap[:, j * 128:(j + 1) * 128], self.vs.ap[:, kb, h * 64:(h + 1) * 64],
                        kb == 4 * G + j, kb == 0, [a.buf, self.vs.buf], [pos[j].buf])
            if st["last"]:
                for j in range(4):
                    self.cp("dve", self.ybs.ap[:, 4 * G + j, h * 64:(h + 1) * 64], pos[j].ap[:, 0:64], [pos[j].buf], [self.ybs.buf])
                self.bg_pump_slot()

        stage_a(steps[0])
        for i, st in enumerate(steps):
            if i + 1 < len(steps):
                stage_a(steps[i + 1])
            stage_b(st)

    def bg_pump_slot(self):
        self.bg_slots_left = max(self.bg_slots_left - 1, 0)
        n = -(-len(self.bg) // (self.bg_slots_left + 1))
        for _ in range(min(n, len(self.bg))):
            self.bg.pop(0)()

    def bg_flush(self):
        while self.bg:
            self.bg.pop(0)()

    def phase_tok(self, l, b):
        cv, d, db, S, NQB = self.cv, self.d, self.db, self.S, self.NQB
        wg = cv.alloc([8, 2048], BF16)
        for k in range(8):
            self.dma("pool", wg.ap[:, k, :], d["w_in"][l, k * 128:(k + 1) * 128, 2304:4352], [db["w_in"]], [wg.buf])
        wab = cv.alloc([8, D], BF16)
        self.dma("pool", wab.ap[:, 0:4, :], d["w_a"][l].rearrange("(k p) n -> p k n", p=128), [db["w_a"]], [wab.buf])
        self.dma("pool", wab.ap[:, 4:8, :], d["w_b"][l].rearrange("(k p) n -> p k n", p=128), [db["w_b"]], [wab.buf])
        wo = cv.alloc([8, D], BF16)
        self.dma("pool", wo.ap, d["w_out"][l].rearrange("(k p) n -> p k n", p=128), [db["w_out"]], [wo.buf])
        bg = cv.alloc([2048], F32)
        self.dma("sp", bg.ap, d["b_in"][l, 2304:4352].partition_broadcast(128), [db["b_in"]], [bg.buf])
        lg = cv.alloc([D], F32)
        lb = cv.alloc([D], F32)
        self.dma("sp", lg.ap, d["ln1_g"][l, :].partition_broadcast(128), [db["ln1_g"]], [lg.buf])
        self.dma("sp", lb.ap, d["ln1_b"][l, :].partition_broadcast(128), [db["ln1_b"]], [lb.buf])
        xTt = Rot([cv.alloc([8, 128], BF16) for _ in range(2)])
        xr = Rot([cv.alloc([D], F32) for _ in range(3)])
        gt = Rot([cv.alloc([2048], F32) for _ in range(1)])
        yT = Rot([cv.alloc([8, 128], BF16) for _ in range(2)])
        t1 = Rot([cv.alloc([512], F32) for _ in range(2)])
        t2 = Rot([cv.alloc([512], F32) for _ in range(2)])
        mg = Rot([cv.alloc([D], BF16) for _ in range(2)])
        mT = Rot([cv.alloc([8, 128], BF16) for _ in range(2)])
        u = Rot([cv.alloc([D], F32) for _ in range(1)])
        x1 = Rot([cv.alloc([D], F32) for _ in range(2)])
        x1b = Rot([cv.alloc([D], BF16) for _ in range(2)])
        x1T = Rot([cv.alloc([8, 128], F32) for _ in range(1)])
        st6 = Rot([cv.alloc([2, 6], F32) for _ in range(2)])
        mv = Rot([cv.alloc([2], F32) for _ in range(2)])
        rstd = Rot([cv.alloc([1], F32) for _ in range(2)])
        bk = Rot([0, 1, 2, 3])
        tb = Rot([4, 5])
        t0 = b * NQB
        src = d["x"] if l == 0 else d["xres"]
        sb_ = db["x"] if l == 0 else db["xres"]
        T_ = {}

        def load(t):
            i = t0 + t
            xt, xrt = xTt.next(), xr.next()
            self.dma("sp", xt.ap, d["xT"][i * 128:(i + 1) * 128, :].rearrange("p (k t) -> p k t", t=128), [db["xT"]], [xt.buf])
            self.dma("sp", xrt.ap, src[i * 128:(i + 1) * 128, :], [sb_], [xrt.buf])
            T_[t] = dict(xt=xt, xrt=xrt)

        def s1(t):
            st = T_[t]
            xt = st["xt"]
            g = gt.next()
            tbi = tb.next()
            pt = self.bank_bf(tbi)
            ptb = self.banks[tbi].buf
            for k in range(4):
                self.tp(pt[:, k, :], self.ya.ap[:, t, k * 128:(k + 1) * 128], self.ident, [self.ya.buf, self.cb.buf], [ptb])
            for k in range(4):
                self.tp(pt[:, 4 + k, :], self.ybs.ap[:, t, k * 128:(k + 1) * 128], self.ident, [self.ybs.buf, self.cb.buf], [ptb])
            yTt = yT.next()
            self.cp("act", yTt.ap, pt, [ptb], [yTt.buf])
            for nh in range(4):
                bank = self.banks[bk.next()]
                for k in range(8):
                    self.mm(bank.ap, xt.ap[:, k, :], wg.ap[:, k, nh * 512:(nh + 1) * 512], k == 0, k == 7, [xt.buf, wg.buf], [bank.buf])
                self.tt("dve", g.ap[:, nh * 512:(nh + 1) * 512], bank.ap, bg.ap[:, nh * 512:(nh + 1) * 512], ALU.add,
                        [bank.buf, bg.buf], [g.buf])
            self.act(g.ap, g.ap, AF.Sigmoid, [g.buf], [g.buf])
            mgt = mg.next()
            for nh in range(2):
                ba = self.banks[bk.next()]
                for k in range(4):
                    self.mm(ba.ap, yTt.ap[:, k, :], wab.ap[:, k, nh * 512:(nh + 1) * 512], k == 0, k == 3, [yTt.buf, wab.buf], [ba.buf])
                bb = self.banks[bk.next()]
                for k in range(4):
                    self.mm(bb.ap, yTt.ap[:, 4 + k, :], wab.ap[:, 4 + k, nh * 512:(nh + 1) * 512], k == 0, k == 3,
                            [yTt.buf, wab.buf], [bb.buf])
                a1, a2 = t1.next(), t2.next()
                self.tt("dve", a1.ap, ba.ap, g.ap[:, nh * 512:(nh + 1) * 512], ALU.mult, [ba.buf, g.buf], [a1.buf])
                self.tt("dve", a2.ap, bb.ap, g.ap[:, 1024 + nh * 512:1024 + (nh + 1) * 512], ALU.mult, [bb.buf, g.buf], [a2.buf])
                self.tt("pool", mgt.ap[:, nh * 512:(nh + 1) * 512], a1.ap, a2.ap, ALU.add, [a1.buf, a2.buf], [mgt.buf])
            st["mgt"] = mgt
            if self.debug:
                i = t0 + t
                self.dma("sp", d["dbg_y"][i * 128:(i + 1) * 128, 0:512], self.ya.ap[:, t, :], [self.ya.buf], [], [db["dbg_y"]])
                self.dma("sp", d["dbg_y"][i * 128:(i + 1) * 128, 512:1024], self.ybs.ap[:, t, :], [self.ybs.buf], [], [db["dbg_y"]])

        def s2a(t):
            st = T_[t]
            mgt = st["mgt"]
            tbi = tb.next()
            pt = self.bank_bf(tbi)
            ptb = self.banks[tbi].buf
            for k in range(8):
                self.tp(pt[:, k, :], mgt.ap[:, k * 128:(k + 1) * 128], self.ident, [mgt.buf, self.cb.buf], [ptb])
            mTt = mT.next()
            self.cp("act", mTt.ap, pt, [ptb], [mTt.buf])
            st["mTt"] = mTt

        def s2b(t):
            st = T_[t]
            i = t0 + t
            mTt, xrt = st["mTt"], st["xrt"]
            ut = u.next()
            for nh in range(2):
                bo = self.banks[bk.next()]
                for k in range(8):
                    self.mm(bo.ap, mTt.ap[:, k, :], wo.ap[:, k, nh * 512:(nh + 1) * 512], k == 0, k == 7, [mTt.buf, wo.buf], [bo.buf])
                self.stt("dve", ut.ap[:, nh * 512:(nh + 1) * 512], xrt.ap[:, nh * 512:(nh + 1) * 512], ALPHA, bo.ap,
                         ALU.mult, ALU.add, [xrt.buf, bo.buf], [ut.buf])
            x1t = x1.next()
            self.layernorm(ut, lg, lb, x1t, st6.next(), mv.next(), rstd.next())
            self.dma("sp", d["x1"][i * 128:(i + 1) * 128, :], x1t.ap, [x1t.buf], [], [db["x1"]])
            x1bt = x1b.next()
            self.cp("pool", x1bt.ap, x1t.ap, [x1t.buf], [x1bt.buf])
            self.dma("sp", d["x1b"][i * 128:(i + 1) * 128, :], x1bt.ap, [x1bt.buf], [], [db["x1b"]])
            st["x1t"] = x1t

        def s3a(t):
            st = T_[t]
            x1t = st["x1t"]
            x1Tt = x1T.next()
            for hf in range(2):
                fb = self.banks[6 + hf]
                for k in range(4):
                    kk = hf * 4 + k
                    self.tp(fb.ap[:, k * 128:(k + 1) * 128], x1t.ap[:, kk * 128:(kk + 1) * 128], self.identf,
                            [x1t.buf, self.cf.buf], [fb.buf])
                self.cp("act", x1Tt.ap[:, hf * 4:hf * 4 + 4, :], fb.ap.rearrange("p (k t) -> p k t", t=128), [fb.buf], [x1Tt.buf])
            st["x1Tt"] = x1Tt

        def s3b(t):
            st = T_.pop(t)
            i = t0 + t
            x1Tt = st["x1Tt"]
            rbk = self.banks[bk.next()]
            for k in range(8):
                self.mm(rbk.ap[:, 0:NE], x1Tt.ap[:, k, :], self.wr.ap[:, k, :], k == 0, k == 7, [x1Tt.buf, self.wr.buf], [rbk.buf])
            self.act(self.aff.ap[:, i, :], rbk.ap[:, 0:NE], AF.Sigmoid, [rbk.buf], [self.aff.buf])

        load(0)
        for step in range(NQB + 2):
            if step + 1 < NQB:
                load(step + 1)
            if step < NQB:
                s1(step)
            if 0 <= step - 1 < NQB:
                s2a(step - 1)
            if 0 <= step - 2 < NQB:
                s3a(step - 2)
            if 0 <= step - 1 < NQB:
                s2b(step - 1)
            if 0 <= step - 2 < NQB:
                s3b(step - 2)

    def phase_route(self, l):
        cv, d, db, NT, NBLK = self.cv, self.d, self.db, self.NT, self.NBLK
        N = NT * NE
        A = lambda shape, dt=F32: cv.alloc(shape, dt)
        self.dlo_i = A([NT], I32)
        self.dhi_i = A([NT], I32)
        self.glo = A([NT])
        self.ghi = A([NT])
        self.widx = A([NBLK], I32)
        cv.mark()
        bsd = A([NT, NE])
        self.tt("dve", bsd.ap, self.aff.ap, self.rb.ap.unsqueeze(1).to_broadcast([128, NT, NE]), ALU.add,
                [self.aff.buf, self.rb.buf], [bsd.buf])
        b4 = bsd.ap.rearrange("p t (g f) -> p (t g) f", f=4)
        NGp = NT * 8
        top2 = A([NGp])
        thr = A([NGp])
        tmp = A([NGp])
        first = True
        for i in range(4):
            for j in range(i + 1, 4):
                if first:
                    self.tt("dve", top2.ap, b4[:, :, i], b4[:, :, j], ALU.add, [bsd.buf], [top2.buf])
                    self.tt("dve", thr.ap, b4[:, :, i], b4[:, :, j], ALU.min, [bsd.buf], [thr.buf])
                    first = False
                else:
                    self.tt("dve", tmp.ap, b4[:, :, i], b4[:, :, j], ALU.add, [bsd.buf], [tmp.buf])
                    self.tt("dve", top2.ap, top2.ap, tmp.ap, ALU.max, [top2.buf, tmp.buf], [top2.buf])
                    self.tt("dve", tmp.ap, b4[:, :, i], b4[:, :, j], ALU.min, [bsd.buf], [tmp.buf])
                    self.tt("dve", thr.ap, thr.ap, tmp.ap, ALU.max, [thr.buf, tmp.buf], [thr.buf])
        gmax = A([NT])
        t2v = top2.ap.rearrange("p (t g) -> p t g", g=8)
        self.red("dve", gmax.ap, t2v, ALU.max, [top2.buf], [gmax.buf])
        gsel = A([NT, 8])
        self.tt("dve", gsel.ap, t2v, gmax.ap.unsqueeze(2).to_broadcast([128, NT, 8]), ALU.is_ge, [top2.buf, gmax.buf], [gsel.buf])
        sel = A([NT, NE])
        s4 = sel.ap.rearrange("p t (g f) -> p (t g) f", f=4)
        self.tt("dve", s4, b4, thr.ap.unsqueeze(2).to_broadcast([128, NGp, 4]), ALU.is_ge, [bsd.buf, thr.buf], [sel.buf])
        self.tt("dve", s4, s4, gsel.ap.rearrange("p t g -> p (t g)").unsqueeze(2).to_broadcast([128, NGp, 4]), ALU.mult,
                [sel.buf, gsel.buf], [sel.buf])
        gd = A([NT, NE])
        self.tt("dve", gd.ap, sel.ap, self.aff.ap, ALU.mult, [sel.buf, self.aff.buf], [gd.buf])
        wsum = A([NT])
        self.red("dve", wsum.ap, gd.ap, ALU.add, [gd.buf], [wsum.buf])
        self.tr.op("dve", lambda E: E.reciprocal(out=wsum.ap, in_=wsum.ap), [wsum.buf], [wsum.buf])
        self.tt("dve", gd.ap, gd.ap, wsum.ap.unsqueeze(2).to_broadcast([128, NT, NE]), ALU.mult, [gd.buf, wsum.buf], [gd.buf])
        selb = A([N], BF16)
        self.cp("dve", selb.ap, sel.ap.rearrange("p t e -> p (t e)"), [sel.buf], [selb.buf])
        cnt = A([NT, NE])
        rank = A([NT, NE])
        cntf = cnt.ap.rearrange("p t e -> p (t e)")
        rankf = rank.ap.rearrange("p t e -> p (t e)")
        for c0 in range(0, N, 512):
            w = min(512, N - c0)
            b0, b1 = self.banks[0], self.banks[1]
            self.mm(b0.ap[:, 0:w], self.ones, selb.ap[:, c0:c0 + w], True, True, [self.cb.buf, selb.buf], [b0.buf])
            self.mm(b1.ap[:, 0:w], self.ustrict, selb.ap[:, c0:c0 + w], True, True, [self.cb.buf, selb.buf], [b1.buf])
            self.cp("dve", cntf[:, c0:c0 + w], b0.ap[:, 0:w], [b0.buf], [cnt.buf])
            self.cp("dve", rankf[:, c0:c0 + w], b1.ap[:, 0:w], [b1.buf], [rank.buf])
        cum = A([NT + 1, NE])
        self.memset("dve", cum.ap[:, 0, :], 0.0, [cum.buf])
        for i in range(NT):
            self.tt("dve", cum.ap[:, i + 1, :], cum.ap[:, i, :], cnt.ap[:, i, :], ALU.add, [cum.buf, cnt.buf], [cum.buf])
        pad = A([NE])
        cmp_ = A([NE, NT])
        self.tt("dve", cmp_.ap, cum.ap[:, NT, :].unsqueeze(2).to_broadcast([128, NE, NT]),
                self.bstart[:, 0:NT].unsqueeze(1).to_broadcast([128, NE, NT]), ALU.is_gt, [cum.buf, self.cf.buf], [cmp_.buf])
        self.red("dve", pad.ap, cmp_.ap, ALU.add, [cmp_.buf], [pad.buf])
        self.ts("dve", pad.ap, pad.ap, 128.0, None, ALU.mult, None, [pad.buf], [pad.buf])
        pend = A([NE + 1])
        self.memset("dve", pend.ap[:, 0:1], 0.0, [pend.buf])
        for e in range(NE):
            self.tt("dve", pend.ap[:, e + 1:e + 2], pend.ap[:, e:e + 1], pad.ap[:, e:e + 1], ALU.add, [pend.buf, pad.buf], [pend.buf])
        dest = A([NT, NE])
        self.tt("dve", dest.ap, cum.ap[:, 0:NT, :], rank.ap, ALU.add, [cum.buf, rank.buf], [dest.buf])
        self.tt("dve", dest.ap, dest.ap, pend.ap[:, 0:NE].unsqueeze(1).to_broadcast([128, NT, NE]), ALU.add,
                [dest.buf, pend.buf], [dest.buf])
        BIG = 1.0e6
        dm = A([NT, NE])
        msk = A([NT, NE])
        self.ts("dve", msk.ap, sel.ap, -1.0, None, ALU.add, None, [sel.buf], [msk.buf])
        self.ts("dve", msk.ap, msk.ap, -BIG, None, ALU.mult, None, [msk.buf], [msk.buf])
        self.tt("dve", dm.ap, dest.ap, msk.ap, ALU.add, [dest.buf, msk.buf], [dm.buf])
        dlo = A([NT])
        dhi = A([NT])
        self.red("dve", dlo.ap, dm.ap, ALU.min, [dm.buf], [dlo.buf])
        self.tt("dve", dm.ap, dest.ap, msk.ap, ALU.subtract, [dest.buf, msk.buf], [dm.buf])
        self.red("dve", dhi.ap, dm.ap, ALU.max, [dm.buf], [dhi.buf])
        glo, ghi = self.glo, self.ghi
        eq = A([NT, NE])
        self.tt("dve", eq.ap, dest.ap, dlo.ap.unsqueeze(2).to_broadcast([128, NT, NE]), ALU.is_equal, [dest.buf, dlo.buf], [eq.buf])
        self.tt("dve", eq.ap, eq.ap, gd.ap, ALU.mult, [eq.buf, gd.buf], [eq.buf])
        self.red("dve", glo.ap, eq.ap, ALU.add, [eq.buf], [glo.buf])
        self.tt("dve", eq.ap, dest.ap, dhi.ap.unsqueeze(2).to_broadcast([128, NT, NE]), ALU.is_equal, [dest.buf, dhi.buf], [eq.buf])
        self.tt("dve", eq.ap, eq.ap, gd.ap, ALU.mult, [eq.buf, gd.buf], [eq.buf])
        self.red("dve", ghi.ap, eq.ap, ALU.add, [eq.buf], [ghi.buf])
        self.cp("dve", self.dlo_i.ap, dlo.ap, [dlo.buf], [self.dlo_i.buf])
        self.cp("dve", self.dhi_i.ap, dhi.ap, [dhi.buf], [self.dhi_i.buf])
        be = A([NBLK])
        self.memset("dve", be.ap, 0.0, [be.buf])
        for e in range(NE):
            self.stt("dve", be.ap, self.bstart, pend.ap[:, e + 1:e + 2], be.ap, ALU.is_ge, ALU.add,
                     [self.cf.buf, pend.buf, be.buf], [be.buf])
        self.ts("dve", be.ap, be.ap, float(NE - 1), None, ALU.min, None, [be.buf], [be.buf])
        self.ts("dve", be.ap, be.ap, 128.0, None, ALU.mult, None, [be.buf], [be.buf])
        self.ts("dve", be.ap, be.ap, self.pidx, None, ALU.add, None, [be.buf, self.cf.buf], [be.buf])
        self.cp("dve", self.widx.ap, be.ap, [be.buf], [self.widx.buf])
        xl = Rot([cv.alloc([D], BF16) for _ in range(3)])
        for i in range(NT):
            xt = xl.next()
            self.dma("sp", xt.ap, d["x1b"][i * 128:(i + 1) * 128, :], [db["x1b"]], [xt.buf])
            self.scatter(d["xs"], self.dlo_i.ap[:, i:i + 1], xt.ap, [xt.buf, self.dlo_i.buf], [db["xs"]])
            self.scatter(d["xs"], self.dhi_i.ap[:, i:i + 1], xt.ap, [xt.buf, self.dhi_i.buf], [db["xs"]])

    def phase_moe_blocks(self, l):
        cv, d, db, NBLK = self.cv, self.d, self.db, self.NBLK
        wgs = Rot([cv.alloc([8, DE], BF16) for _ in range(4)])
        wus = Rot([cv.alloc([8, DE], BF16) for _ in range(4)])
        wds = Rot([cv.alloc([4, D], BF16) for _ in range(4)])
        xsb = Rot([cv.alloc([D], BF16) for _ in range(2)])
        xsT = Rot([cv.alloc([8, 128], BF16) for _ in range(2)])
        sg = Rot([cv.alloc([DE], F32) for _ in range(2)])
        hb = Rot([cv.alloc([DE], BF16) for _ in range(2)])
        hT = Rot([cv.alloc([4, 128], BF16) for _ in range(2)])
        yo = Rot([cv.alloc([D], F32) for _ in range(2)])
        bk = Rot([0, 1, 2, 3, 4, 5])
        tb = Rot([6, 7])
        for blk in range(NBLK):
            wgt, wut, wdt = wgs.next(), wus.next(), wds.next()
            ix = self.widx.ap[:, blk:blk + 1]
            sfx = str(l % 2)
            self.gather(wgt.ap.rearrange("p k n -> p (k n)"), d["wg_d" + sfx], ix, [db["wg_d" + sfx], self.widx.buf], [wgt.buf])
            self.gather(wut.ap.rearrange("p k n -> p (k n)"), d["wu_d" + sfx], ix, [db["wu_d" + sfx], self.widx.buf], [wut.buf])
            self.gather(wdt.ap.rearrange("p k n -> p (k n)"), d["wd_d" + sfx], ix, [db["wd_d" + sfx], self.widx.buf], [wdt.buf])
            xt = xsb.next()
            self.dma("sp", xt.ap, d["xs"][blk * 128:(blk + 1) * 128, :], [db["xs"]], [xt.buf])
            tbi = tb.next()
            pt, ptb = self.bank_bf(tbi), self.banks[tbi].buf
            for k in range(8):
                self.tp(pt[:, k, :], xt.ap[:, k * 128:(k + 1) * 128], self.ident, [xt.buf, self.cb.buf], [ptb])
            xT = xsT.next()
            self.cp("act", xT.ap, pt, [ptb], [xT.buf])
            bg_, bu_ = self.banks[bk.next()], self.banks[bk.next()]
            for k in range(8):
                self.mm(bg_.ap, xT.ap[:, k, :], wgt.ap[:, k, :], k == 0, k == 7, [xT.buf, wgt.buf], [bg_.buf])
            for k in range(8):
                self.mm(bu_.ap, xT.ap[:, k, :], wut.ap[:, k, :], k == 0, k == 7, [xT.buf, wut.buf], [bu_.buf])
            sgt, ht = sg.next(), hb.next()
            self.act(sgt.ap, bg_.ap, AF.Silu, [bg_.buf], [sgt.buf])
            self.tt("dve", ht.ap, sgt.ap, bu_.ap, ALU.mult, [sgt.buf, bu_.buf], [ht.buf])
            tbi = tb.next()
            pt, ptb = self.bank_bf(tbi), self.banks[tbi].buf
            for k in range(4):
                self.tp(pt[:, k, :], ht.ap[:, k * 128:(k + 1) * 128], self.ident, [ht.buf, self.cb.buf], [ptb])
            hTt = hT.next()
            self.cp("act", hTt.ap, pt[:, 0:4, :], [ptb], [hTt.buf])
            yt = yo.next()
            for nh in range(2):
                by = self.banks[bk.next()]
                for k in range(4):
                    self.mm(by.ap, hTt.ap[:, k, :], wdt.ap[:, k, nh * 512:(nh + 1) * 512], k == 0, k == 3, [hTt.buf, wdt.buf], [by.buf])
                self.cp("dve", yt.ap[:, nh * 512:(nh + 1) * 512], by.ap, [by.buf], [yt.buf])
            self.dma("pool", d["yb"][blk * 128:(blk + 1) * 128, :], yt.ap, [yt.buf], [], [db["yb"]])

    def phase_combine(self, l):
        cv, d, db, NT = self.cv, self.d, self.db, self.NT
        last = l == self.L - 1
        lg = cv.alloc([D], F32)
        lb = cv.alloc([D], F32)
        self.dma("sp", lg.ap, d["ln2_g"][l, :].partition_broadcast(128), [db["ln2_g"]], [lg.buf])
        self.dma("sp", lb.ap, d["ln2_b"][l, :].partition_broadcast(128), [db["ln2_b"]], [lb.buf])
        y0 = Rot([cv.alloc([D], F32) for _ in range(2)])
        y1 = Rot([cv.alloc([D], F32) for _ in range(2)])
        x1 = Rot([cv.alloc([D], F32) for _ in range(2)])
        u = Rot([cv.alloc([D], F32) for _ in range(2)])
        xo = Rot([cv.alloc([D], F32) for _ in range(2)])
        xb = Rot([cv.alloc([D], BF16) for _ in range(2)])
        xTt = Rot([cv.alloc([8, 128], BF16) for _ in range(2)])
        st6 = Rot([cv.alloc([2, 6], F32) for _ in range(2)])
        mv = Rot([cv.alloc([2], F32) for _ in range(2)])
        rstd = Rot([cv.alloc([1], F32) for _ in range(2)])
        bk = Rot([6, 7])
        loaded = {}

        def load(i):
            a0, a1, xt = y0.next(), y1.next(), x1.next()
            self.gather(a0.ap, d["yb"], self.dlo_i.ap[:, i:i + 1], [db["yb"], self.dlo_i.buf], [a0.buf])
            self.gather(a1.ap, d["yb"], self.dhi_i.ap[:, i:i + 1], [db["yb"], self.dhi_i.buf], [a1.buf])
            self.dma("sp", xt.ap, d["x1"][i * 128:(i + 1) * 128, :], [db["x1"]], [xt.buf])
            loaded[i] = (a0, a1, xt)

        load(0)
        for i in range(NT):
            if i + 1 < NT:
                load(i + 1)
            a0, a1, xt = loaded.pop(i)
            ut = u.next()
            self.ts("dve", ut.ap, a0.ap, self.glo.ap[:, i:i + 1], None, ALU.mult, None, [a0.buf, self.glo.buf], [ut.buf])
            self.stt("dve", ut.ap, a1.ap, self.ghi.ap[:, i:i + 1], ut.ap, ALU.mult, ALU.add, [a1.buf, self.ghi.buf, ut.buf], [ut.buf])
            self.stt("dve", ut.ap, xt.ap, ALPHA, ut.ap, ALU.mult, ALU.add, [xt.buf, ut.buf], [ut.buf])
            xot = xo.next()
            self.layernorm(ut, lg, lb, xot, st6.next(), mv.next(), rstd.next())
            if last:
                self.dma("sp", d["y"][i * 128:(i + 1) * 128, :], xot.ap, [xot.buf], [], [db["y"]])
            else:
                self.dma("sp", d["xres"][i * 128:(i + 1) * 128, :], xot.ap, [xot.buf], [], [db["xres"]])
                self.emit_xT(xot, i, xb.next(), xTt.next(), bk.next())


def make_consts(NBLK):
    cb = np.zeros((128, 6, 128), np.float32)
    i = np.arange(128)
    cb[:, 0] = np.eye(128)
    cb[:, 1] = -1.0 * (i[:, None] >= i[None, :])
    cb[:, 2] = -1.0
    cb[:, 3] = np.where(i[:, None] >= i[None, :], NEG, 0.0)
    cb[:, 4] = (i[:, None] < i[None, :])
    cb[:, 5] = 1.0
    ncf = 128 + 8 * 256 + 1 + NBLK + 2
    cf = np.zeros((128, ncf), np.float32)
    cf[:, 0:128] = np.eye(128)
    slopes = np.exp2(-8.0 * (np.arange(8, dtype=np.float32) + 1.0) / 8).astype(np.float32)
    j = np.arange(256)
    dist = i[:, None] + 128 - j[None, :]
    valid = (dist >= 0) & (dist < 128)
    sb = np.where(valid[None], -slopes[:, None, None] * dist[None].astype(np.float32), NEG).astype(np.float32)
    cf[:, 128:128 + 2048] = sb.transpose(1, 0, 2).reshape(128, 2048)
    o = 128 + 2048
    cf[:, o] = i
    cf[:, o + 1:o + 1 + NBLK] = 128.0 * np.arange(NBLK)[None, :]
    return cb.reshape(128, 768).astype(NPBF), cf


_CACHE = {}


def get_prog(S, NB, L, debug=False):
    key = (S, NB, L, debug)
    if key not in _CACHE:
        p = Prog(S, NB, L, debug)
        nc = p.build()
        _CACHE[key] = (p, nc)
    return _CACHE[key]


def prep_shared(inp, L):
    f = lambda a: np.ascontiguousarray(np.asarray(a, dtype=np.float32))
    w_in = f(inp["w_in"])[:L]
    b_in = f(inp["b_in"])[:L]
    perm = np.arange(NPROJ)
    perm[:512] = np.concatenate([np.arange(h * 64, (h + 1) * 64) for h in QPERM])
    w_in = np.ascontiguousarray(w_in[:, :, perm])
    b_in = np.ascontiguousarray(b_in[:, perm])
    b_in_fm = np.ascontiguousarray(b_in.reshape(L, 34, 128).transpose(0, 2, 1))
    sh = dict(
        w_in=w_in, b_in=b_in, b_in_fm=b_in_fm, sinks=f(inp["attn_sinks"])[:L],
        w_a=f(inp["w_branch_a"])[:L], w_b=f(inp["w_branch_b"])[:L], w_out=f(inp["w_out"])[:L],
        ln1_g=f(inp["ln1_g"])[:L], ln1_b=f(inp["ln1_b"])[:L], ln2_g=f(inp["ln2_g"])[:L], ln2_b=f(inp["ln2_b"])[:L],
        w_router=f(inp["w_router"]), router_bias=f(inp["router_bias"]).reshape(1, NE),
        w_gate=f(inp["w_gate"])[:L], w_up=f(inp["w_up"])[:L], w_down=f(inp["w_down"])[:L],
    )
    return sh


def run(inp, S, NB, L, n_cores, debug=False):
    p, nc = get_prog(S, NB, L, debug)
    sh = prep_shared(inp, L)
    cb, cf = make_consts(p.NBLK)
    sh["cb"] = cb
    sh["cf"] = cf
    x = np.ascontiguousarray(np.asarray(inp["x"], dtype=np.float32)).reshape(n_cores, NB * S, D)
    in_maps = []
    for c in range(n_cores):
        m = dict(sh)
        m["x"] = x[c]
        in_maps.append(m)
    res = run_bass_kernel_spmd(nc, in_maps, core_ids=list(range(n_cores)))
    return res.results


def kernel(x, w_in, b_in, attn_sinks, w_branch_a, w_branch_b, w_out, ln1_g, ln1_b,
           w_router, router_bias, w_gate, w_up, w_down, ln2_g, ln2_b):
    inp = dict(x=x, w_in=w_in, b_in=b_in, attn_sinks=attn_sinks, w_branch_a=w_branch_a, w_branch_b=w_branch_b,
               w_out=w_out, ln1_g=ln1_g, ln1_b=ln1_b, w_router=w_router, router_bias=router_bias,
               w_gate=w_gate, w_up=w_up, w_down=w_down, ln2_g=ln2_g, ln2_b=ln2_b)
    B, S, _ = np.asarray(x).shape
    n_cores = 8
    NB = B // n_cores
    res = run(inp, S, NB, 4, n_cores)
    out = np.stack([r["y"] for r in res], 0).reshape(B, S, D)
    return out.astype(np.float32)
```

```python
from contextlib import ExitStack

import ml_dtypes
import numpy as np

import concourse.bass as bass
import concourse.mybir as mybir
from concourse.bass_utils import run_bass_kernel_spmd

F32 = mybir.dt.float32
BF16 = mybir.dt.bfloat16
I32 = mybir.dt.int32
U8 = mybir.dt.uint8
ALU = mybir.AluOpType
AF = mybir.ActivationFunctionType
AX = mybir.AxisListType
NPBF = ml_dtypes.bfloat16

D = 1024
NE = 32
DE = 512
NPROJ = 4352
ALPHA = float((2 * 4) ** 0.25)
EPS = 1e-5
NEG = -30000.0
QPERM = [0, 4, 1, 5, 2, 6, 3, 7]
SBUF_BYTES = 206 * 1024
BG_IN_SB = False


class Buf:
    __slots__ = ("w", "r")

    def __init__(self):
        self.w = {}
        self.r = {}


class TT:
    __slots__ = ("ap", "buf")

    def __init__(self, ap, buf=None):
        self.ap = ap
        self.buf = buf if buf is not None else Buf()


class Rot:
    def __init__(self, items):
        self.items = items
        self.i = 0

    def next(self):
        it = self.items[self.i % len(self.items)]
        self.i += 1
        return it


class Tracker:
    COMPUTE = ("pe", "act", "dve", "pool")
    ALL = ("pe", "act", "dve", "pool", "sp")

    def __init__(self, nc, stack, n_dma_sems=8):
        self.nc = nc
        self.stack = stack
        self.ops = {e: [] for e in self.ALL}
        self.sem = {}
        self.cnt = {}
        self.nsem = 0
        self.seen = {e: {} for e in self.ALL}
        for e in self.COMPUTE:
            self._new_sem(e)
        self.dpool = {}
        self.dnext = {}
        for q in ("sp", "act", "pool"):
            self.dpool[q] = [[self._alloc(f"d_{q}_{i}"), 0] for i in range(n_dma_sems)]
            self.dnext[q] = 0
        self.bsem = self._alloc("barrier")
        self.bcount = 0
        self.ninstr = {e: 0 for e in self.ALL}
        self.abs = {e: [] for e in self.ALL}

    def _alloc(self, name):
        self.nsem += 1
        return self.stack.enter_context(self.nc.semaphore(name))

    def _new_sem(self, e):
        self.sem[e] = self._alloc(f"c_{e}_{self.nsem}")
        self.cnt[e] = 0

    def _wait(self, eng, s, v):
        if self.seen[eng].get(s, 0) >= v:
            return
        self.seen[eng][s] = v
        self.ops[eng].append(lambda E, s=s, v=v: E.wait_ge(s, v))
        self.abs[eng].append(("w", id(s), v))
        self.ninstr[eng] += 1

    @staticmethod
    def _deps(reads, writes):
        deps = {}
        for b in reads:
            for s, v in b.w.items():
                if deps.get(s, 0) < v:
                    deps[s] = v
        for b in writes:
            for d in (b.w, b.r):
                for s, v in d.items():
                    if deps.get(s, 0) < v:
                        deps[s] = v
        return deps

    def op(self, eng, fn, reads=(), writes=()):
        deps = self._deps(reads, writes)
        own = self.sem[eng]
        for s, v in deps.items():
            if eng == "pe" and s is own:
                continue
            self._wait(eng, s, v)
        self.cnt[eng] += 1
        v = self.cnt[eng]
        self.ops[eng].append(lambda E, fn=fn, own=own: fn(E).then_inc(own, 1))
        self.abs[eng].append(("i", id(own), 1))
        self.ninstr[eng] += 1
        for b in reads:
            if b.r.get(own, 0) < v:
                b.r[own] = v
        for b in writes:
            b.w = {own: v}
            b.r = {}
        if v >= 60000:
            self._new_sem(eng)

    def dma(self, q, fn, reads=(), writes=(), swrites=()):
        deps = self._deps(reads, writes)
        for b in swrites:
            for s, v in b.r.items():
                if deps.get(s, 0) < v:
                    deps[s] = v
        for s, v in deps.items():
            self._wait(q, s, v)
        slot = self.dpool[q][self.dnext[q]]
        self.dnext[q] = (self.dnext[q] + 1) % len(self.dpool[q])
        s = slot[0]
        if slot[1] > 0:
            self._wait(q, s, slot[1])
        slot[1] += 16
        v = slot[1]
        assert v < 65000
        self.ops[q].append(lambda E, fn=fn, s=s: fn(E).then_inc(s, 16))
        self.abs[q].append(("i", id(s), 16))
        self.ninstr[q] += 1
        for b in reads:
            if b.r.get(s, 0) < v:
                b.r[s] = v
        for b in writes:
            b.w = {s: v}
            b.r = {}
        for b in swrites:
            b.w[s] = v

    def barrier(self):
        self.bcount += len(self.ALL)
        bs, bc = self.bsem, self.bcount
        assert bc < 65000
        for e in self.ALL:
            if e in self.COMPUTE and self.cnt[e] > 0:
                self._wait(e, self.sem[e], self.cnt[e])
            if e in self.dpool:
                for s, v in self.dpool[e]:
                    if v > 0:
                        self._wait(e, s, v)
            self.ops[e].append(lambda E: E.sem_inc(bs, 1))
            self.ops[e].append(lambda E: E.wait_ge(bs, bc))
            self.abs[e].append(("i", id(bs), 1))
            self.abs[e].append(("w", id(bs), bc))
        floor = {}
        for e in self.COMPUTE:
            floor[self.sem[e]] = self.cnt[e]
        for q in self.dpool:
            for s, v in self.dpool[q]:
                floor[s] = v
        for e in self.ALL:
            for s, v in floor.items():
                if self.seen[e].get(s, 0) < v:
                    self.seen[e][s] = v

    def finish(self, block):
        self.barrier()
        ops = self.ops

        @block.tensor
        def _(E):
            for f in ops["pe"]:
                f(E)

        @block.scalar
        def _(E):
            for f in ops["act"]:
                f(E)

        @block.vector
        def _(E):
            for f in ops["dve"]:
                f(E)

        @block.gpsimd
        def _(E):
            for f in ops["pool"]:
                f(E)

        @block.sync
        def _(E):
            for f in ops["sp"]:
                f(E)


class Carver:
    def __init__(self, big, nbytes):
        self.big = big
        self.nbytes = nbytes
        self.off = 0
        self.marks = []
        self.peak = 0

    def mark(self):
        self.marks.append(self.off)

    def release(self):
        self.off = self.marks.pop()

    def alloc(self, shape, dtype, buf=None):
        esz = {F32: 4, BF16: 2, I32: 4}[dtype]
        n = int(np.prod(shape))
        nb = n * esz
        self.off = (self.off + 63) // 64 * 64
        assert self.off + nb <= self.nbytes, f"SBUF carve overflow {self.off}+{nb}>{self.nbytes}"
        ap = self.big[:, self.off:self.off + nb].bitcast(dtype)
        self.off += nb
        self.peak = max(self.peak, self.off)
        if len(shape) == 2:
            ap = ap.rearrange("p (a b) -> p a b", b=shape[1])
        elif len(shape) == 3:
            ap = ap.rearrange("p (a b c) -> p a b c", b=shape[1], c=shape[2])
        return TT(ap, buf)


class Prog:
    def __init__(self, S, NB, L, debug=False):
        self.S, self.NB, self.L, self.debug = S, NB, L, debug
        self.T = S * NB
        self.NT = self.T // 128
        self.NQB = S // 128
        self.NG = S // 512
        self.NBLK = 2 * self.NT + NE
        self.NCF = 128 + 8 * 256 + 1 + self.NBLK + 2

    def mm(self, out, lhsT, rhs, start, stop, reads, writes):
        self.tr.op("pe", lambda E: E.matmul(out, lhsT=lhsT, rhs=rhs, start=start, stop=stop), reads, writes)

    def tp(self, out, in_, ident, reads, writes):
        self.tr.op("pe", lambda E: E.transpose(out=out, in_=in_, identity=ident), reads, writes)

    def act(self, out, in_, func, reads, writes, bias=0.0, scale=1.0, accum=None):
        if accum is None:
            self.tr.op("act", lambda E: E.activation(out=out, in_=in_, func=func, bias=bias, scale=scale), reads, writes)
        else:
            self.tr.op("act", lambda E: E.activation(out=out, in_=in_, func=func, bias=bias, scale=scale,
                                                     accum_out=accum), reads, writes)

    def tt(self, eng, out, in0, in1, op, reads, writes):
        self.tr.op(eng, lambda E: E.tensor_tensor(out=out, in0=in0, in1=in1, op=op), reads, writes)

    def ts(self, eng, out, in0, s1, s2, op0, op1, reads, writes):
        if s2 is None:
            self.tr.op(eng, lambda E: E.tensor_scalar(out=out, in0=in0, scalar1=s1, scalar2=None, op0=op0), reads, writes)
        else:
            self.tr.op(eng, lambda E: E.tensor_scalar(out=out, in0=in0, scalar1=s1, scalar2=s2, op0=op0, op1=op1),
                       reads, writes)

    def stt(self, eng, out, in0, scalar, in1, op0, op1, reads, writes):
        self.tr.op(eng, lambda E: E.scalar_tensor_tensor(out=out, in0=in0, scalar=scalar, in1=in1, op0=op0, op1=op1),
                   reads, writes)

    def cp(self, eng, out, in_, reads, writes):
        if eng == "act":
            self.tr.op("act", lambda E: E.activation(out=out, in_=in_, func=AF.Copy), reads, writes)
        else:
            self.tr.op(eng, lambda E: E.tensor_copy(out=out, in_=in_), reads, writes)

    def red(self, eng, out, in_, op, reads, writes):
        self.tr.op(eng, lambda E: E.tensor_reduce(out=out, in_=in_, axis=AX.X, op=op), reads, writes)

    def memset(self, eng, ap, val, writes):
        self.tr.op(eng, lambda E: E.memset(ap, val), (), writes)

    def dma(self, q, out, in_, reads, writes, swrites=()):
        self.tr.dma(q, lambda E: E.dma_start(out=out, in_=in_), reads, writes, swrites)

    def gather(self, out, src, idx, reads, writes, bound=None):
        if bound is None:
            self.tr.dma("pool", lambda E: E.indirect_dma_start(
                out=out, out_offset=None, in_=src, in_offset=bass.IndirectOffsetOnAxis(ap=idx, axis=0)), reads, writes)
        else:
            self.tr.dma("pool", lambda E: E.indirect_dma_start(
                out=out, out_offset=None, in_=src, in_offset=bass.IndirectOffsetOnAxis(ap=idx, axis=0),
                bounds_check=bound, oob_is_err=False), reads, writes)

    def scatter(self, dst, idx, in_, reads, swrites):
        self.tr.dma("pool", lambda E: E.indirect_dma_start(
            out=dst, out_offset=bass.IndirectOffsetOnAxis(ap=idx, axis=0), in_=in_, in_offset=None), reads, (), swrites)

    def build(self):
        S, NB, L, T, NT, NBLK = self.S, self.NB, self.L, self.T, self.NT, self.NBLK
        nc = bass.Bass("TRN2", target_bir_lowering=False)
        self.nc = nc

        def din(name, shape, dt=F32):
            return nc.dram_tensor(name, list(shape), dt, kind="ExternalInput").ap()

        def dscr(name, shape, dt):
            return nc.dram_tensor(name, list(shape), dt, kind="Internal").ap()

        d = self.d = {}
        d["x"] = din("x", [T, D])
        d["w_in"] = din("w_in", [L, D, NPROJ])
        d["b_in"] = din("b_in", [L, NPROJ])
        d["b_in_fm"] = din("b_in_fm", [L, 128, 34])
        d["sinks"] = din("sinks", [L, 8])
        d["w_a"] = din("w_a", [L, 512, D])
        d["w_b"] = din("w_b", [L, 512, D])
        d["w_out"] = din("w_out", [L, D, D])
        for n in ("ln1_g", "ln1_b", "ln2_g", "ln2_b"):
            d[n] = din(n, [L, D])
        d["w_router"] = din("w_router", [D, NE])
        d["router_bias"] = din("router_bias", [1, NE])
        d["w_gate"] = din("w_gate", [L * NE * 128, 8 * DE])
        d["w_up"] = din("w_up", [L * NE * 128, 8 * DE])
        d["w_down"] = din("w_down", [L * NE * 128, 4 * D])
        d["cb"] = din("cb", [128, 6 * 128], BF16)
        d["cf"] = din("cf", [128, self.NCF])
        d["y"] = nc.dram_tensor("y", [T, D], F32, kind="ExternalOutput").ap()
        if self.debug:
            d["dbg_x1"] = nc.dram_tensor("dbg_x1", [T, D], F32, kind="ExternalOutput").ap()
            d["dbg_y"] = nc.dram_tensor("dbg_y", [T, D], BF16, kind="ExternalOutput").ap()
        d["xres"] = dscr("xres", [T, D], F32)
        d["x1"] = d["dbg_x1"] if self.debug else dscr("x1", [T, D], F32)
        d["x1b"] = dscr("x1b", [T, D], BF16)
        d["xT"] = dscr("xT", [NT * 128, D], BF16)
        d["xs"] = dscr("xs", [NBLK * 128, D], BF16)
        d["yb"] = dscr("yb", [NBLK * 128, D], F32)
        self.db = {k: Buf() for k in d}

        with ExitStack() as st:
            big = st.enter_context(nc.sbuf_tensor("big", [128, SBUF_BYTES], U8))
            self.banks = []
            for i in range(8):
                t = st.enter_context(nc.psum_tensor(f"bank{i}", [128, 512], F32))
                self.banks.append(TT(t[:, :]))
            self.tr = Tracker(nc, st)
            self.cv = Carver(big, SBUF_BYTES)
            blk = st.enter_context(nc.Block())
            self.emit()
            self.tr.finish(blk)
            self.stats = dict(instr=dict(self.tr.ninstr), sems=self.tr.nsem, sbuf_peak=self.cv.peak)
        return nc

    def bank_bf(self, b):
        return self.banks[b].ap.bitcast(BF16).rearrange("p (a b) -> p a b", b=128)

    def emit(self):
        cv, d, db = self.cv, self.d, self.db
        NT, NBLK, L = self.NT, self.NBLK, self.L
        self.cb = cv.alloc([6, 128], BF16)
        self.cf = cv.alloc([self.NCF], F32)
        self.dma("sp", self.cb.ap, d["cb"].rearrange("p (a b) -> p a b", b=128), [], [self.cb.buf])
        self.dma("sp", self.cf.ap, d["cf"], [], [self.cf.buf])
        cbb = self.cb.buf
        self.ident = self.cb.ap[:, 0, :]
        self.wsuf = self.cb.ap[:, 1, :]
        self.negones = self.cb.ap[:, 2, :]
        self.negmask = self.cb.ap[:, 3, :]
        self.ustrict = self.cb.ap[:, 4, :]
        self.ones = self.cb.ap[:, 5, :]
        self.identf = self.cf.ap[:, 0:128]
        self.swab = self.cf.ap[:, 128:128 + 2048].rearrange("p (h k) -> p h k", k=256)
        o = 128 + 2048
        self.pidx = self.cf.ap[:, o:o + 1]
        self.bstart = self.cf.ap[:, o + 1:o + 1 + NBLK]
        self.aff = cv.alloc([NT, NE], F32)
        self.wr = cv.alloc([8, NE], F32)
        self.rb = cv.alloc([NE], F32)
        self.dma("sp", self.wr.ap, d["w_router"].rearrange("(k p) e -> p k e", p=128), [], [self.wr.buf])
        self.dma("sp", self.rb.ap, d["router_bias"][0, :].partition_broadcast(128), [], [self.rb.buf])

        cv.mark()
        self.phase_xT0()
        self.tr.barrier()
        cv.release()
        self.bg = []
        self.bg_slots_left = 0
        for l in range(L):
            for b in range(self.NB):
                cv.mark()
                self.alloc_seq()
                cv.mark()
                self.phase_inproj(l, b)
                self.tr.barrier()
                cv.release()
                cv.mark()
                self.phase_swa(l, b)
                self.tr.barrier()
                cv.release()
                cv.mark()
                self.phase_sb(l, b)
                self.tr.barrier()
                cv.release()
                cv.release()
                cv.mark()
                self.phase_tok(l, b)
                self.tr.barrier()
                cv.release()
                cv.release()
            self.bg_flush()
            cv.mark()
            self.phase_route(l)
            self.tr.barrier()
            cv.release()
            cv.mark()
            self.phase_moe_blocks(l)
            self.tr.barrier()
            cv.release()
            cv.mark()
            self.phase_combine(l)
            self.tr.barrier()
            cv.release()
            cv.release()

    def emit_xT(self, src, i, xb, xTt, bank):
        d, db = self.d, self.db
        self.cp("pool", xb.ap, src.ap, [src.buf], [xb.buf])
        pt = self.bank_bf(bank)
        for k in range(8):
            self.tp(pt[:, k, :], xb.ap[:, k * 128:(k + 1) * 128], self.ident, [xb.buf, self.cb.buf], [self.banks[bank].buf])
        self.cp("act", xTt.ap, pt, [self.banks[bank].buf], [xTt.buf])
        self.dma("sp", d["xT"][i * 128:(i + 1) * 128, :].rearrange("p (k t) -> p k t", t=128), xTt.ap, [xTt.buf], [], [db["xT"]])

    def layernorm(self, u, g_bc, b_bc, out, st6, mv, rstd):
        for c in range(2):
            self.tr.op("dve", lambda E, c=c: E.bn_stats(out=st6.ap[:, c, :], in_=u.ap[:, c * 512:(c + 1) * 512]),
                       [u.buf], [st6.buf])
        self.tr.op("dve", lambda E: E.bn_aggr(out=mv.ap, in_=st6.ap), [st6.buf], [mv.buf])
        self.ts("dve", rstd.ap, mv.ap[:, 1:2], EPS, None, ALU.add, None, [mv.buf], [rstd.buf])
        self.act(rstd.ap, rstd.ap, AF.Sqrt, [rstd.buf], [rstd.buf])
        self.tr.op("dve", lambda E: E.reciprocal(out=rstd.ap, in_=rstd.ap), [rstd.buf], [rstd.buf])
        self.ts("dve", out.ap, u.ap, mv.ap[:, 0:1], rstd.ap[:, 0:1], ALU.subtract, ALU.mult,
                [u.buf, mv.buf, rstd.buf], [out.buf])
        self.tt("pool", out.ap, out.ap, g_bc.ap, ALU.mult, [out.buf, g_bc.buf], [out.buf])
        self.tt("dve", out.ap, out.ap, b_bc.ap, ALU.add, [out.buf, b_bc.buf], [out.buf])

    def phase_xT0(self):
        cv, d, db = self.cv, self.d, self.db
        xin = Rot([cv.alloc([D], F32) for _ in range(2)])
        xb = Rot([cv.alloc([D], BF16) for _ in range(2)])
        xTt = Rot([cv.alloc([8, 128], BF16) for _ in range(2)])
        bk = Rot([6, 7])
        for i in range(self.NT):
            xi = xin.next()
            self.dma("sp", xi.ap, d["x"][i * 128:(i + 1) * 128, :], [db["x"]], [xi.buf])
            self.emit_xT(xi, i, xb.next(), xTt.next(), bk.next())

    def precast_experts(self, l):
        d, db = self.d, self.db
        sfx = str(l % 2)
        for e in range(NE):
            for (dst, src, n) in (("wg_d", "w_gate", DE), ("wu_d", "w_up", DE), ("wd_d", "w_down", D)):
                self.bg.append(lambda dst=dst, src=src, n=n, e=e: self.dma(
                    "pool", d[dst + sfx][e * 128:(e + 1) * 128, :].rearrange("p (k n) -> p k n", n=n),
                    d[src][l, e].rearrange("(k p) n -> p k n", p=128), [db[src]], [], [db[dst + sfx]]))

    def alloc_seq(self):
        cv, S, NQB = self.cv, self.S, self.NQB
        self.ya = cv.alloc([NQB, 512], BF16)
        self.ybs = cv.alloc([NQB, 512], BF16)
        cv.mark()
        self.qTa = cv.alloc([4, S], BF16)
        self.kTa = cv.alloc([S], BF16)
        self.va = cv.alloc([NQB, 128], BF16)
        self.qTs = cv.alloc([4, S], BF16)
        self.kTs = cv.alloc([4, S], BF16)
        self.vs = cv.alloc([NQB, 512], BF16)

    def phase_inproj(self, l, b):
        cv, d, db, S = self.cv, self.d, self.db, self.S
        wq = cv.alloc([8, 2304], BF16)
        for k in range(8):
            self.dma("pool", wq.ap[:, k, :], d["w_in"][l, k * 128:(k + 1) * 128, 0:2304], [db["w_in"]], [wq.buf])
        bfm = cv.alloc([34], F32)
        self.dma("sp", bfm.ap, d["b_in_fm"][l], [db["b_in_fm"]], [bfm.buf])
        bfs = cv.alloc([34], F32)
        self.ts("dve", bfs.ap, bfm.ap, 0.125, None, ALU.mult, None, [bfm.buf], [bfs.buf])
        bva = cv.alloc([128], F32)
        bvs = cv.alloc([512], F32)
        self.dma("sp", bva.ap, d["b_in"][l, 640:768].partition_broadcast(128), [db["b_in"]], [bva.buf])
        self.dma("sp", bvs.ap, d["b_in"][l, 1792:2304].partition_broadcast(128), [db["b_in"]], [bvs.buf])
        xc = Rot([cv.alloc([4, 8, 128], BF16) for _ in range(2)])
        bk = Rot([0, 1, 2, 3, 4, 5])
        fm = []
        for c in range(4):
            fm.append((c * 128, self.qTa, c, 0.125))
        fm.append((512, self.kTa, None, 1.0))
        for c in range(4):
            fm.append((768 + c * 128, self.qTs, c, 0.125))
        for c in range(4):
            fm.append((1280 + c * 128, self.kTs, c, 1.0))
        t0 = b * S // 128
        ev = 0
        for tg in range(S // 512):
            x4 = xc.next()
            r0 = (t0 + tg * 4) * 128
            self.dma("sp", x4.ap, d["xT"][r0:r0 + 512, :].rearrange("(j p) (k t) -> p j k t", p=128, t=128),
                     [db["xT"]], [x4.buf])
            for (col, dst, chunk, scale) in fm:
                bi = bk.next()
                bank = self.banks[bi]
                for k in range(8):
                    self.mm(bank.ap.rearrange("p (j t) -> p j t", t=128), wq.ap[:, k, col:col + 128], x4.ap[:, :, k, :],
                            k == 0, k == 7, [wq.buf, x4.buf], [bank.buf])
                if chunk is None:
                    dap = dst.ap[:, tg * 512:(tg + 1) * 512]
                else:
                    dap = dst.ap[:, chunk, tg * 512:(tg + 1) * 512]
                bias = bfm.ap[:, col // 128:col // 128 + 1]
                if scale != 1.0:
                    self.act(dap, bank.ap, AF.Identity, [bank.buf, bfs.buf], [dst.buf],
                             bias=bfs.ap[:, col // 128:col // 128 + 1], scale=scale)
                else:
                    self.ts("dve", dap, bank.ap, bias, None, ALU.add, None, [bank.buf, bfm.buf], [dst.buf])
            for j in range(4):
                kb = tg * 4 + j
                bi = bk.next()
                bank = self.banks[bi]
                for k in range(8):
                    self.mm(bank.ap[:, 0:128], x4.ap[:, j, k, :], wq.ap[:, k, 640:768], k == 0, k == 7, [wq.buf, x4.buf], [bank.buf])
                self.tt("dve", self.va.ap[:, kb, :], bank.ap[:, 0:128], bva.ap, ALU.add, [bank.buf, bva.buf], [self.va.buf])
                bi = bk.next()
                bank = self.banks[bi]
                for k in range(8):
                    self.mm(bank.ap, x4.ap[:, j, k, :], wq.ap[:, k, 1792:2304], k == 0, k == 7, [wq.buf, x4.buf], [bank.buf])
                self.tt("dve", self.vs.ap[:, kb, :], bank.ap, bvs.ap, ALU.add, [bank.buf, bvs.buf], [self.vs.buf])

    def phase_swa(self, l, b):
        cv, d, db, S, NQB = self.cv, self.d, self.db, self.S, self.NQB
        sink = cv.alloc([8], F32)
        self.dma("sp", sink.ap, d["sinks"][l, :].partition_broadcast(128), [db["sinks"]], [sink.buf])
        sets = []
        for base in (0, 4):
            sets.append(dict(
                ps=[self.banks[base], self.banks[base + 1]], pt=base + 2, po=self.banks[base + 3],
                s=cv.alloc([4, 256], F32), p=cv.alloc([4, 256], BF16), pT=cv.alloc([4, 2, 128], BF16),
                m=cv.alloc([4], F32), negm=cv.alloc([4], F32), rs=cv.alloc([4], F32), es=cv.alloc([4], F32),
                rden=cv.alloc([4], F32)))
        iters = [(qb, g) for qb in range(NQB) for g in range(2)]

        def geom(qb):
            nkb = 1 if qb == 0 else 2
            k0 = qb * 128 if qb == 0 else (qb - 1) * 128
            koff = 128 if qb == 0 else 0
            return nkb, nkb * 128, k0, koff

        def stage_a(it):
            qb, g = iters[it]
            nkb, nk, k0, koff = geom(qb)
            W = sets[it % 2]
            for hh in range(4):
                bank = W["ps"][hh // 2]
                self.mm(bank.ap[:, (hh % 2) * 256:(hh % 2) * 256 + nk],
                        self.qTa.ap[g * 64:(g + 1) * 64, hh, qb * 128:(qb + 1) * 128],
                        self.kTa.ap[g * 64:(g + 1) * 64, k0:k0 + nk], True, True,
                        [self.qTa.buf, self.kTa.buf], [bank.buf])
            s, p = W["s"], W["p"]
            for half in range(2):
                bank = W["ps"][half]
                self.tt("dve", s.ap[:, half * 2:half * 2 + 2, 0:nk],
                        bank.ap.rearrange("p (h k) -> p h k", k=256)[:, :, 0:nk],
                        self.swab[:, g * 4 + half * 2:g * 4 + half * 2 + 2, koff:koff + nk], ALU.add,
                        [bank.buf, self.cf.buf], [s.buf])
            m, negm, rs, es, rden = W["m"], W["negm"], W["rs"], W["es"], W["rden"]
            self.red("dve", m.ap, s.ap[:, :, 0:nk], ALU.max, [s.buf], [m.buf])
            self.tt("dve", m.ap, m.ap, sink.ap[:, g * 4:(g + 1) * 4], ALU.max, [m.buf, sink.buf], [m.buf])
            self.ts("dve", negm.ap, m.ap, -1.0, None, ALU.mult, None, [m.buf], [negm.buf])
            self.memset("pool", rs.ap, 0.0, [rs.buf])
            for hh in range(4):
                self.act(p.ap[:, hh, 0:nk], s.ap[:, hh, 0:nk], AF.Exp, [s.buf, negm.buf, rs.buf], [p.buf, rs.buf],
                         bias=negm.ap[:, hh:hh + 1], accum=rs.ap[:, hh:hh + 1])
            self.tt("dve", es.ap, sink.ap[:, g * 4:(g + 1) * 4], negm.ap, ALU.add, [sink.buf, negm.buf], [es.buf])
            self.act(es.ap, es.ap, AF.Exp, [es.buf], [es.buf])
            self.tt("dve", rden.ap, rs.ap, es.ap, ALU.add, [rs.buf, es.buf], [rden.buf])
            self.tr.op("dve", lambda E, rden=rden: E.reciprocal(out=rden.ap, in_=rden.ap), [rden.buf], [rden.buf])

        def stage_b(it):
            qb, g = iters[it]
            nkb, nk, k0, koff = geom(qb)
            W = sets[it % 2]
            p, pT, rden = W["p"], W["pT"], W["rden"]
            ptv = self.bank_bf(W["pt"]).rearrange("p (h k) t -> p h k t", k=2)
            ptb = self.banks[W["pt"]].buf
            for hh in range(4):
                for kk in range(nkb):
                    self.tp(ptv[:, hh, kk, :], p.ap[:, hh, kk * 128:(kk + 1) * 128], self.ident, [p.buf, self.cb.buf], [ptb])
            self.cp("act", pT.ap[:, :, 0:nkb, :], ptv[:, :, 0:nkb, :], [ptb], [pT.buf])
            po = W["po"]
            for hh in range(4):
                for kk in range(nkb):
                    kb = qb if qb == 0 else qb - 1 + kk
                    self.mm(po.ap[:, hh * 64:(hh + 1) * 64], pT.ap[:, hh, kk, :], self.va.ap[:, kb, g * 64:(g + 1) * 64],
                            kk == 0, kk == nkb - 1, [pT.buf, self.va.buf], [po.buf])
            for hh in range(4):
                h = g * 4 + hh
                self.ts("dve", self.ya.ap[:, qb, h * 64:(h + 1) * 64], po.ap[:, hh * 64:(hh + 1) * 64],
                        rden.ap[:, hh:hh + 1], None, ALU.mult, None, [po.buf, rden.buf], [self.ya.buf])

        stage_a(0)
        for it in range(len(iters)):
            if it + 1 < len(iters):
                stage_a(it + 1)
            stage_b(it)

    def phase_sb(self, l, b):
        cv, S, NG = self.cv, self.S, self.NG
        zb = Rot([0, 1, 2, 3])
        e_r = Rot([cv.alloc([512], F32) for _ in range(3)])
        sp_r = Rot([cv.alloc([512], BF16) for _ in range(3)])
        a_r = Rot([cv.alloc([512], BF16) for _ in range(3)])
        Ssets = [[cv.alloc([512], BF16) for _ in range(2)] for _ in range(2)]
        cbb = self.cb.buf
        pos = [self.banks[4 + j] for j in range(4)]
        steps = []
        hg = 0
        for h in range(8):
            for G in range(NG):
                for kb in range(4 * G + 3, -1, -1):
                    steps.append(dict(h=h, G=G, kb=kb, hg=hg, first=(kb == 4 * G + 3), last=(kb == 0),
                                      step=4 * G + 3 - kb))
                hg += 1
        n_slots = 8 * NG

        def stage_a(st):
            h, G, kb = st["h"], st["G"], st["kb"]
            c, pb = h // 2, (h % 2) * 64
            q0 = G * 512
            qlo = max(kb * 128, q0)
            off = qlo - q0
            zbank = self.banks[zb.next()]
            z = zbank.ap[:, off:512]
            self.mm(z, self.kTs.ap[pb:pb + 64, c, kb * 128:(kb + 1) * 128], self.qTs.ap[pb:pb + 64, c, qlo:q0 + 512],
                    True, False, [self.kTs.buf, self.qTs.buf], [zbank.buf])
            if kb >= 4 * G:
                self.mm(zbank.ap[:, off:off + 128], self.ident, self.negmask, False, False, [cbb], [zbank.buf])
            e, sp = e_r.next(), sp_r.next()
            self.act(e.ap[:, off:512], z, AF.Exp, [zbank.buf], [e.buf])
            self.act(sp.ap[:, off:512], e.ap[:, off:512], AF.Ln, [e.buf], [sp.buf], bias=1.0)
            st.update(zbank=zbank, z=z, off=off, sp=sp)

        def stage_b(st):
            h, G, kb, off, zbank, z, sp = st["h"], st["G"], st["kb"], st["off"], st["zbank"], st["z"], st["sp"]
            Sb = Ssets[st["hg"] % 2]
            if st["first"]:
                self.memset("pool", Sb[0].ap, 0.0, [Sb[0].buf])
                self.memset("pool", Sb[1].ap, 0.0, [Sb[1].buf])
            Scur, Snxt = Sb[st["step"] % 2], Sb[(st["step"] + 1) % 2]
            self.mm(z, self.wsuf, sp.ap[:, off:512], False, False, [cbb, sp.buf], [zbank.buf])
            self.mm(z, self.negones, Scur.ap[:, off:512], False, True, [cbb, Scur.buf], [zbank.buf])
            if kb > 0:
                self.tt("pool", Snxt.ap[:, off:512], Scur.ap[:, off:512], sp.ap[:, off:512], ALU.add,
                        [Scur.buf, sp.buf], [Snxt.buf])
            a = a_r.next()
            self.act(a.ap[:, off:512], z, AF.Exp, [zbank.buf], [a.buf])
            for j in range(off // 128, 4):
                self.mm(pos[j].ap[:, 0:64], a.ap[:, j * 128:(j + 1) * 128], self.vs.ap[:, kb, h * 64:(h + 1) * 64],
                        kb == 4 * G + j, kb == 0, [a.buf, self.vs.buf], [pos[j].buf])
            if st["last"]:
                for j in range(4):
                    self.cp("dve", self.ybs.ap[:, 4 * G + j, h * 64:(h + 1) * 64], pos[j].ap[:, 0:64], [pos[j].buf], [self.ybs.buf])
                self.bg_pump_slot()

        stage_a(steps[0])
        for i, st in enumerate(steps):
            if i + 1 < len(steps):
                stage_a(steps[i + 1])
            stage_b(st)

    def bg_pump_slot(self):
        self.bg_slots_left = max(self.bg_slots_left - 1, 0)
        n = -(-len(self.bg) // (self.bg_slots_left + 1))
        for _ in range(min(n, len(self.bg))):
            self.bg.pop(0)()

    def bg_flush(self):
        while self.bg:
            self.bg.pop(0)()

    def phase_tok(self, l, b):
        cv, d, db, S, NQB = self.cv, self.d, self.db, self.S, self.NQB
        wg = cv.alloc([8, 2048], BF16)
        for k in range(8):
            self.dma("pool", wg.ap[:, k, :], d["w_in"][l, k * 128:(k + 1) * 128, 2304:4352], [db["w_in"]], [wg.buf])
        wab = cv.alloc([8, D], BF16)
        self.dma("pool", wab.ap[:, 0:4, :], d["w_a"][l].rearrange("(k p) n -> p k n", p=128), [db["w_a"]], [wab.buf])
        self.dma("pool", wab.ap[:, 4:8, :], d["w_b"][l].rearrange("(k p) n -> p k n", p=128), [db["w_b"]], [wab.buf])
        wo = cv.alloc([8, D], BF16)
        self.dma("pool", wo.ap, d["w_out"][l].rearrange("(k p) n -> p k n", p=128), [db["w_out"]], [wo.buf])
        bg = cv.alloc([2048], F32)
        self.dma("sp", bg.ap, d["b_in"][l, 2304:4352].partition_broadcast(128), [db["b_in"]], [bg.buf])
        lg = cv.alloc([D], F32)
        lb = cv.alloc([D], F32)
        self.dma("sp", lg.ap, d["ln1_g"][l, :].partition_broadcast(128), [db["ln1_g"]], [lg.buf])
        self.dma("sp", lb.ap, d["ln1_b"][l, :].partition_broadcast(128), [db["ln1_b"]], [lb.buf])
        xTt = Rot([cv.alloc([8, 128], BF16) for _ in range(2)])
        xr = Rot([cv.alloc([D], F32) for _ in range(3)])
        gt = Rot([cv.alloc([2048], F32) for _ in range(1)])
        yT = Rot([cv.alloc([8, 128], BF16) for _ in range(2)])
        t1 = Rot([cv.alloc([512], F32) for _ in range(2)])
        t2 = Rot([cv.alloc([512], F32) for _ in range(2)])
        mg = Rot([cv.alloc([D], BF16) for _ in range(2)])
        mT = Rot([cv.alloc([8, 128], BF16) for _ in range(2)])
        u = Rot([cv.alloc([D], F32) for _ in range(1)])
        x1 = Rot([cv.alloc([D], F32) for _ in range(2)])
        x1b = Rot([cv.alloc([D], BF16) for _ in range(2)])
        x1T = Rot([cv.alloc([8, 128], F32) for _ in range(1)])
        st6 = Rot([cv.alloc([2, 6], F32) for _ in range(2)])
        mv = Rot([cv.alloc([2], F32) for _ in range(2)])
        rstd = Rot([cv.alloc([1], F32) for _ in range(2)])
        bk = Rot([0, 1, 2, 3])
        tb = Rot([4, 5])
        t0 = b * NQB
        src = d["x"] if l == 0 else d["xres"]
        sb_ = db["x"] if l == 0 else db["xres"]
        T_ = {}

        def load(t):
            i = t0 + t
            xt, xrt = xTt.next(), xr.next()
            self.dma("sp", xt.ap, d["xT"][i * 128:(i + 1) * 128, :].rearrange("p (k t) -> p k t", t=128), [db["xT"]], [xt.buf])
            self.dma("sp", xrt.ap, src[i * 128:(i + 1) * 128, :], [sb_], [xrt.buf])
            T_[t] = dict(xt=xt, xrt=xrt)

        def s1(t):
            st = T_[t]
            xt = st["xt"]
            g = gt.next()
            tbi = tb.next()
            pt = self.bank_bf(tbi)
            ptb = self.banks[tbi].buf
            for k in range(4):
                self.tp(pt[:, k, :], self.ya.ap[:, t, k * 128:(k + 1) * 128], self.ident, [self.ya.buf, self.cb.buf], [ptb])
            for k in range(4):
                self.tp(pt[:, 4 + k, :], self.ybs.ap[:, t, k * 128:(k + 1) * 128], self.ident, [self.ybs.buf, self.cb.buf], [ptb])
            yTt = yT.next()
            self.cp("act", yTt.ap, pt, [ptb], [yTt.buf])
            for nh in range(4):
                bank = self.banks[bk.next()]
                for k in range(8):
                    self.mm(bank.ap, xt.ap[:, k, :], wg.ap[:, k, nh * 512:(nh + 1) * 512], k == 0, k == 7, [xt.buf, wg.buf], [bank.buf])
                self.tt("dve", g.ap[:, nh * 512:(nh + 1) * 512], bank.ap, bg.ap[:, nh * 512:(nh + 1) * 512], ALU.add,
                        [bank.buf, bg.buf], [g.buf])
            self.act(g.ap, g.ap, AF.Sigmoid, [g.buf], [g.buf])
            mgt = mg.next()
            for nh in range(2):
                ba = self.banks[bk.next()]
                for k in range(4):
                    self.mm(ba.ap, yTt.ap[:, k, :], wab.ap[:, k, nh * 512:(nh + 1) * 512], k == 0, k == 3, [yTt.buf, wab.buf], [ba.buf])
                bb = self.banks[bk.next()]
                for k in range(4):
                    self.mm(bb.ap, yTt.ap[:, 4 + k, :], wab.ap[:, 4 + k, nh * 512:(nh + 1) * 512], k == 0, k == 3,
                            [yTt.buf, wab.buf], [bb.buf])
                a1, a2 = t1.next(), t2.next()
                self.tt("dve", a1.ap, ba.ap, g.ap[:, nh * 512:(nh + 1) * 512], ALU.mult, [ba.buf, g.buf], [a1.buf])
                self.tt("dve", a2.ap, bb.ap, g.ap[:, 1024 + nh * 512:1024 + (nh + 1) * 512], ALU.mult, [bb.buf, g.buf], [a2.buf])
                self.tt("pool", mgt.ap[:, nh * 512:(nh + 1) * 512], a1.ap, a2.ap, ALU.add, [a1.buf, a2.buf], [mgt.buf])
            st["mgt"] = mgt
            if self.debug:
                i = t0 + t
                self.dma("sp", d["dbg_y"][i * 128:(i + 1) * 128, 0:512], self.ya.ap[:, t, :], [self.ya.buf], [], [db["dbg_y"]])
                self.dma("sp", d["dbg_y"][i * 128:(i + 1) * 128, 512:1024], self.ybs.ap[:, t, :], [self.ybs.buf], [], [db["dbg_y"]])

        def s2a(t):
            st = T_[t]
            mgt = st["mgt"]
            tbi = tb.next()
            pt = self.bank_bf(tbi)
            ptb = self.banks[tbi].buf
            for k in range(8):
                self.tp(pt[:, k, :], mgt.ap[:, k * 128:(k + 1) * 128], self.ident, [mgt.buf, self.cb.buf], [ptb])
            mTt = mT.next()
            self.cp("act", mTt.ap, pt, [ptb], [mTt.buf])
            st["mTt"] = mTt

        def s2b(t):
            st = T_[t]
            i = t0 + t
            mTt, xrt = st["mTt"], st["xrt"]
            ut = u.next()
            for nh in range(2):
                bo = self.banks[bk.next()]
                for k in range(8):
                    self.mm(bo.ap, mTt.ap[:, k, :], wo.ap[:, k, nh * 512:(nh + 1) * 512], k == 0, k == 7, [mTt.buf, wo.buf], [bo.buf])
                self.stt("dve", ut.ap[:, nh * 512:(nh + 1) * 512], xrt.ap[:, nh * 512:(nh + 1) * 512], ALPHA, bo.ap,
                         ALU.mult, ALU.add, [xrt.buf, bo.buf], [ut.buf])
            x1t = x1.next()
            self.layernorm(ut, lg, lb, x1t, st6.next(), mv.next(), rstd.next())
            self.dma("sp", d["x1"][i * 128:(i + 1) * 128, :], x1t.ap, [x1t.buf], [], [db["x1"]])
            x1bt = x1b.next()
            self.cp("pool", x1bt.ap, x1t.ap, [x1t.buf], [x1bt.buf])
            self.dma("sp", d["x1b"][i * 128:(i + 1) * 128, :], x1bt.ap, [x1bt.buf], [], [db["x1b"]])
            st["x1t"] = x1t

        def s3a(t):
            st = T_[t]
            x1t = st["x1t"]
            x1Tt = x1T.next()
            for hf in range(2):
                fb = self.banks[6 + hf]
                for k in range(4):
                    kk = hf * 4 + k
                    self.tp(fb.ap[:, k * 128:(k + 1) * 128], x1t.ap[:, kk * 128:(kk + 1) * 128], self.identf,
                            [x1t.buf, self.cf.buf], [fb.buf])
                self.cp("act", x1Tt.ap[:, hf * 4:hf * 4 + 4, :], fb.ap.rearrange("p (k t) -> p k t", t=128), [fb.buf], [x1Tt.buf])
            st["x1Tt"] = x1Tt

        def s3b(t):
            st = T_.pop(t)
            i = t0 + t
            x1Tt = st["x1Tt"]
            rbk = self.banks[bk.next()]
            for k in range(8):
                self.mm(rbk.ap[:, 0:NE], x1Tt.ap[:, k, :], self.wr.ap[:, k, :], k == 0, k == 7, [x1Tt.buf, self.wr.buf], [rbk.buf])
            self.act(self.aff.ap[:, i, :], rbk.ap[:, 0:NE], AF.Sigmoid, [rbk.buf], [self.aff.buf])

        load(0)
        for step in range(NQB + 2):
            if step + 1 < NQB:
                load(step + 1)
            if step < NQB:
                s1(step)
            if 0 <= step - 1 < NQB:
                s2a(step - 1)
            if 0 <= step - 2 < NQB:
                s3a(step - 2)
            if 0 <= step - 1 < NQB:
                s2b(step - 1)
            if 0 <= step - 2 < NQB:
                s3b(step - 2)

    def phase_route(self, l):
        cv, d, db, NT, NBLK = self.cv, self.d, self.db, self.NT, self.NBLK
        N = NT * NE
        A = lambda shape, dt=F32: cv.alloc(shape, dt)
        self.dlo_i = A([NT], I32)
        self.dhi_i = A([NT], I32)
        self.glo = A([NT])
        self.ghi = A([NT])
        self.widx = A([NBLK], I32)
        cv.mark()
        bsd = A([NT, NE])
        self.tt("dve", bsd.ap, self.aff.ap, self.rb.ap.unsqueeze(1).to_broadcast([128, NT, NE]), ALU.add,
                [self.aff.buf, self.rb.buf], [bsd.buf])
        b4 = bsd.ap.rearrange("p t (g f) -> p (t g) f", f=4)
        NGp = NT * 8
        top2 = A([NGp])
        thr = A([NGp])
        tmp = A([NGp])
        first = True
        for i in range(4):
            for j in range(i + 1, 4):
                if first:
                    self.tt("dve", top2.ap, b4[:, :, i], b4[:, :, j], ALU.add, [bsd.buf], [top2.buf])
                    self.tt("dve", thr.ap, b4[:, :, i], b4[:, :, j], ALU.min, [bsd.buf], [thr.buf])
                    first = False
                else:
                    self.tt("dve", tmp.ap, b4[:, :, i], b4[:, :, j], ALU.add, [bsd.buf], [tmp.buf])
                    self.tt("dve", top2.ap, top2.ap, tmp.ap, ALU.max, [top2.buf, tmp.buf], [top2.buf])
                    self.tt("dve", tmp.ap, b4[:, :, i], b4[:, :, j], ALU.min, [bsd.buf], [tmp.buf])
                    self.tt("dve", thr.ap, thr.ap, tmp.ap, ALU.max, [thr.buf, tmp.buf], [thr.buf])
        gmax = A([NT])
        t2v = top2.ap.rearrange("p (t g) -> p t g", g=8)
        self.red("dve", gmax.ap, t2v, ALU.max, [top2.buf], [gmax.buf])
        gsel = A([NT, 8])
        self.tt("dve", gsel.ap, t2v, gmax.ap.unsqueeze(2).to_broadcast([128, NT, 8]), ALU.is_ge, [top2.buf, gmax.buf], [gsel.buf])
        sel = A([NT, NE])
        s4 = sel.ap.rearrange("p t (g f) -> p (t g) f", f=4)
        self.tt("dve", s4, b4, thr.ap.unsqueeze(2).to_broadcast([128, NGp, 4]), ALU.is_ge, [bsd.buf, thr.buf], [sel.buf])
        self.tt("dve", s4, s4, gsel.ap.rearrange("p t g -> p (t g)").unsqueeze(2).to_broadcast([128, NGp, 4]), ALU.mult,
                [sel.buf, gsel.buf], [sel.buf])
        gd = A([NT, NE])
        self.tt("dve", gd.ap, sel.ap, self.aff.ap, ALU.mult, [sel.buf, self.aff.buf], [gd.buf])
        wsum = A([NT])
        self.red("dve", wsum.ap, gd.ap, ALU.add, [gd.buf], [wsum.buf])
        self.tr.op("dve", lambda E: E.reciprocal(out=wsum.ap, in_=wsum.ap), [wsum.buf], [wsum.buf])
        self.tt("dve", gd.ap, gd.ap, wsum.ap.unsqueeze(2).to_broadcast([128, NT, NE]), ALU.mult, [gd.buf, wsum.buf], [gd.buf])
        selb = A([N], BF16)
        self.cp("dve", selb.ap, sel.ap.rearrange("p t e -> p (t e)"), [sel.buf], [selb.buf])
        cnt = A([NT, NE])
        rank = A([NT, NE])
        cntf = cnt.ap.rearrange("p t e -> p (t e)")
        rankf = rank.ap.rearrange("p t e -> p (t e)")
        for c0 in range(0, N, 512):
            w = min(512, N - c0)
            b0, b1 = self.banks[0], self.banks[1]
            self.mm(b0.ap[:, 0:w], self.ones, selb.ap[:, c0:c0 + w], True, True, [self.cb.buf, selb.buf], [b0.buf])
            self.mm(b1.ap[:, 0:w], self.ustrict, selb.ap[:, c0:c0 + w], True, True, [self.cb.buf, selb.buf], [b1.buf])
            self.cp("dve", cntf[:, c0:c0 + w], b0.ap[:, 0:w], [b0.buf], [cnt.buf])
            self.cp("dve", rankf[:, c0:c0 + w], b1.ap[:, 0:w], [b1.buf], [rank.buf])
        cum = A([NT + 1, NE])
        self.memset("dve", cum.ap[:, 0, :], 0.0, [cum.buf])
        for i in range(NT):
            self.tt("dve", cum.ap[:, i + 1, :], cum.ap[:, i, :], cnt.ap[:, i, :], ALU.add, [cum.buf, cnt.buf], [cum.buf])
        pad = A([NE])
        cmp_ = A([NE, NT])
        self.tt("dve", cmp_.ap, cum.ap[:, NT, :].unsqueeze(2).to_broadcast([128, NE, NT]),
                self.bstart[:, 0:NT].unsqueeze(1).to_broadcast([128, NE, NT]), ALU.is_gt, [cum.buf, self.cf.buf], [cmp_.buf])
        self.red("dve", pad.ap, cmp_.ap, ALU.add, [cmp_.buf], [pad.buf])
        self.ts("dve", pad.ap, pad.ap, 128.0, None, ALU.mult, None, [pad.buf], [pad.buf])
        pend = A([NE + 1])
        self.memset("dve", pend.ap[:, 0:1], 0.0, [pend.buf])
        for e in range(NE):
            self.tt("dve", pend.ap[:, e + 1:e + 2], pend.ap[:, e:e + 1], pad.ap[:, e:e + 1], ALU.add, [pend.buf, pad.buf], [pend.buf])
        dest = A([NT, NE])
        self.tt("dve", dest.ap, cum.ap[:, 0:NT, :], rank.ap, ALU.add, [cum.buf, rank.buf], [dest.buf])
        self.tt("dve", dest.ap, dest.ap, pend.ap[:, 0:NE].unsqueeze(1).to_broadcast([128, NT, NE]), ALU.add,
                [dest.buf, pend.buf], [dest.buf])
        BIG = 1.0e6
        dm = A([NT, NE])
        msk = A([NT, NE])
        self.ts("dve", msk.ap, sel.ap, -1.0, None, ALU.add, None, [sel.buf], [msk.buf])
        self.ts("dve", msk.ap, msk.ap, -BIG, None, ALU.mult, None, [msk.buf], [msk.buf])
        self.tt("dve", dm.ap, dest.ap, msk.ap, ALU.add, [dest.buf, msk.buf], [dm.buf])
        dlo = A([NT])
        dhi = A([NT])
        self.red("dve", dlo.ap, dm.ap, ALU.min, [dm.buf], [dlo.buf])
        self.tt("dve", dm.ap, dest.ap, msk.ap, ALU.subtract, [dest.buf, msk.buf], [dm.buf])
        self.red("dve", dhi.ap, dm.ap, ALU.max, [dm.buf], [dhi.buf])
        glo, ghi = self.glo, self.ghi
        eq = A([NT, NE])
        self.tt("dve", eq.ap, dest.ap, dlo.ap.unsqueeze(2).to_broadcast([128, NT, NE]), ALU.is_equal, [dest.buf, dlo.buf], [eq.buf])
        self.tt("dve", eq.ap, eq.ap, gd.ap, ALU.mult, [eq.buf, gd.buf], [eq.buf])
        self.red("dve", glo.ap, eq.ap, ALU.add, [eq.buf], [glo.buf])
        self.tt("dve", eq.ap, dest.ap, dhi.ap.unsqueeze(2).to_broadcast([128, NT, NE]), ALU.is_equal, [dest.buf, dhi.buf], [eq.buf])
        self.tt("dve", eq.ap, eq.ap, gd.ap, ALU.mult, [eq.buf, gd.buf], [eq.buf])
        self.red("dve", ghi.ap, eq.ap, ALU.add, [eq.buf], [ghi.buf])
        self.cp("dve", self.dlo_i.ap, dlo.ap, [dlo.buf], [self.dlo_i.buf])
        self.cp("dve", self.dhi_i.ap, dhi.ap, [dhi.buf], [self.dhi_i.buf])
        be = A([NBLK])
        self.memset("dve", be.ap, 0.0, [be.buf])
        for e in range(NE):
            self.stt("dve", be.ap, self.bstart, pend.ap[:, e + 1:e + 2], be.ap, ALU.is_ge, ALU.add,
                     [self.cf.buf, pend.buf, be.buf], [be.buf])
        self.ts("dve", be.ap, be.ap, float(NE - 1), None, ALU.min, None, [be.buf], [be.buf])
        self.ts("dve", be.ap, be.ap, 128.0, None, ALU.mult, None, [be.buf], [be.buf])
        self.ts("dve", be.ap, be.ap, self.pidx, None, ALU.add, None, [be.buf, self.cf.buf], [be.buf])
        if l > 0:
            self.ts("dve", be.ap, be.ap, float(l * NE * 128), None, ALU.add, None, [be.buf], [be.buf])
        self.cp("dve", self.widx.ap, be.ap, [be.buf], [self.widx.buf])
        xl = Rot([cv.alloc([D], BF16) for _ in range(3)])
        for i in range(NT):
            xt = xl.next()
            self.dma("sp", xt.ap, d["x1b"][i * 128:(i + 1) * 128, :], [db["x1b"]], [xt.buf])
            self.scatter(d["xs"], self.dlo_i.ap[:, i:i + 1], xt.ap, [xt.buf, self.dlo_i.buf], [db["xs"]])
            self.scatter(d["xs"], self.dhi_i.ap[:, i:i + 1], xt.ap, [xt.buf, self.dhi_i.buf], [db["xs"]])

    def phase_moe_blocks(self, l):
        cv, d, db, NBLK = self.cv, self.d, self.db, self.NBLK
        wgs = Rot([cv.alloc([8, DE], BF16) for _ in range(3)])
        wus = Rot([cv.alloc([8, DE], BF16) for _ in range(3)])
        wds = Rot([cv.alloc([4, D], BF16) for _ in range(3)])
        xsb = Rot([cv.alloc([D], BF16) for _ in range(3)])
        xsT = Rot([cv.alloc([8, 128], BF16) for _ in range(2)])
        sg = Rot([cv.alloc([DE], F32) for _ in range(2)])
        hb = Rot([cv.alloc([DE], BF16) for _ in range(2)])
        hT = Rot([cv.alloc([4, 128], BF16) for _ in range(2)])
        yo = Rot([cv.alloc([D], F32) for _ in range(2)])
        bk = Rot([0, 1, 2, 3, 4, 5])
        tb = Rot([6, 7])
        B_ = {}

        def loads(blk):
            wgt, wut, wdt = wgs.next(), wus.next(), wds.next()
            ix = self.widx.ap[:, blk:blk + 1]
            self.gather(wgt.ap.rearrange("p k n -> p (k n)"), d["w_gate"], ix, [db["w_gate"], self.widx.buf], [wgt.buf])
            self.gather(wut.ap.rearrange("p k n -> p (k n)"), d["w_up"], ix, [db["w_up"], self.widx.buf], [wut.buf])
            self.gather(wdt.ap.rearrange("p k n -> p (k n)"), d["w_down"], ix, [db["w_down"], self.widx.buf], [wdt.buf])
            xt = xsb.next()
            self.dma("sp", xt.ap, d["xs"][blk * 128:(blk + 1) * 128, :], [db["xs"]], [xt.buf])
            B_[blk] = dict(wgt=wgt, wut=wut, wdt=wdt, xt=xt)

        def sA(blk):
            st = B_[blk]
            wgt, wut, xt = st["wgt"], st["wut"], st["xt"]
            tbi = tb.next()
            pt, ptb = self.bank_bf(tbi), self.banks[tbi].buf
            for k in range(8):
                self.tp(pt[:, k, :], xt.ap[:, k * 128:(k + 1) * 128], self.ident, [xt.buf, self.cb.buf], [ptb])
            xT = xsT.next()
            self.cp("act", xT.ap, pt, [ptb], [xT.buf])
            bg_, bu_ = self.banks[bk.next()], self.banks[bk.next()]
            for k in range(8):
                self.mm(bg_.ap, xT.ap[:, k, :], wgt.ap[:, k, :], k == 0, k == 7, [xT.buf, wgt.buf], [bg_.buf])
            for k in range(8):
                self.mm(bu_.ap, xT.ap[:, k, :], wut.ap[:, k, :], k == 0, k == 7, [xT.buf, wut.buf], [bu_.buf])
            sgt, ht = sg.next(), hb.next()
            self.act(sgt.ap, bg_.ap, AF.Silu, [bg_.buf], [sgt.buf])
            self.tt("dve", ht.ap, sgt.ap, bu_.ap, ALU.mult, [sgt.buf, bu_.buf], [ht.buf])
            st["ht"] = ht

        def sB(blk):
            st = B_.pop(blk)
            ht, wdt = st["ht"], st["wdt"]
            tbi = tb.next()
            pt, ptb = self.bank_bf(tbi), self.banks[tbi].buf
            for k in range(4):
                self.tp(pt[:, k, :], ht.ap[:, k * 128:(k + 1) * 128], self.ident, [ht.buf, self.cb.buf], [ptb])
            hTt = hT.next()
            self.cp("act", hTt.ap, pt[:, 0:4, :], [ptb], [hTt.buf])
            yt = yo.next()
            for nh in range(2):
                by = self.banks[bk.next()]
                for k in range(4):
                    self.mm(by.ap, hTt.ap[:, k, :], wdt.ap[:, k, nh * 512:(nh + 1) * 512], k == 0, k == 3, [hTt.buf, wdt.buf], [by.buf])
                self.cp("dve", yt.ap[:, nh * 512:(nh + 1) * 512], by.ap, [by.buf], [yt.buf])
            self.dma("sp", d["yb"][blk * 128:(blk + 1) * 128, :], yt.ap, [yt.buf], [], [db["yb"]])

        loads(0)
        if NBLK > 1:
            loads(1)
        sA(0)
        for blk in range(NBLK):
            if blk + 2 < NBLK:
                loads(blk + 2)
            if blk + 1 < NBLK:
                sA(blk + 1)
            sB(blk)

    def phase_combine(self, l):
        cv, d, db, NT = self.cv, self.d, self.db, self.NT
        last = l == self.L - 1
        lg = cv.alloc([D], F32)
        lb = cv.alloc([D], F32)
        self.dma("sp", lg.ap, d["ln2_g"][l, :].partition_broadcast(128), [db["ln2_g"]], [lg.buf])
        self.dma("sp", lb.ap, d["ln2_b"][l, :].partition_broadcast(128), [db["ln2_b"]], [lb.buf])
        y0 = Rot([cv.alloc([D], F32) for _ in range(2)])
        y1 = Rot([cv.alloc([D], F32) for _ in range(2)])
        x1 = Rot([cv.alloc([D], F32) for _ in range(2)])
        u = Rot([cv.alloc([D], F32) for _ in range(2)])
        xo = Rot([cv.alloc([D], F32) for _ in range(2)])
        xb = Rot([cv.alloc([D], BF16) for _ in range(2)])
        xTt = Rot([cv.alloc([8, 128], BF16) for _ in range(2)])
        st6 = Rot([cv.alloc([2, 6], F32) for _ in range(2)])
        mv = Rot([cv.alloc([2], F32) for _ in range(2)])
        rstd = Rot([cv.alloc([1], F32) for _ in range(2)])
        bk = Rot([6, 7])
        loaded = {}

        def load(i):
            a0, a1, xt = y0.next(), y1.next(), x1.next()
            self.gather(a0.ap, d["yb"], self.dlo_i.ap[:, i:i + 1], [db["yb"], self.dlo_i.buf], [a0.buf])
            self.gather(a1.ap, d["yb"], self.dhi_i.ap[:, i:i + 1], [db["yb"], self.dhi_i.buf], [a1.buf])
            self.dma("sp", xt.ap, d["x1"][i * 128:(i + 1) * 128, :], [db["x1"]], [xt.buf])
            loaded[i] = (a0, a1, xt)

        load(0)
        for i in range(NT):
            if i + 1 < NT:
                load(i + 1)
            a0, a1, xt = loaded.pop(i)
            ut = u.next()
            self.ts("dve", ut.ap, a0.ap, self.glo.ap[:, i:i + 1], None, ALU.mult, None, [a0.buf, self.glo.buf], [ut.buf])
            self.stt("dve", ut.ap, a1.ap, self.ghi.ap[:, i:i + 1], ut.ap, ALU.mult, ALU.add, [a1.buf, self.ghi.buf, ut.buf], [ut.buf])
            self.stt("dve", ut.ap, xt.ap, ALPHA, ut.ap, ALU.mult, ALU.add, [xt.buf, ut.buf], [ut.buf])
            xot = xo.next()
            self.layernorm(ut, lg, lb, xot, st6.next(), mv.next(), rstd.next())
            if last:
                self.dma("sp", d["y"][i * 128:(i + 1) * 128, :], xot.ap, [xot.buf], [], [db["y"]])
            else:
                self.dma("sp", d["xres"][i * 128:(i + 1) * 128, :], xot.ap, [xot.buf], [], [db["xres"]])
                self.emit_xT(xot, i, xb.next(), xTt.next(), bk.next())


def make_consts(NBLK):
    cb = np.zeros((128, 6, 128), np.float32)
    i = np.arange(128)
    cb[:, 0] = np.eye(128)
    cb[:, 1] = -1.0 * (i[:, None] >= i[None, :])
    cb[:, 2] = -1.0
    cb[:, 3] = np.where(i[:, None] >= i[None, :], NEG, 0.0)
    cb[:, 4] = (i[:, None] < i[None, :])
    cb[:, 5] = 1.0
    ncf = 128 + 8 * 256 + 1 + NBLK + 2
    cf = np.zeros((128, ncf), np.float32)
    cf[:, 0:128] = np.eye(128)
    slopes = np.exp2(-8.0 * (np.arange(8, dtype=np.float32) + 1.0) / 8).astype(np.float32)
    j = np.arange(256)
    dist = i[:, None] + 128 - j[None, :]
    valid = (dist >= 0) & (dist < 128)
    sb = np.where(valid[None], -slopes[:, None, None] * dist[None].astype(np.float32), NEG).astype(np.float32)
    cf[:, 128:128 + 2048] = sb.transpose(1, 0, 2).reshape(128, 2048)
    o = 128 + 2048
    cf[:, o] = i
    cf[:, o + 1:o + 1 + NBLK] = 128.0 * np.arange(NBLK)[None, :]
    return cb.reshape(128, 768).astype(NPBF), cf


_CACHE = {}


def get_prog(S, NB, L, debug=False):
    key = (S, NB, L, debug)
    if key not in _CACHE:
        p = Prog(S, NB, L, debug)
        nc = p.build()
        _CACHE[key] = (p, nc)
    return _CACHE[key]


def prep_shared(inp, L):
    f = lambda a: np.ascontiguousarray(np.asarray(a, dtype=np.float32))
    w_in = f(inp["w_in"])[:L]
    b_in = f(inp["b_in"])[:L]
    perm = np.arange(NPROJ)
    perm[:512] = np.concatenate([np.arange(h * 64, (h + 1) * 64) for h in QPERM])
    w_in = np.ascontiguousarray(w_in[:, :, perm])
    b_in = np.ascontiguousarray(b_in[:, perm])
    b_in_fm = np.ascontiguousarray(b_in.reshape(L, 34, 128).transpose(0, 2, 1))
    sh = dict(
        w_in=w_in, b_in=b_in, b_in_fm=b_in_fm, sinks=f(inp["attn_sinks"])[:L],
        w_a=f(inp["w_branch_a"])[:L], w_b=f(inp["w_branch_b"])[:L], w_out=f(inp["w_out"])[:L],
        ln1_g=f(inp["ln1_g"])[:L], ln1_b=f(inp["ln1_b"])[:L], ln2_g=f(inp["ln2_g"])[:L], ln2_b=f(inp["ln2_b"])[:L],
        w_router=f(inp["w_router"]), router_bias=f(inp["router_bias"]).reshape(1, NE),
        w_gate=np.ascontiguousarray(f(inp["w_gate"])[:L].reshape(L, NE, 8, 128, DE).transpose(0, 1, 3, 2, 4)).reshape(L * NE * 128, 8 * DE),
        w_up=np.ascontiguousarray(f(inp["w_up"])[:L].reshape(L, NE, 8, 128, DE).transpose(0, 1, 3, 2, 4)).reshape(L * NE * 128, 8 * DE),
        w_down=np.ascontiguousarray(f(inp["w_down"])[:L].reshape(L, NE, 4, 128, D).transpose(0, 1, 3, 2, 4)).reshape(L * NE * 128, 4 * D),
    )
    return sh


def run(inp, S, NB, L, n_cores, debug=False):
    p, nc = get_prog(S, NB, L, debug)
    sh = prep_shared(inp, L)
    cb, cf = make_consts(p.NBLK)
    sh["cb"] = cb
    sh["cf"] = cf
    x = np.ascontiguousarray(np.asarray(inp["x"], dtype=np.float32)).reshape(n_cores, NB * S, D)
    in_maps = []
    for c in range(n_cores):
        m = dict(sh)
        m["x"] = x[c]
        in_maps.append(m)
    res = run_bass_kernel_spmd(nc, in_maps, core_ids=list(range(n_cores)))
    return res.results


def kernel(x, w_in, b_in, attn_sinks, w_branch_a, w_branch_b, w_out, ln1_g, ln1_b,
           w_router, router_bias, w_gate, w_up, w_down, ln2_g, ln2_b):
    inp = dict(x=x, w_in=w_in, b_in=b_in, attn_sinks=attn_sinks, w_branch_a=w_branch_a, w_branch_b=w_branch_b,
               w_out=w_out, ln1_g=ln1_g, ln1_b=ln1_b, w_router=w_router, router_bias=router_bias,
               w_gate=w_gate, w_up=w_up, w_down=w_down, ln2_g=ln2_g, ln2_b=ln2_b)
    B, S, _ = np.asarray(x).shape
    n_cores = 8
    NB = B // n_cores
    res = run(inp, S, NB, 4, n_cores)
    out = np.stack([r["y"] for r in res], 0).reshape(B, S, D)
    return out.astype(np.float32)
```

```python
from contextlib import ExitStack

import ml_dtypes
import numpy as np

import concourse.bass as bass
import concourse.mybir as mybir
from concourse.bass_utils import run_bass_kernel_spmd

F32 = mybir.dt.float32
BF16 = mybir.dt.bfloat16
I32 = mybir.dt.int32
U8 = mybir.dt.uint8
ALU = mybir.AluOpType
AF = mybir.ActivationFunctionType
AX = mybir.AxisListType
NPBF = ml_dtypes.bfloat16

D = 1024
NE = 32
DE = 512
NPROJ = 4352
ALPHA = float((2 * 4) ** 0.25)
EPS = 1e-5
NEG = -30000.0
QPERM = [0, 4, 1, 5, 2, 6, 3, 7]
SBUF_BYTES = 206 * 1024
BG_IN_SB = False


class Buf:
    __slots__ = ("w", "r")

    def __init__(self):
        self.w = {}
        self.r = {}


class TT:
    __slots__ = ("ap", "buf")

    def __init__(self, ap, buf=None):
        self.ap = ap
        self.buf = buf if buf is not None else Buf()


class Rot:
    def __init__(self, items):
        self.items = items
        self.i = 0

    def next(self):
        it = self.items[self.i % len(self.items)]
        self.i += 1
        return it


class Tracker:
    COMPUTE = ("pe", "act", "dve", "pool")
    ALL = ("pe", "act", "dve", "pool", "sp")

    def __init__(self, nc, stack, n_dma_sems=8):
        self.nc = nc
        self.stack = stack
        self.ops = {e: [] for e in self.ALL}
        self.sem = {}
        self.cnt = {}
        self.nsem = 0
        self.seen = {e: {} for e in self.ALL}
        for e in self.COMPUTE:
            self._new_sem(e)
        self.dpool = {}
        self.dnext = {}
        for q in ("sp", "act", "pool"):
            self.dpool[q] = [[self._alloc(f"d_{q}_{i}"), 0] for i in range(n_dma_sems)]
            self.dnext[q] = 0
        self.bsem = self._alloc("barrier")
        self.bcount = 0
        self.ninstr = {e: 0 for e in self.ALL}
        self.abs = {e: [] for e in self.ALL}

    def _alloc(self, name):
        self.nsem += 1
        return self.stack.enter_context(self.nc.semaphore(name))

    def _new_sem(self, e):
        self.sem[e] = self._alloc(f"c_{e}_{self.nsem}")
        self.cnt[e] = 0

    def _wait(self, eng, s, v):
        if self.seen[eng].get(s, 0) >= v:
            return
        self.seen[eng][s] = v
        self.ops[eng].append(lambda E, s=s, v=v: E.wait_ge(s, v))
        self.abs[eng].append(("w", id(s), v))
        self.ninstr[eng] += 1

    @staticmethod
    def _deps(reads, writes):
        deps = {}
        for b in reads:
            for s, v in b.w.items():
                if deps.get(s, 0) < v:
                    deps[s] = v
        for b in writes:
            for d in (b.w, b.r):
                for s, v in d.items():
                    if deps.get(s, 0) < v:
                        deps[s] = v
        return deps

    def op(self, eng, fn, reads=(), writes=()):
        deps = self._deps(reads, writes)
        own = self.sem[eng]
        for s, v in deps.items():
            if eng == "pe" and s is own:
                continue
            self._wait(eng, s, v)
        self.cnt[eng] += 1
        v = self.cnt[eng]
        self.ops[eng].append(lambda E, fn=fn, own=own: fn(E).then_inc(own, 1))
        self.abs[eng].append(("i", id(own), 1))
        self.ninstr[eng] += 1
        for b in reads:
            if b.r.get(own, 0) < v:
                b.r[own] = v
        for b in writes:
            b.w = {own: v}
            b.r = {}
        if v >= 60000:
            self._new_sem(eng)

    def dma(self, q, fn, reads=(), writes=(), swrites=()):
        deps = self._deps(reads, writes)
        for b in swrites:
            for s, v in b.r.items():
                if deps.get(s, 0) < v:
                    deps[s] = v
        for s, v in deps.items():
            self._wait(q, s, v)
        slot = self.dpool[q][self.dnext[q]]
        self.dnext[q] = (self.dnext[q] + 1) % len(self.dpool[q])
        s = slot[0]
        if slot[1] > 0:
            self._wait(q, s, slot[1])
        slot[1] += 16
        v = slot[1]
        assert v < 65000
        self.ops[q].append(lambda E, fn=fn, s=s: fn(E).then_inc(s, 16))
        self.abs[q].append(("i", id(s), 16))
        self.ninstr[q] += 1
        for b in reads:
            if b.r.get(s, 0) < v:
                b.r[s] = v
        for b in writes:
            b.w = {s: v}
            b.r = {}
        for b in swrites:
            b.w[s] = v

    def barrier(self):
        self.bcount += len(self.ALL)
        bs, bc = self.bsem, self.bcount
        assert bc < 65000
        for e in self.ALL:
            if e in self.COMPUTE and self.cnt[e] > 0:
                self._wait(e, self.sem[e], self.cnt[e])
            if e in self.dpool:
                for s, v in self.dpool[e]:
                    if v > 0:
                        self._wait(e, s, v)
            self.ops[e].append(lambda E: E.sem_inc(bs, 1))
            self.ops[e].append(lambda E: E.wait_ge(bs, bc))
            self.abs[e].append(("i", id(bs), 1))
            self.abs[e].append(("w", id(bs), bc))
        floor = {}
        for e in self.COMPUTE:
            floor[self.sem[e]] = self.cnt[e]
        for q in self.dpool:
            for s, v in self.dpool[q]:
                floor[s] = v
        for e in self.ALL:
            for s, v in floor.items():
                if self.seen[e].get(s, 0) < v:
                    self.seen[e][s] = v

    def finish(self, block):
        self.barrier()
        ops = self.ops

        @block.tensor
        def _(E):
            for f in ops["pe"]:
                f(E)

        @block.scalar
        def _(E):
            for f in ops["act"]:
                f(E)

        @block.vector
        def _(E):
            for f in ops["dve"]:
                f(E)

        @block.gpsimd
        def _(E):
            for f in ops["pool"]:
                f(E)

        @block.sync
        def _(E):
            for f in ops["sp"]:
                f(E)


class Carver:
    def __init__(self, big, nbytes):
        self.big = big
        self.nbytes = nbytes
        self.off = 0
        self.marks = []
        self.peak = 0

    def mark(self):
        self.marks.append(self.off)

    def release(self):
        self.off = self.marks.pop()

    def alloc(self, shape, dtype, buf=None):
        esz = {F32: 4, BF16: 2, I32: 4}[dtype]
        n = int(np.prod(shape))
        nb = n * esz
        self.off = (self.off + 63) // 64 * 64
        assert self.off + nb <= self.nbytes, f"SBUF carve overflow {self.off}+{nb}>{self.nbytes}"
        ap = self.big[:, self.off:self.off + nb].bitcast(dtype)
        self.off += nb
        self.peak = max(self.peak, self.off)
        if len(shape) == 2:
            ap = ap.rearrange("p (a b) -> p a b", b=shape[1])
        elif len(shape) == 3:
            ap = ap.rearrange("p (a b c) -> p a b c", b=shape[1], c=shape[2])
        return TT(ap, buf)


class Prog:
    def __init__(self, S, NB, L, debug=False):
        self.S, self.NB, self.L, self.debug = S, NB, L, debug
        self.T = S * NB
        self.NT = self.T // 128
        self.NQB = S // 128
        self.NG = S // 512
        self.NBLK = 2 * self.NT + 2 * NE
        self.NB2 = self.NT + NE
        self.NCF = 128 + 8 * 256 + 1 + self.NBLK + 2

    def mm(self, out, lhsT, rhs, start, stop, reads, writes):
        self.tr.op("pe", lambda E: E.matmul(out, lhsT=lhsT, rhs=rhs, start=start, stop=stop), reads, writes)

    def tp(self, out, in_, ident, reads, writes):
        self.tr.op("pe", lambda E: E.transpose(out=out, in_=in_, identity=ident), reads, writes)

    def act(self, out, in_, func, reads, writes, bias=0.0, scale=1.0, accum=None):
        if accum is None:
            self.tr.op("act", lambda E: E.activation(out=out, in_=in_, func=func, bias=bias, scale=scale), reads, writes)
        else:
            self.tr.op("act", lambda E: E.activation(out=out, in_=in_, func=func, bias=bias, scale=scale,
                                                     accum_out=accum), reads, writes)

    def tt(self, eng, out, in0, in1, op, reads, writes):
        self.tr.op(eng, lambda E: E.tensor_tensor(out=out, in0=in0, in1=in1, op=op), reads, writes)

    def ts(self, eng, out, in0, s1, s2, op0, op1, reads, writes):
        if s2 is None:
            self.tr.op(eng, lambda E: E.tensor_scalar(out=out, in0=in0, scalar1=s1, scalar2=None, op0=op0), reads, writes)
        else:
            self.tr.op(eng, lambda E: E.tensor_scalar(out=out, in0=in0, scalar1=s1, scalar2=s2, op0=op0, op1=op1),
                       reads, writes)

    def stt(self, eng, out, in0, scalar, in1, op0, op1, reads, writes):
        self.tr.op(eng, lambda E: E.scalar_tensor_tensor(out=out, in0=in0, scalar=scalar, in1=in1, op0=op0, op1=op1),
                   reads, writes)

    def cp(self, eng, out, in_, reads, writes):
        if eng == "act":
            self.tr.op("act", lambda E: E.activation(out=out, in_=in_, func=AF.Copy), reads, writes)
        else:
            self.tr.op(eng, lambda E: E.tensor_copy(out=out, in_=in_), reads, writes)

    def red(self, eng, out, in_, op, reads, writes):
        self.tr.op(eng, lambda E: E.tensor_reduce(out=out, in_=in_, axis=AX.X, op=op), reads, writes)

    def memset(self, eng, ap, val, writes):
        self.tr.op(eng, lambda E: E.memset(ap, val), (), writes)

    def dma(self, q, out, in_, reads, writes, swrites=()):
        self.tr.dma(q, lambda E: E.dma_start(out=out, in_=in_), reads, writes, swrites)

    def gather(self, out, src, idx, reads, writes, bound=None):
        if bound is None:
            self.tr.dma("pool", lambda E: E.indirect_dma_start(
                out=out, out_offset=None, in_=src, in_offset=bass.IndirectOffsetOnAxis(ap=idx, axis=0)), reads, writes)
        else:
            self.tr.dma("pool", lambda E: E.indirect_dma_start(
                out=out, out_offset=None, in_=src, in_offset=bass.IndirectOffsetOnAxis(ap=idx, axis=0),
                bounds_check=bound, oob_is_err=False), reads, writes)

    def scatter(self, dst, idx, in_, reads, swrites):
        self.tr.dma("pool", lambda E: E.indirect_dma_start(
            out=dst, out_offset=bass.IndirectOffsetOnAxis(ap=idx, axis=0), in_=in_, in_offset=None), reads, (), swrites)

    def build(self):
        S, NB, L, T, NT, NBLK = self.S, self.NB, self.L, self.T, self.NT, self.NBLK
        nc = bass.Bass("TRN2", target_bir_lowering=False)
        self.nc = nc

        def din(name, shape, dt=F32):
            return nc.dram_tensor(name, list(shape), dt, kind="ExternalInput").ap()

        def dscr(name, shape, dt):
            return nc.dram_tensor(name, list(shape), dt, kind="Internal").ap()

        d = self.d = {}
        d["x"] = din("x", [T, D])
        d["w_in"] = din("w_in", [L, D, NPROJ])
        d["b_in"] = din("b_in", [L, NPROJ])
        d["b_in_fm"] = din("b_in_fm", [L, 128, 34])
        d["sinks"] = din("sinks", [L, 8])
        d["w_a"] = din("w_a", [L, 512, D])
        d["w_b"] = din("w_b", [L, 512, D])
        d["w_out"] = din("w_out", [L, D, D])
        for n in ("ln1_g", "ln1_b", "ln2_g", "ln2_b"):
            d[n] = din(n, [L, D])
        d["w_router"] = din("w_router", [D, NE])
        d["router_bias"] = din("router_bias", [1, NE])
        d["w_gate"] = din("w_gate", [L * NE * 128, 8 * DE])
        d["w_up"] = din("w_up", [L * NE * 128, 8 * DE])
        d["w_down"] = din("w_down", [L * NE * 128, 4 * D])
        d["cb"] = din("cb", [128, 6 * 128], BF16)
        d["cf"] = din("cf", [128, self.NCF])
        d["y"] = nc.dram_tensor("y", [T, D], F32, kind="ExternalOutput").ap()
        if self.debug:
            d["dbg_x1"] = nc.dram_tensor("dbg_x1", [T, D], F32, kind="ExternalOutput").ap()
            d["dbg_y"] = nc.dram_tensor("dbg_y", [T, D], BF16, kind="ExternalOutput").ap()
        d["xres"] = dscr("xres", [T, D], F32)
        d["x1"] = d["dbg_x1"] if self.debug else dscr("x1", [T, D], F32)
        d["x1b"] = dscr("x1b", [T, D], BF16)
        d["xT"] = dscr("xT", [NT * 128, D], BF16)
        d["xs"] = dscr("xs", [self.NB2 * 256, D], BF16)
        d["yb"] = dscr("yb", [self.NB2 * 256, D], F32)
        self.db = {k: Buf() for k in d}

        with ExitStack() as st:
            big = st.enter_context(nc.sbuf_tensor("big", [128, SBUF_BYTES], U8))
            self.banks = []
            for i in range(8):
                t = st.enter_context(nc.psum_tensor(f"bank{i}", [128, 512], F32))
                self.banks.append(TT(t[:, :]))
            self.tr = Tracker(nc, st)
            self.cv = Carver(big, SBUF_BYTES)
            blk = st.enter_context(nc.Block())
            self.emit()
            self.tr.finish(blk)
            self.stats = dict(instr=dict(self.tr.ninstr), sems=self.tr.nsem, sbuf_peak=self.cv.peak)
        return nc

    def bank_bf(self, b):
        return self.banks[b].ap.bitcast(BF16).rearrange("p (a b) -> p a b", b=128)

    def emit(self):
        cv, d, db = self.cv, self.d, self.db
        NT, NBLK, L = self.NT, self.NBLK, self.L
        self.cb = cv.alloc([6, 128], BF16)
        self.cf = cv.alloc([self.NCF], F32)
        self.dma("sp", self.cb.ap, d["cb"].rearrange("p (a b) -> p a b", b=128), [], [self.cb.buf])
        self.dma("sp", self.cf.ap, d["cf"], [], [self.cf.buf])
        cbb = self.cb.buf
        self.ident = self.cb.ap[:, 0, :]
        self.wsuf = self.cb.ap[:, 1, :]
        self.negones = self.cb.ap[:, 2, :]
        self.negmask = self.cb.ap[:, 3, :]
        self.ustrict = self.cb.ap[:, 4, :]
        self.ones = self.cb.ap[:, 5, :]
        self.identf = self.cf.ap[:, 0:128]
        self.swab = self.cf.ap[:, 128:128 + 2048].rearrange("p (h k) -> p h k", k=256)
        o = 128 + 2048
        self.pidx = self.cf.ap[:, o:o + 1]
        self.bstart = self.cf.ap[:, o + 1:o + 1 + NBLK]
        self.aff = cv.alloc([NT, NE], F32)
        self.wr = cv.alloc([8, NE], F32)
        self.rb = cv.alloc([NE], F32)
        self.dma("sp", self.wr.ap, d["w_router"].rearrange("(k p) e -> p k e", p=128), [], [self.wr.buf])
        self.dma("sp", self.rb.ap, d["router_bias"][0, :].partition_broadcast(128), [], [self.rb.buf])

        cv.mark()
        self.phase_xT0()
        self.tr.barrier()
        cv.release()
        self.bg = []
        self.bg_slots_left = 0
        for l in range(L):
            for b in range(self.NB):
                cv.mark()
                self.alloc_seq()
                cv.mark()
                self.phase_inproj(l, b)
                self.tr.barrier()
                cv.release()
                cv.mark()
                self.phase_swa(l, b)
                self.tr.barrier()
                cv.release()
                cv.mark()
                self.phase_sb(l, b)
                self.tr.barrier()
                cv.release()
                cv.release()
                cv.mark()
                self.phase_tok(l, b)
                self.tr.barrier()
                cv.release()
                cv.release()
            self.bg_flush()
            cv.mark()
            self.phase_route(l)
            self.tr.barrier()
            cv.release()
            cv.mark()
            self.phase_moe_blocks(l)
            self.tr.barrier()
            cv.release()
            cv.mark()
            self.phase_combine(l)
            self.tr.barrier()
            cv.release()
            cv.release()

    def emit_xT(self, src, i, xb, xTt, bank):
        d, db = self.d, self.db
        self.cp("pool", xb.ap, src.ap, [src.buf], [xb.buf])
        pt = self.bank_bf(bank)
        for k in range(8):
            self.tp(pt[:, k, :], xb.ap[:, k * 128:(k + 1) * 128], self.ident, [xb.buf, self.cb.buf], [self.banks[bank].buf])
        self.cp("act", xTt.ap, pt, [self.banks[bank].buf], [xTt.buf])
        self.dma("sp", d["xT"][i * 128:(i + 1) * 128, :].rearrange("p (k t) -> p k t", t=128), xTt.ap, [xTt.buf], [], [db["xT"]])

    def layernorm(self, u, g_bc, b_bc, out, st6, mv, rstd):
        for c in range(2):
            self.tr.op("dve", lambda E, c=c: E.bn_stats(out=st6.ap[:, c, :], in_=u.ap[:, c * 512:(c + 1) * 512]),
                       [u.buf], [st6.buf])
        self.tr.op("dve", lambda E: E.bn_aggr(out=mv.ap, in_=st6.ap), [st6.buf], [mv.buf])
        self.ts("dve", rstd.ap, mv.ap[:, 1:2], EPS, None, ALU.add, None, [mv.buf], [rstd.buf])
        self.act(rstd.ap, rstd.ap, AF.Sqrt, [rstd.buf], [rstd.buf])
        self.tr.op("dve", lambda E: E.reciprocal(out=rstd.ap, in_=rstd.ap), [rstd.buf], [rstd.buf])
        self.ts("dve", out.ap, u.ap, mv.ap[:, 0:1], rstd.ap[:, 0:1], ALU.subtract, ALU.mult,
                [u.buf, mv.buf, rstd.buf], [out.buf])
        self.tt("pool", out.ap, out.ap, g_bc.ap, ALU.mult, [out.buf, g_bc.buf], [out.buf])
        self.tt("dve", out.ap, out.ap, b_bc.ap, ALU.add, [out.buf, b_bc.buf], [out.buf])

    def phase_xT0(self):
        cv, d, db = self.cv, self.d, self.db
        xin = Rot([cv.alloc([D], F32) for _ in range(2)])
        xb = Rot([cv.alloc([D], BF16) for _ in range(2)])
        xTt = Rot([cv.alloc([8, 128], BF16) for _ in range(2)])
        bk = Rot([6, 7])
        for i in range(self.NT):
            xi = xin.next()
            self.dma("sp", xi.ap, d["x"][i * 128:(i + 1) * 128, :], [db["x"]], [xi.buf])
            self.emit_xT(xi, i, xb.next(), xTt.next(), bk.next())

    def precast_experts(self, l):
        d, db = self.d, self.db
        sfx = str(l % 2)
        for e in range(NE):
            for (dst, src, n) in (("wg_d", "w_gate", DE), ("wu_d", "w_up", DE), ("wd_d", "w_down", D)):
                self.bg.append(lambda dst=dst, src=src, n=n, e=e: self.dma(
                    "pool", d[dst + sfx][e * 128:(e + 1) * 128, :].rearrange("p (k n) -> p k n", n=n),
                    d[src][l, e].rearrange("(k p) n -> p k n", p=128), [db[src]], [], [db[dst + sfx]]))

    def alloc_seq(self):
        cv, S, NQB = self.cv, self.S, self.NQB
        self.ya = cv.alloc([NQB, 512], BF16)
        self.ybs = cv.alloc([NQB, 512], BF16)
        cv.mark()
        self.qTa = cv.alloc([4, S], BF16)
        self.kTa = cv.alloc([S], BF16)
        self.va = cv.alloc([NQB, 128], BF16)
        self.qTs = cv.alloc([4, S], BF16)
        self.kTs = cv.alloc([4, S], BF16)
        self.vs = cv.alloc([NQB, 512], BF16)

    def phase_inproj(self, l, b):
        cv, d, db, S = self.cv, self.d, self.db, self.S
        wq = cv.alloc([8, 2304], BF16)
        for k in range(8):
            self.dma("pool", wq.ap[:, k, :], d["w_in"][l, k * 128:(k + 1) * 128, 0:2304], [db["w_in"]], [wq.buf])
        bfm = cv.alloc([34], F32)
        self.dma("sp", bfm.ap, d["b_in_fm"][l], [db["b_in_fm"]], [bfm.buf])
        bfs = cv.alloc([34], F32)
        self.ts("dve", bfs.ap, bfm.ap, 0.125, None, ALU.mult, None, [bfm.buf], [bfs.buf])
        bva = cv.alloc([128], F32)
        bvs = cv.alloc([512], F32)
        self.dma("sp", bva.ap, d["b_in"][l, 640:768].partition_broadcast(128), [db["b_in"]], [bva.buf])
        self.dma("sp", bvs.ap, d["b_in"][l, 1792:2304].partition_broadcast(128), [db["b_in"]], [bvs.buf])
        xc = Rot([cv.alloc([4, 8, 128], BF16) for _ in range(2)])
        bk = Rot([0, 1, 2, 3, 4, 5])
        fm = []
        for c in range(4):
            fm.append((c * 128, self.qTa, c, 0.125))
        fm.append((512, self.kTa, None, 1.0))
        for c in range(4):
            fm.append((768 + c * 128, self.qTs, c, 0.125))
        for c in range(4):
            fm.append((1280 + c * 128, self.kTs, c, 1.0))
        t0 = b * S // 128
        ev = 0
        for tg in range(S // 512):
            x4 = xc.next()
            r0 = (t0 + tg * 4) * 128
            self.dma("sp", x4.ap, d["xT"][r0:r0 + 512, :].rearrange("(j p) (k t) -> p j k t", p=128, t=128),
                     [db["xT"]], [x4.buf])
            for (col, dst, chunk, scale) in fm:
                bi = bk.next()
                bank = self.banks[bi]
                for k in range(8):
                    self.mm(bank.ap.rearrange("p (j t) -> p j t", t=128), wq.ap[:, k, col:col + 128], x4.ap[:, :, k, :],
                            k == 0, k == 7, [wq.buf, x4.buf], [bank.buf])
                if chunk is None:
                    dap = dst.ap[:, tg * 512:(tg + 1) * 512]
                else:
                    dap = dst.ap[:, chunk, tg * 512:(tg + 1) * 512]
                bias = bfm.ap[:, col // 128:col // 128 + 1]
                if scale != 1.0:
                    self.act(dap, bank.ap, AF.Identity, [bank.buf, bfs.buf], [dst.buf],
                             bias=bfs.ap[:, col // 128:col // 128 + 1], scale=scale)
                else:
                    self.ts("dve", dap, bank.ap, bias, None, ALU.add, None, [bank.buf, bfm.buf], [dst.buf])
            for j in range(4):
                kb = tg * 4 + j
                bi = bk.next()
                bank = self.banks[bi]
                for k in range(8):
                    self.mm(bank.ap[:, 0:128], x4.ap[:, j, k, :], wq.ap[:, k, 640:768], k == 0, k == 7, [wq.buf, x4.buf], [bank.buf])
                self.tt("dve", self.va.ap[:, kb, :], bank.ap[:, 0:128], bva.ap, ALU.add, [bank.buf, bva.buf], [self.va.buf])
                bi = bk.next()
                bank = self.banks[bi]
                for k in range(8):
                    self.mm(bank.ap, x4.ap[:, j, k, :], wq.ap[:, k, 1792:2304], k == 0, k == 7, [wq.buf, x4.buf], [bank.buf])
                self.tt("dve", self.vs.ap[:, kb, :], bank.ap, bvs.ap, ALU.add, [bank.buf, bvs.buf], [self.vs.buf])

    def phase_swa(self, l, b):
        cv, d, db, S, NQB = self.cv, self.d, self.db, self.S, self.NQB
        sink = cv.alloc([8], F32)
        self.dma("sp", sink.ap, d["sinks"][l, :].partition_broadcast(128), [db["sinks"]], [sink.buf])
        sets = []
        for base in (0, 4):
            sets.append(dict(
                ps=[self.banks[base], self.banks[base + 1]], pt=base + 2, po=self.banks[base + 3],
                s=cv.alloc([4, 256], F32), p=cv.alloc([4, 256], BF16), pT=cv.alloc([4, 2, 128], BF16),
                m=cv.alloc([4], F32), negm=cv.alloc([4], F32), rs=cv.alloc([4], F32), es=cv.alloc([4], F32),
                rden=cv.alloc([4], F32)))
        iters = [(qb, g) for qb in range(NQB) for g in range(2)]

        def geom(qb):
            nkb = 1 if qb == 0 else 2
            k0 = qb * 128 if qb == 0 else (qb - 1) * 128
            koff = 128 if qb == 0 else 0
            return nkb, nkb * 128, k0, koff

        def stage_a(it):
            qb, g = iters[it]
            nkb, nk, k0, koff = geom(qb)
            W = sets[it % 2]
            for hh in range(4):
                bank = W["ps"][hh // 2]
                self.mm(bank.ap[:, (hh % 2) * 256:(hh % 2) * 256 + nk],
                        self.qTa.ap[g * 64:(g + 1) * 64, hh, qb * 128:(qb + 1) * 128],
                        self.kTa.ap[g * 64:(g + 1) * 64, k0:k0 + nk], True, True,
                        [self.qTa.buf, self.kTa.buf], [bank.buf])
            s, p = W["s"], W["p"]
            for half in range(2):
                bank = W["ps"][half]
                self.tt("dve", s.ap[:, half * 2:half * 2 + 2, 0:nk],
                        bank.ap.rearrange("p (h k) -> p h k", k=256)[:, :, 0:nk],
                        self.swab[:, g * 4 + half * 2:g * 4 + half * 2 + 2, koff:koff + nk], ALU.add,
                        [bank.buf, self.cf.buf], [s.buf])
            m, negm, rs, es, rden = W["m"], W["negm"], W["rs"], W["es"], W["rden"]
            self.red("dve", m.ap, s.ap[:, :, 0:nk], ALU.max, [s.buf], [m.buf])
            self.tt("dve", m.ap, m.ap, sink.ap[:, g * 4:(g + 1) * 4], ALU.max, [m.buf, sink.buf], [m.buf])
            self.ts("dve", negm.ap, m.ap, -1.0, None, ALU.mult, None, [m.buf], [negm.buf])
            self.memset("pool", rs.ap, 0.0, [rs.buf])
            for hh in range(4):
                self.act(p.ap[:, hh, 0:nk], s.ap[:, hh, 0:nk], AF.Exp, [s.buf, negm.buf, rs.buf], [p.buf, rs.buf],
                         bias=negm.ap[:, hh:hh + 1], accum=rs.ap[:, hh:hh + 1])
            self.tt("dve", es.ap, sink.ap[:, g * 4:(g + 1) * 4], negm.ap, ALU.add, [sink.buf, negm.buf], [es.buf])
            self.act(es.ap, es.ap, AF.Exp, [es.buf], [es.buf])
            self.tt("dve", rden.ap, rs.ap, es.ap, ALU.add, [rs.buf, es.buf], [rden.buf])
            self.tr.op("dve", lambda E, rden=rden: E.reciprocal(out=rden.ap, in_=rden.ap), [rden.buf], [rden.buf])

        def stage_b(it):
            qb, g = iters[it]
            nkb, nk, k0, koff = geom(qb)
            W = sets[it % 2]
            p, pT, rden = W["p"], W["pT"], W["rden"]
            ptv = self.bank_bf(W["pt"]).rearrange("p (h k) t -> p h k t", k=2)
            ptb = self.banks[W["pt"]].buf
            for hh in range(4):
                for kk in range(nkb):
                    self.tp(ptv[:, hh, kk, :], p.ap[:, hh, kk * 128:(kk + 1) * 128], self.ident, [p.buf, self.cb.buf], [ptb])
            self.cp("act", pT.ap[:, :, 0:nkb, :], ptv[:, :, 0:nkb, :], [ptb], [pT.buf])
            po = W["po"]
            for hh in range(4):
                for kk in range(nkb):
                    kb = qb if qb == 0 else qb - 1 + kk
                    self.mm(po.ap[:, hh * 64:(hh + 1) * 64], pT.ap[:, hh, kk, :], self.va.ap[:, kb, g * 64:(g + 1) * 64],
                            kk == 0, kk == nkb - 1, [pT.buf, self.va.buf], [po.buf])
            for hh in range(4):
                h = g * 4 + hh
                self.ts("dve", self.ya.ap[:, qb, h * 64:(h + 1) * 64], po.ap[:, hh * 64:(hh + 1) * 64],
                        rden.ap[:, hh:hh + 1], None, ALU.mult, None, [po.buf, rden.buf], [self.ya.buf])

        stage_a(0)
        for it in range(len(iters)):
            if it + 1 < len(iters):
                stage_a(it + 1)
            stage_b(it)

    def phase_sb(self, l, b):
        cv, S, NG = self.cv, self.S, self.NG
        zb = Rot([0, 1, 2, 3])
        e_r = Rot([cv.alloc([512], F32) for _ in range(3)])
        sp_r = Rot([cv.alloc([512], BF16) for _ in range(3)])
        a_r = Rot([cv.alloc([512], BF16) for _ in range(3)])
        Ssets = [[cv.alloc([512], BF16) for _ in range(2)] for _ in range(2)]
        cbb = self.cb.buf
        pos = [self.banks[4 + j] for j in range(4)]
        steps = []
        hg = 0
        for h in range(8):
            for G in range(NG):
                for kb in range(4 * G + 3, -1, -1):
                    steps.append(dict(h=h, G=G, kb=kb, hg=hg, first=(kb == 4 * G + 3), last=(kb == 0),
                                      step=4 * G + 3 - kb))
                hg += 1
        n_slots = 8 * NG

        def stage_a(st):
            h, G, kb = st["h"], st["G"], st["kb"]
            c, pb = h // 2, (h % 2) * 64
            q0 = G * 512
            qlo = max(kb * 128, q0)
            off = qlo - q0
            zbank = self.banks[zb.next()]
            z = zbank.ap[:, off:512]
            self.mm(z, self.kTs.ap[pb:pb + 64, c, kb * 128:(kb + 1) * 128], self.qTs.ap[pb:pb + 64, c, qlo:q0 + 512],
                    True, False, [self.kTs.buf, self.qTs.buf], [zbank.buf])
            if kb >= 4 * G:
                self.mm(zbank.ap[:, off:off + 128], self.ident, self.negmask, False, False, [cbb], [zbank.buf])
            e, sp = e_r.next(), sp_r.next()
            self.act(e.ap[:, off:512], z, AF.Exp, [zbank.buf], [e.buf])
            self.act(sp.ap[:, off:512], e.ap[:, off:512], AF.Ln, [e.buf], [sp.buf], bias=1.0)
            st.update(zbank=zbank, z=z, off=off, sp=sp)

        def stage_b(st):
            h, G, kb, off, zbank, z, sp = st["h"], st["G"], st["kb"], st["off"], st["zbank"], st["z"], st["sp"]
            Sb = Ssets[st["hg"] % 2]
            if st["first"]:
                self.memset("pool", Sb[0].ap, 0.0, [Sb[0].buf])
                self.memset("pool", Sb[1].ap, 0.0, [Sb[1].buf])
            Scur, Snxt = Sb[st["step"] % 2], Sb[(st["step"] + 1) % 2]
            self.mm(z, self.wsuf, sp.ap[:, off:512], False, False, [cbb, sp.buf], [zbank.buf])
            self.mm(z, self.negones, Scur.ap[:, off:512], False, True, [cbb, Scur.buf], [zbank.buf])
            if kb > 0:
                self.tt("pool", Snxt.ap[:, off:512], Scur.ap[:, off:512], sp.ap[:, off:512], ALU.add,
                        [Scur.buf, sp.buf], [Snxt.buf])
            a = a_r.next()
            self.act(a.ap[:, off:512], z, AF.Exp, [zbank.buf], [a.buf])
            for j in range(off // 128, 4):
                self.mm(pos[j].ap[:, 0:64], a.ap[:, j * 128:(j + 1) * 128], self.vs.ap[:, kb, h * 64:(h + 1) * 64],
                        kb == 4 * G + j, kb == 0, [a.buf, self.vs.buf], [pos[j].buf])
            if st["last"]:
                for j in range(4):
                    self.cp("dve", self.ybs.ap[:, 4 * G + j, h * 64:(h + 1) * 64], pos[j].ap[:, 0:64], [pos[j].buf], [self.ybs.buf])
                self.bg_pump_slot()

        stage_a(steps[0])
        for i, st in enumerate(steps):
            if i + 1 < len(steps):
                stage_a(steps[i + 1])
            stage_b(st)

    def bg_pump_slot(self):
        self.bg_slots_left = max(self.bg_slots_left - 1, 0)
        n = -(-len(self.bg) // (self.bg_slots_left + 1))
        for _ in range(min(n, len(self.bg))):
            self.bg.pop(0)()

    def bg_flush(self):
        while self.bg:
            self.bg.pop(0)()

    def phase_tok(self, l, b):
        cv, d, db, S, NQB = self.cv, self.d, self.db, self.S, self.NQB
        wg = cv.alloc([8, 2048], BF16)
        for k in range(8):
            self.dma("pool", wg.ap[:, k, :], d["w_in"][l, k * 128:(k + 1) * 128, 2304:4352], [db["w_in"]], [wg.buf])
        wab = cv.alloc([8, D], BF16)
        self.dma("pool", wab.ap[:, 0:4, :], d["w_a"][l].rearrange("(k p) n -> p k n", p=128), [db["w_a"]], [wab.buf])
        self.dma("pool", wab.ap[:, 4:8, :], d["w_b"][l].rearrange("(k p) n -> p k n", p=128), [db["w_b"]], [wab.buf])
        wo = cv.alloc([8, D], BF16)
        self.dma("pool", wo.ap, d["w_out"][l].rearrange("(k p) n -> p k n", p=128), [db["w_out"]], [wo.buf])
        bg = cv.alloc([2048], F32)
        self.dma("sp", bg.ap, d["b_in"][l, 2304:4352].partition_broadcast(128), [db["b_in"]], [bg.buf])
        lg = cv.alloc([D], F32)
        lb = cv.alloc([D], F32)
        self.dma("sp", lg.ap, d["ln1_g"][l, :].partition_broadcast(128), [db["ln1_g"]], [lg.buf])
        self.dma("sp", lb.ap, d["ln1_b"][l, :].partition_broadcast(128), [db["ln1_b"]], [lb.buf])
        xTt = Rot([cv.alloc([8, 128], BF16) for _ in range(2)])
        xr = Rot([cv.alloc([D], F32) for _ in range(3)])
        gt = Rot([cv.alloc([2048], F32) for _ in range(1)])
        yT = Rot([cv.alloc([8, 128], BF16) for _ in range(2)])
        t1 = Rot([cv.alloc([512], F32) for _ in range(2)])
        t2 = Rot([cv.alloc([512], F32) for _ in range(2)])
        mg = Rot([cv.alloc([D], BF16) for _ in range(2)])
        mT = Rot([cv.alloc([8, 128], BF16) for _ in range(2)])
        u = Rot([cv.alloc([D], F32) for _ in range(1)])
        x1 = Rot([cv.alloc([D], F32) for _ in range(2)])
        x1b = Rot([cv.alloc([D], BF16) for _ in range(2)])
        x1T = Rot([cv.alloc([8, 128], F32) for _ in range(1)])
        st6 = Rot([cv.alloc([2, 6], F32) for _ in range(2)])
        mv = Rot([cv.alloc([2], F32) for _ in range(2)])
        rstd = Rot([cv.alloc([1], F32) for _ in range(2)])
        bk = Rot([0, 1, 2, 3])
        tb = Rot([4, 5])
        t0 = b * NQB
        src = d["x"] if l == 0 else d["xres"]
        sb_ = db["x"] if l == 0 else db["xres"]
        T_ = {}

        def load(t):
            i = t0 + t
            xt, xrt = xTt.next(), xr.next()
            self.dma("sp", xt.ap, d["xT"][i * 128:(i + 1) * 128, :].rearrange("p (k t) -> p k t", t=128), [db["xT"]], [xt.buf])
            self.dma("sp", xrt.ap, src[i * 128:(i + 1) * 128, :], [sb_], [xrt.buf])
            T_[t] = dict(xt=xt, xrt=xrt)

        def s1(t):
            st = T_[t]
            xt = st["xt"]
            g = gt.next()
            tbi = tb.next()
            pt = self.bank_bf(tbi)
            ptb = self.banks[tbi].buf
            for k in range(4):
                self.tp(pt[:, k, :], self.ya.ap[:, t, k * 128:(k + 1) * 128], self.ident, [self.ya.buf, self.cb.buf], [ptb])
            for k in range(4):
                self.tp(pt[:, 4 + k, :], self.ybs.ap[:, t, k * 128:(k + 1) * 128], self.ident, [self.ybs.buf, self.cb.buf], [ptb])
            yTt = yT.next()
            self.cp("act", yTt.ap, pt, [ptb], [yTt.buf])
            for nh in range(4):
                bank = self.banks[bk.next()]
                for k in range(8):
                    self.mm(bank.ap, xt.ap[:, k, :], wg.ap[:, k, nh * 512:(nh + 1) * 512], k == 0, k == 7, [xt.buf, wg.buf], [bank.buf])
                self.tt("dve", g.ap[:, nh * 512:(nh + 1) * 512], bank.ap, bg.ap[:, nh * 512:(nh + 1) * 512], ALU.add,
                        [bank.buf, bg.buf], [g.buf])
            self.act(g.ap, g.ap, AF.Sigmoid, [g.buf], [g.buf])
            mgt = mg.next()
            for nh in range(2):
                ba = self.banks[bk.next()]
                for k in range(4):
                    self.mm(ba.ap, yTt.ap[:, k, :], wab.ap[:, k, nh * 512:(nh + 1) * 512], k == 0, k == 3, [yTt.buf, wab.buf], [ba.buf])
                bb = self.banks[bk.next()]
                for k in range(4):
                    self.mm(bb.ap, yTt.ap[:, 4 + k, :], wab.ap[:, 4 + k, nh * 512:(nh + 1) * 512], k == 0, k == 3,
                            [yTt.buf, wab.buf], [bb.buf])
                a1, a2 = t1.next(), t2.next()
                self.tt("dve", a1.ap, ba.ap, g.ap[:, nh * 512:(nh + 1) * 512], ALU.mult, [ba.buf, g.buf], [a1.buf])
                self.tt("dve", a2.ap, bb.ap, g.ap[:, 1024 + nh * 512:1024 + (nh + 1) * 512], ALU.mult, [bb.buf, g.buf], [a2.buf])
                self.tt("pool", mgt.ap[:, nh * 512:(nh + 1) * 512], a1.ap, a2.ap, ALU.add, [a1.buf, a2.buf], [mgt.buf])
            st["mgt"] = mgt
            if self.debug:
                i = t0 + t
                self.dma("sp", d["dbg_y"][i * 128:(i + 1) * 128, 0:512], self.ya.ap[:, t, :], [self.ya.buf], [], [db["dbg_y"]])
                self.dma("sp", d["dbg_y"][i * 128:(i + 1) * 128, 512:1024], self.ybs.ap[:, t, :], [self.ybs.buf], [], [db["dbg_y"]])

        def s2a(t):
            st = T_[t]
            mgt = st["mgt"]
            tbi = tb.next()
            pt = self.bank_bf(tbi)
            ptb = self.banks[tbi].buf
            for k in range(8):
                self.tp(pt[:, k, :], mgt.ap[:, k * 128:(k + 1) * 128], self.ident, [mgt.buf, self.cb.buf], [ptb])
            mTt = mT.next()
            self.cp("act", mTt.ap, pt, [ptb], [mTt.buf])
            st["mTt"] = mTt

        def s2b(t):
            st = T_[t]
            i = t0 + t
            mTt, xrt = st["mTt"], st["xrt"]
            ut = u.next()
            for nh in range(2):
                bo = self.banks[bk.next()]
                for k in range(8):
                    self.mm(bo.ap, mTt.ap[:, k, :], wo.ap[:, k, nh * 512:(nh + 1) * 512], k == 0, k == 7, [mTt.buf, wo.buf], [bo.buf])
                self.stt("dve", ut.ap[:, nh * 512:(nh + 1) * 512], xrt.ap[:, nh * 512:(nh + 1) * 512], ALPHA, bo.ap,
                         ALU.mult, ALU.add, [xrt.buf, bo.buf], [ut.buf])
            x1t = x1.next()
            self.layernorm(ut, lg, lb, x1t, st6.next(), mv.next(), rstd.next())
            self.dma("sp", d["x1"][i * 128:(i + 1) * 128, :], x1t.ap, [x1t.buf], [], [db["x1"]])
            x1bt = x1b.next()
            self.cp("pool", x1bt.ap, x1t.ap, [x1t.buf], [x1bt.buf])
            self.dma("sp", d["x1b"][i * 128:(i + 1) * 128, :], x1bt.ap, [x1bt.buf], [], [db["x1b"]])
            st["x1t"] = x1t

        def s3a(t):
            st = T_[t]
            x1t = st["x1t"]
            x1Tt = x1T.next()
            for hf in range(2):
                fb = self.banks[6 + hf]
                for k in range(4):
                    kk = hf * 4 + k
                    self.tp(fb.ap[:, k * 128:(k + 1) * 128], x1t.ap[:, kk * 128:(kk + 1) * 128], self.identf,
                            [x1t.buf, self.cf.buf], [fb.buf])
                self.cp("act", x1Tt.ap[:, hf * 4:hf * 4 + 4, :], fb.ap.rearrange("p (k t) -> p k t", t=128), [fb.buf], [x1Tt.buf])
            st["x1Tt"] = x1Tt

        def s3b(t):
            st = T_.pop(t)
            i = t0 + t
            x1Tt = st["x1Tt"]
            rbk = self.banks[bk.next()]
            for k in range(8):
                self.mm(rbk.ap[:, 0:NE], x1Tt.ap[:, k, :], self.wr.ap[:, k, :], k == 0, k == 7, [x1Tt.buf, self.wr.buf], [rbk.buf])
            self.act(self.aff.ap[:, i, :], rbk.ap[:, 0:NE], AF.Sigmoid, [rbk.buf], [self.aff.buf])

        load(0)
        for step in range(NQB + 2):
            if step + 1 < NQB:
                load(step + 1)
            if step < NQB:
                s1(step)
            if 0 <= step - 1 < NQB:
                s2a(step - 1)
            if 0 <= step - 2 < NQB:
                s3a(step - 2)
            if 0 <= step - 1 < NQB:
                s2b(step - 1)
            if 0 <= step - 2 < NQB:
                s3b(step - 2)

    def phase_route(self, l):
        cv, d, db, NT, NBLK = self.cv, self.d, self.db, self.NT, self.NBLK
        N = NT * NE
        A = lambda shape, dt=F32: cv.alloc(shape, dt)
        self.dlo_i = A([NT], I32)
        self.dhi_i = A([NT], I32)
        self.glo = A([NT])
        self.ghi = A([NT])
        self.widx = A([self.NB2], I32)
        cv.mark()
        bsd = A([NT, NE])
        self.tt("dve", bsd.ap, self.aff.ap, self.rb.ap.unsqueeze(1).to_broadcast([128, NT, NE]), ALU.add,
                [self.aff.buf, self.rb.buf], [bsd.buf])
        b4 = bsd.ap.rearrange("p t (g f) -> p (t g) f", f=4)
        NGp = NT * 8
        top2 = A([NGp])
        thr = A([NGp])
        tmp = A([NGp])
        first = True
        for i in range(4):
            for j in range(i + 1, 4):
                if first:
                    self.tt("dve", top2.ap, b4[:, :, i], b4[:, :, j], ALU.add, [bsd.buf], [top2.buf])
                    self.tt("dve", thr.ap, b4[:, :, i], b4[:, :, j], ALU.min, [bsd.buf], [thr.buf])
                    first = False
                else:
                    self.tt("dve", tmp.ap, b4[:, :, i], b4[:, :, j], ALU.add, [bsd.buf], [tmp.buf])
                    self.tt("dve", top2.ap, top2.ap, tmp.ap, ALU.max, [top2.buf, tmp.buf], [top2.buf])
                    self.tt("dve", tmp.ap, b4[:, :, i], b4[:, :, j], ALU.min, [bsd.buf], [tmp.buf])
                    self.tt("dve", thr.ap, thr.ap, tmp.ap, ALU.max, [thr.buf, tmp.buf], [thr.buf])
        gmax = A([NT])
        t2v = top2.ap.rearrange("p (t g) -> p t g", g=8)
        self.red("dve", gmax.ap, t2v, ALU.max, [top2.buf], [gmax.buf])
        gsel = A([NT, 8])
        self.tt("dve", gsel.ap, t2v, gmax.ap.unsqueeze(2).to_broadcast([128, NT, 8]), ALU.is_ge, [top2.buf, gmax.buf], [gsel.buf])
        sel = A([NT, NE])
        s4 = sel.ap.rearrange("p t (g f) -> p (t g) f", f=4)
        self.tt("dve", s4, b4, thr.ap.unsqueeze(2).to_broadcast([128, NGp, 4]), ALU.is_ge, [bsd.buf, thr.buf], [sel.buf])
        self.tt("dve", s4, s4, gsel.ap.rearrange("p t g -> p (t g)").unsqueeze(2).to_broadcast([128, NGp, 4]), ALU.mult,
                [sel.buf, gsel.buf], [sel.buf])
        gd = A([NT, NE])
        self.tt("dve", gd.ap, sel.ap, self.aff.ap, ALU.mult, [sel.buf, self.aff.buf], [gd.buf])
        wsum = A([NT])
        self.red("dve", wsum.ap, gd.ap, ALU.add, [gd.buf], [wsum.buf])
        self.tr.op("dve", lambda E: E.reciprocal(out=wsum.ap, in_=wsum.ap), [wsum.buf], [wsum.buf])
        self.tt("dve", gd.ap, gd.ap, wsum.ap.unsqueeze(2).to_broadcast([128, NT, NE]), ALU.mult, [gd.buf, wsum.buf], [gd.buf])
        selb = A([N], BF16)
        self.cp("dve", selb.ap, sel.ap.rearrange("p t e -> p (t e)"), [sel.buf], [selb.buf])
        cnt = A([NT, NE])
        rank = A([NT, NE])
        cntf = cnt.ap.rearrange("p t e -> p (t e)")
        rankf = rank.ap.rearrange("p t e -> p (t e)")
        for c0 in range(0, N, 512):
            w = min(512, N - c0)
            b0, b1 = self.banks[0], self.banks[1]
            self.mm(b0.ap[:, 0:w], self.ones, selb.ap[:, c0:c0 + w], True, True, [self.cb.buf, selb.buf], [b0.buf])
            self.mm(b1.ap[:, 0:w], self.ustrict, selb.ap[:, c0:c0 + w], True, True, [self.cb.buf, selb.buf], [b1.buf])
            self.cp("dve", cntf[:, c0:c0 + w], b0.ap[:, 0:w], [b0.buf], [cnt.buf])
            self.cp("dve", rankf[:, c0:c0 + w], b1.ap[:, 0:w], [b1.buf], [rank.buf])
        cum = A([NT + 1, NE])
        self.memset("dve", cum.ap[:, 0, :], 0.0, [cum.buf])
        for i in range(NT):
            self.tt("dve", cum.ap[:, i + 1, :], cum.ap[:, i, :], cnt.ap[:, i, :], ALU.add, [cum.buf, cnt.buf], [cum.buf])
        pad = A([NE])
        NH = NT // 2
        cmp_ = A([NE, NH])
        self.tt("dve", cmp_.ap, cum.ap[:, NT, :].unsqueeze(2).to_broadcast([128, NE, NH]),
                self.bstart[:, 0:NT:2].unsqueeze(1).to_broadcast([128, NE, NH]), ALU.is_gt, [cum.buf, self.cf.buf], [cmp_.buf])
        self.red("dve", pad.ap, cmp_.ap, ALU.add, [cmp_.buf], [pad.buf])
        self.ts("dve", pad.ap, pad.ap, 256.0, None, ALU.mult, None, [pad.buf], [pad.buf])
        pend = A([NE + 1])
        self.memset("dve", pend.ap[:, 0:1], 0.0, [pend.buf])
        for e in range(NE):
            self.tt("dve", pend.ap[:, e + 1:e + 2], pend.ap[:, e:e + 1], pad.ap[:, e:e + 1], ALU.add, [pend.buf, pad.buf], [pend.buf])
        dest = A([NT, NE])
        self.tt("dve", dest.ap, cum.ap[:, 0:NT, :], rank.ap, ALU.add, [cum.buf, rank.buf], [dest.buf])
        self.tt("dve", dest.ap, dest.ap, pend.ap[:, 0:NE].unsqueeze(1).to_broadcast([128, NT, NE]), ALU.add,
                [dest.buf, pend.buf], [dest.buf])
        BIG = 1.0e6
        dm = A([NT, NE])
        msk = A([NT, NE])
        self.ts("dve", msk.ap, sel.ap, -1.0, None, ALU.add, None, [sel.buf], [msk.buf])
        self.ts("dve", msk.ap, msk.ap, -BIG, None, ALU.mult, None, [msk.buf], [msk.buf])
        self.tt("dve", dm.ap, dest.ap, msk.ap, ALU.add, [dest.buf, msk.buf], [dm.buf])
        dlo = A([NT])
        dhi = A([NT])
        self.red("dve", dlo.ap, dm.ap, ALU.min, [dm.buf], [dlo.buf])
        self.tt("dve", dm.ap, dest.ap, msk.ap, ALU.subtract, [dest.buf, msk.buf], [dm.buf])
        self.red("dve", dhi.ap, dm.ap, ALU.max, [dm.buf], [dhi.buf])
        glo, ghi = self.glo, self.ghi
        eq = A([NT, NE])
        self.tt("dve", eq.ap, dest.ap, dlo.ap.unsqueeze(2).to_broadcast([128, NT, NE]), ALU.is_equal, [dest.buf, dlo.buf], [eq.buf])
        self.tt("dve", eq.ap, eq.ap, gd.ap, ALU.mult, [eq.buf, gd.buf], [eq.buf])
        self.red("dve", glo.ap, eq.ap, ALU.add, [eq.buf], [glo.buf])
        self.tt("dve", eq.ap, dest.ap, dhi.ap.unsqueeze(2).to_broadcast([128, NT, NE]), ALU.is_equal, [dest.buf, dhi.buf], [eq.buf])
        self.tt("dve", eq.ap, eq.ap, gd.ap, ALU.mult, [eq.buf, gd.buf], [eq.buf])
        self.red("dve", ghi.ap, eq.ap, ALU.add, [eq.buf], [ghi.buf])
        self.cp("dve", self.dlo_i.ap, dlo.ap, [dlo.buf], [self.dlo_i.buf])
        self.cp("dve", self.dhi_i.ap, dhi.ap, [dhi.buf], [self.dhi_i.buf])
        NB2 = self.NB2
        be = A([NB2])
        self.memset("dve", be.ap, 0.0, [be.buf])
        for e in range(NE):
            self.stt("dve", be.ap, self.bstart[:, 0:2 * NB2:2], pend.ap[:, e + 1:e + 2], be.ap, ALU.is_ge, ALU.add,
                     [self.cf.buf, pend.buf, be.buf], [be.buf])
        self.ts("dve", be.ap, be.ap, float(NE - 1), None, ALU.min, None, [be.buf], [be.buf])
        self.ts("dve", be.ap, be.ap, 128.0, None, ALU.mult, None, [be.buf], [be.buf])
        self.ts("dve", be.ap, be.ap, self.pidx, None, ALU.add, None, [be.buf, self.cf.buf], [be.buf])
        if l > 0:
            self.ts("dve", be.ap, be.ap, float(l * NE * 128), None, ALU.add, None, [be.buf], [be.buf])
        self.cp("dve", self.widx.ap, be.ap, [be.buf], [self.widx.buf])
        xl = Rot([cv.alloc([D], BF16) for _ in range(3)])
        for i in range(NT):
            xt = xl.next()
            self.dma("sp", xt.ap, d["x1b"][i * 128:(i + 1) * 128, :], [db["x1b"]], [xt.buf])
            self.scatter(d["xs"], self.dlo_i.ap[:, i:i + 1], xt.ap, [xt.buf, self.dlo_i.buf], [db["xs"]])
            self.scatter(d["xs"], self.dhi_i.ap[:, i:i + 1], xt.ap, [xt.buf, self.dhi_i.buf], [db["xs"]])

    def phase_moe_blocks(self, l):
        cv, d, db, NBLK = self.cv, self.d, self.db, self.NBLK
        wgs = Rot([cv.alloc([8, DE], BF16) for _ in range(3)])
        wus = Rot([cv.alloc([8, DE], BF16) for _ in range(3)])
        wds = Rot([cv.alloc([4, D], BF16) for _ in range(3)])
        xsb = Rot([cv.alloc([D], BF16) for _ in range(3)])
        xsT = Rot([cv.alloc([8, 128], BF16) for _ in range(2)])
        sg = Rot([cv.alloc([DE], F32) for _ in range(2)])
        hb = Rot([cv.alloc([DE], BF16) for _ in range(2)])
        hT = Rot([cv.alloc([4, 128], BF16) for _ in range(2)])
        yo = Rot([cv.alloc([D], F32) for _ in range(2)])
        bk = Rot([0, 1, 2, 3, 4, 5])
        tb = Rot([6, 7])
        B_ = {}
        U_ = {}
        NB2 = self.NB2
        NU = 2 * NB2

        def loadw(blk):
            wgt, wut, wdt = wgs.next(), wus.next(), wds.next()
            ix = self.widx.ap[:, blk:blk + 1]
            self.gather(wgt.ap.rearrange("p k n -> p (k n)"), d["w_gate"], ix, [db["w_gate"], self.widx.buf], [wgt.buf])
            self.gather(wut.ap.rearrange("p k n -> p (k n)"), d["w_up"], ix, [db["w_up"], self.widx.buf], [wut.buf])
            self.gather(wdt.ap.rearrange("p k n -> p (k n)"), d["w_down"], ix, [db["w_down"], self.widx.buf], [wdt.buf])
            B_[blk] = dict(wgt=wgt, wut=wut, wdt=wdt)

        def loadx(u):
            xt = xsb.next()
            self.dma("sp", xt.ap, d["xs"][u * 128:(u + 1) * 128, :], [db["xs"]], [xt.buf])
            U_[u] = dict(xt=xt)

        def sA(u):
            st = U_[u]
            wgt, wut, xt = B_[u // 2]["wgt"], B_[u // 2]["wut"], st["xt"]
            tbi = tb.next()
            pt, ptb = self.bank_bf(tbi), self.banks[tbi].buf
            for k in range(8):
                self.tp(pt[:, k, :], xt.ap[:, k * 128:(k + 1) * 128], self.ident, [xt.buf, self.cb.buf], [ptb])
            xT = xsT.next()
            self.cp("act", xT.ap, pt, [ptb], [xT.buf])
            bg_, bu_ = self.banks[bk.next()], self.banks[bk.next()]
            for k in range(8):
                self.mm(bg_.ap, xT.ap[:, k, :], wgt.ap[:, k, :], k == 0, k == 7, [xT.buf, wgt.buf], [bg_.buf])
            for k in range(8):
                self.mm(bu_.ap, xT.ap[:, k, :], wut.ap[:, k, :], k == 0, k == 7, [xT.buf, wut.buf], [bu_.buf])
            sgt, ht = sg.next(), hb.next()
            self.act(sgt.ap, bg_.ap, AF.Silu, [bg_.buf], [sgt.buf])
            self.tt("dve", ht.ap, sgt.ap, bu_.ap, ALU.mult, [sgt.buf, bu_.buf], [ht.buf])
            st["ht"] = ht

        def sB(u):
            st = U_.pop(u)
            ht, wdt = st["ht"], B_[u // 2]["wdt"]
            tbi = tb.next()
            pt, ptb = self.bank_bf(tbi), self.banks[tbi].buf
            for k in range(4):
                self.tp(pt[:, k, :], ht.ap[:, k * 128:(k + 1) * 128], self.ident, [ht.buf, self.cb.buf], [ptb])
            hTt = hT.next()
            self.cp("act", hTt.ap, pt[:, 0:4, :], [ptb], [hTt.buf])
            yt = yo.next()
            for nh in range(2):
                by = self.banks[bk.next()]
                for k in range(4):
                    self.mm(by.ap, hTt.ap[:, k, :], wdt.ap[:, k, nh * 512:(nh + 1) * 512], k == 0, k == 3, [hTt.buf, wdt.buf], [by.buf])
                self.cp("dve", yt.ap[:, nh * 512:(nh + 1) * 512], by.ap, [by.buf], [yt.buf])
            self.dma("sp", d["yb"][u * 128:(u + 1) * 128, :], yt.ap, [yt.buf], [], [db["yb"]])
            if u % 2 == 1:
                B_.pop(u // 2)

        loadw(0)
        loadw(1)
        loadx(0)
        loadx(1)
        sA(0)
        for u in range(NU):
            if u % 2 == 0 and u // 2 + 2 < NB2:
                loadw(u // 2 + 2)
            if u + 2 < NU:
                loadx(u + 2)
            if u + 1 < NU:
                sA(u + 1)
            sB(u)

    def phase_combine(self, l):
        cv, d, db, NT = self.cv, self.d, self.db, self.NT
        last = l == self.L - 1
        lg = cv.alloc([D], F32)
        lb = cv.alloc([D], F32)
        self.dma("sp", lg.ap, d["ln2_g"][l, :].partition_broadcast(128), [db["ln2_g"]], [lg.buf])
        self.dma("sp", lb.ap, d["ln2_b"][l, :].partition_broadcast(128), [db["ln2_b"]], [lb.buf])
        y0 = Rot([cv.alloc([D], F32) for _ in range(2)])
        y1 = Rot([cv.alloc([D], F32) for _ in range(2)])
        x1 = Rot([cv.alloc([D], F32) for _ in range(2)])
        u = Rot([cv.alloc([D], F32) for _ in range(2)])
        xo = Rot([cv.alloc([D], F32) for _ in range(2)])
        xb = Rot([cv.alloc([D], BF16) for _ in range(2)])
        xTt = Rot([cv.alloc([8, 128], BF16) for _ in range(2)])
        st6 = Rot([cv.alloc([2, 6], F32) for _ in range(2)])
        mv = Rot([cv.alloc([2], F32) for _ in range(2)])
        rstd = Rot([cv.alloc([1], F32) for _ in range(2)])
        bk = Rot([6, 7])
        loaded = {}

        def load(i):
            a0, a1, xt = y0.next(), y1.next(), x1.next()
            self.gather(a0.ap, d["yb"], self.dlo_i.ap[:, i:i + 1], [db["yb"], self.dlo_i.buf], [a0.buf])
            self.gather(a1.ap, d["yb"], self.dhi_i.ap[:, i:i + 1], [db["yb"], self.dhi_i.buf], [a1.buf])
            self.dma("sp", xt.ap, d["x1"][i * 128:(i + 1) * 128, :], [db["x1"]], [xt.buf])
            loaded[i] = (a0, a1, xt)

        load(0)
        for i in range(NT):
            if i + 1 < NT:
                load(i + 1)
            a0, a1, xt = loaded.pop(i)
            ut = u.next()
            self.ts("dve", ut.ap, a0.ap, self.glo.ap[:, i:i + 1], None, ALU.mult, None, [a0.buf, self.glo.buf], [ut.buf])
            self.stt("dve", ut.ap, a1.ap, self.ghi.ap[:, i:i + 1], ut.ap, ALU.mult, ALU.add, [a1.buf, self.ghi.buf, ut.buf], [ut.buf])
            self.stt("dve", ut.ap, xt.ap, ALPHA, ut.ap, ALU.mult, ALU.add, [xt.buf, ut.buf], [ut.buf])
            xot = xo.next()
            self.layernorm(ut, lg, lb, xot, st6.next(), mv.next(), rstd.next())
            if last:
                self.dma("sp", d["y"][i * 128:(i + 1) * 128, :], xot.ap, [xot.buf], [], [db["y"]])
            else:
                self.dma("sp", d["xres"][i * 128:(i + 1) * 128, :], xot.ap, [xot.buf], [], [db["xres"]])
                self.emit_xT(xot, i, xb.next(), xTt.next(), bk.next())


def make_consts(NBLK):
    cb = np.zeros((128, 6, 128), np.float32)
    i = np.arange(128)
    cb[:, 0] = np.eye(128)
    cb[:, 1] = -1.0 * (i[:, None] >= i[None, :])
    cb[:, 2] = -1.0
    cb[:, 3] = np.where(i[:, None] >= i[None, :], NEG, 0.0)
    cb[:, 4] = (i[:, None] < i[None, :])
    cb[:, 5] = 1.0
    ncf = 128 + 8 * 256 + 1 + NBLK + 2
    cf = np.zeros((128, ncf), np.float32)
    cf[:, 0:128] = np.eye(128)
    slopes = np.exp2(-8.0 * (np.arange(8, dtype=np.float32) + 1.0) / 8).astype(np.float32)
    j = np.arange(256)
    dist = i[:, None] + 128 - j[None, :]
    valid = (dist >= 0) & (dist < 128)
    sb = np.where(valid[None], -slopes[:, None, None] * dist[None].astype(np.float32), NEG).astype(np.float32)
    cf[:, 128:128 + 2048] = sb.transpose(1, 0, 2).reshape(128, 2048)
    o = 128 + 2048
    cf[:, o] = i
    cf[:, o + 1:o + 1 + NBLK] = 128.0 * np.arange(NBLK)[None, :]
    return cb.reshape(128, 768).astype(NPBF), cf


_CACHE = {}


def get_prog(S, NB, L, debug=False):
    key = (S, NB, L, debug)
    if key not in _CACHE:
        p = Prog(S, NB, L, debug)
        nc = p.build()
        _CACHE[key] = (p, nc)
    return _CACHE[key]


def prep_shared(inp, L):
    f = lambda a: np.ascontiguousarray(np.asarray(a, dtype=np.float32))
    w_in = f(inp["w_in"])[:L]
    b_in = f(inp["b_in"])[:L]
    perm = np.arange(NPROJ)
    perm[:512] = np.concatenate([np.arange(h * 64, (h + 1) * 64) for h in QPERM])
    w_in = np.ascontiguousarray(w_in[:, :, perm])
    b_in = np.ascontiguousarray(b_in[:, perm])
    b_in_fm = np.ascontiguousarray(b_in.reshape(L, 34, 128).transpose(0, 2, 1))
    sh = dict(
        w_in=w_in, b_in=b_in, b_in_fm=b_in_fm, sinks=f(inp["attn_sinks"])[:L],
        w_a=f(inp["w_branch_a"])[:L], w_b=f(inp["w_branch_b"])[:L], w_out=f(inp["w_out"])[:L],
        ln1_g=f(inp["ln1_g"])[:L], ln1_b=f(inp["ln1_b"])[:L], ln2_g=f(inp["ln2_g"])[:L], ln2_b=f(inp["ln2_b"])[:L],
        w_router=f(inp["w_router"]), router_bias=f(inp["router_bias"]).reshape(1, NE),
        w_gate=np.ascontiguousarray(f(inp["w_gate"])[:L].reshape(L, NE, 8, 128, DE).transpose(0, 1, 3, 2, 4)).reshape(L * NE * 128, 8 * DE),
        w_up=np.ascontiguousarray(f(inp["w_up"])[:L].reshape(L, NE, 8, 128, DE).transpose(0, 1, 3, 2, 4)).reshape(L * NE * 128, 8 * DE),
        w_down=np.ascontiguousarray(f(inp["w_down"])[:L].reshape(L, NE, 4, 128, D).transpose(0, 1, 3, 2, 4)).reshape(L * NE * 128, 4 * D),
    )
    return sh


def run(inp, S, NB, L, n_cores, debug=False):
    p, nc = get_prog(S, NB, L, debug)
    sh = prep_shared(inp, L)
    cb, cf = make_consts(p.NBLK)
    sh["cb"] = cb
    sh["cf"] = cf
    x = np.ascontiguousarray(np.asarray(inp["x"], dtype=np.float32)).reshape(n_cores, NB * S, D)
    in_maps = []
    for c in range(n_cores):
        m = dict(sh)
        m["x"] = x[c]
        in_maps.append(m)
    res = run_bass_kernel_spmd(nc, in_maps, core_ids=list(range(n_cores)))
    return res.results


def kernel(x, w_in, b_in, attn_sinks, w_branch_a, w_branch_b, w_out, ln1_g, ln1_b,
           w_router, router_bias, w_gate, w_up, w_down, ln2_g, ln2_b):
    inp = dict(x=x, w_in=w_in, b_in=b_in, attn_sinks=attn_sinks, w_branch_a=w_branch_a, w_branch_b=w_branch_b,
               w_out=w_out, ln1_g=ln1_g, ln1_b=ln1_b, w_router=w_router, router_bias=router_bias,
               w_gate=w_gate, w_up=w_up, w_down=w_down, ln2_g=ln2_g, ln2_b=ln2_b)
    B, S, _ = np.asarray(x).shape
    n_cores = 8
    NB = B // n_cores
    res = run(inp, S, NB, 4, n_cores)
    out = np.stack([r["y"] for r in res], 0).reshape(B, S, D)
    return out.astype(np.float32)
```
